# Optimizing a Trainium2 kernel written in Bass

```python
import math
import jax, jax.numpy as jnp
from jax import lax
import numpy as np

D_MODEL = 1024
BATCH = 8
SEQ = 2048
DEPTH = 2

N_A_LAYERS = DEPTH // 2
N_B_LAYERS = DEPTH - N_A_LAYERS
N_EVEN = (DEPTH + 1) // 2
N_ODD = DEPTH // 2
DEEPNORM_ALPHA = (2.0 * DEPTH) ** 0.25
DEEPNORM_BETA = (8.0 * DEPTH) ** -0.25
LN_EPS = 1e-5

DN_HEADS = 8
DN_HEAD_DIM = D_MODEL // DN_HEADS
DN_WIDTH = DN_HEADS * DN_HEAD_DIM
DN_CONV = 4
DN_CHUNK = 64
DN_NORM_EPS = 1e-6
DN_PROJ = 4 * DN_WIDTH + 2 * DN_HEADS

MB_HEADS = 8
MB_HEAD_DIM = D_MODEL // MB_HEADS
MB_WIDTH = MB_HEADS * MB_HEAD_DIM
MOBA_BLOCK = 256
MOBA_TOPK = 3
MOBA_Q_CHUNK = 8
ROPE_THETA = 10000.0
NEG_INF = -1e30

FFN_HIDDEN = D_MODEL * 7 // 2
N_EXPERTS = 8
TOP_K = 2

kernel_name = "yoco_deltanet_moba_moe_block"


def layer_norm(x, g, b):
    xf = x.astype(jnp.float32)
    mu = jnp.mean(xf, axis=-1, keepdims=True)
    var = jnp.mean(jnp.square(xf - mu), axis=-1, keepdims=True)
    return ((xf - mu) * lax.rsqrt(var + LN_EPS) * g + b).astype(x.dtype)


def apply_rope(t, positions):
    dh = t.shape[-1]
    half = dh // 2
    inv_freq = ROPE_THETA ** (-jnp.arange(half, dtype=jnp.float32) / half)
    ang = positions.astype(jnp.float32)[:, None] * inv_freq[None, :]
    cos = jnp.cos(ang)[None, :, None, :]
    sin = jnp.sin(ang)[None, :, None, :]
    tf = t.astype(jnp.float32)
    t1, t2 = tf[..., :half], tf[..., half:]
    return jnp.concatenate([t1 * cos - t2 * sin, t2 * cos + t1 * sin], axis=-1).astype(t.dtype)


def causal_short_conv(x, w):
    k_len = w.shape[0]
    s = x.shape[1]
    xp = jnp.pad(x, ((0, 0), (k_len - 1, 0), (0, 0)))
    y = xp[:, 0:s, :] * w[0]
    for j in range(1, k_len):
        y = y + xp[:, j:j + s, :] * w[j]
    return y


def l2norm(t):
    return t * lax.rsqrt(jnp.sum(t * t, axis=-1, keepdims=True) + DN_NORM_EPS)


def gated_delta_rule(q, k, v, g, beta):
    bsz, s, h, dk = q.shape
    dv = v.shape[-1]
    c = DN_CHUNK
    n = s // c

    def to_chunks(t):
        return jnp.moveaxis(t.reshape(bsz, n, c, h, *t.shape[3:]), 3, 1)

    q, k, v, g, beta = (to_chunks(t) for t in (q, k, v, g, beta))
    gc = jnp.cumsum(g, axis=-1)
    tri = jnp.tril(jnp.ones((c, c), dtype=bool))
    stri = jnp.tril(jnp.ones((c, c), dtype=bool), -1)
    decay = jnp.exp(jnp.where(tri, gc[..., :, None] - gc[..., None, :], NEG_INF))
    kb = k * beta[..., None]
    vb = v * beta[..., None]
    lmat = jnp.where(stri, jnp.einsum('bhnid,bhnjd->bhnij', kb, k) * decay, 0.0)
    amat = lmat + jnp.eye(c, dtype=jnp.float32)
    rhs = jnp.concatenate([vb, kb * jnp.exp(gc)[..., None]], axis=-1)
    sol = lax.linalg.triangular_solve(amat, rhs, left_side=True, lower=True, unit_diagonal=True)
    u, w = sol[..., :dv], sol[..., dv:]
    intra = jnp.einsum('bhnid,bhnjd->bhnij', q, k) * decay
    q_dec = q * jnp.exp(gc)[..., None]
    glast = gc[..., -1]
    k_dec = k * jnp.exp(glast[..., None] - gc)[..., None]
    xs = tuple(jnp.moveaxis(t, 2, 0) for t in (q_dec, k_dec, u, w, intra, glast))

    def step(state, inp):
        qd, kd, ui, wi, ai, gl = inp
        v_new = ui - jnp.einsum('bhcd,bhde->bhce', wi, state)
        o = jnp.einsum('bhcd,bhde->bhce', qd, state) + jnp.einsum('bhcs,bhse->bhce', ai, v_new)
        state = state * jnp.exp(gl)[..., None, None] + jnp.einsum('bhcd,bhce->bhde', kd, v_new)
        return state, o

    s0 = jnp.zeros((bsz, h, dk, dv), jnp.float32)
    _, o = lax.scan(step, s0, xs)
    o = jnp.moveaxis(o, 0, 2)
    return o.transpose(0, 2, 3, 1, 4).reshape(bsz, s, h, dv)


def gated_deltanet_mixer(x, w_in, conv_w, a_log, dt_bias, norm_w, w_out):
    bsz, s, _ = x.shape
    wd = DN_WIDTH
    proj = x @ w_in
    qkv = jax.nn.silu(causal_short_conv(proj[..., :3 * wd], conv_w))
    gate = proj[..., 3 * wd:4 * wd].reshape(bsz, s, DN_HEADS, DN_HEAD_DIM).astype(jnp.float32)
    a_in = proj[..., 4 * wd:4 * wd + DN_HEADS].astype(jnp.float32)
    b_in = proj[..., 4 * wd + DN_HEADS:].astype(jnp.float32)
    shp = (bsz, s, DN_HEADS, DN_HEAD_DIM)
    q = l2norm(qkv[..., :wd].reshape(shp).astype(jnp.float32)) * (DN_HEAD_DIM ** -0.5)
    k = l2norm(qkv[..., wd:2 * wd].reshape(shp).astype(jnp.float32))
    v = qkv[..., 2 * wd:].reshape(shp).astype(jnp.float32)
    beta = jax.nn.sigmoid(b_in)
    g = -jnp.exp(a_log.astype(jnp.float32)) * jax.nn.softplus(a_in + dt_bias.astype(jnp.float32))
    o = gated_delta_rule(q, k, v, g, beta)
    o = o * lax.rsqrt(jnp.mean(o * o, axis=-1, keepdims=True) + DN_NORM_EPS) * norm_w
    o = o * jax.nn.silu(gate)
    return o.reshape(bsz, s, wd).astype(x.dtype) @ w_out


def moba_shared_kv(h, w_kv):
    bsz, s, _ = h.shape
    kv = h @ w_kv
    k = kv[..., :MB_WIDTH].reshape(bsz, s, MB_HEADS, MB_HEAD_DIM)
    v = kv[..., MB_WIDTH:].reshape(bsz, s, MB_HEADS, MB_HEAD_DIM)
    k = apply_rope(k, jnp.arange(s))
    nb = -(-s // MOBA_BLOCK)
    pad = nb * MOBA_BLOCK - s

    def blocks(t):
        t = jnp.pad(t.astype(jnp.float32).transpose(0, 2, 1, 3), ((0, 0), (0, 0), (0, pad), (0, 0)))
        return t.reshape(bsz, MB_HEADS, nb, MOBA_BLOCK, MB_HEAD_DIM)

    kb, vb = blocks(k), blocks(v)
    kmean = jnp.mean(kb, axis=3)
    return kb, vb, kmean


def moba_mixer(x, w_q, w_o, kb, vb, kmean):
    bsz, s, _ = x.shape
    h, dh = MB_HEADS, MB_HEAD_DIM
    nb = kb.shape[2]
    sp = nb * MOBA_BLOCK
    scale = dh ** -0.5
    q = apply_rope((x @ w_q).reshape(bsz, s, h, dh), jnp.arange(s))
    q = q.astype(jnp.float32).transpose(0, 2, 1, 3)

    qb = jnp.pad(q, ((0, 0), (0, 0), (0, sp - s), (0, 0))).reshape(bsz, h, nb, MOBA_BLOCK, dh)
    s_own = jnp.einsum('bhnqd,bhnkd->bhnqk', qb, kb) * scale
    causal = jnp.tril(jnp.ones((MOBA_BLOCK, MOBA_BLOCK), dtype=bool))
    s_own = jnp.where(causal, s_own, NEG_INF)
    m_own = jnp.max(s_own, axis=-1)
    p_own = jnp.exp(s_own - m_own[..., None])
    l_own = jnp.sum(p_own, axis=-1).reshape(bsz, h, sp)[..., :s]
    o_own = jnp.einsum('bhnqk,bhnkd->bhnqd', p_own, vb).reshape(bsz, h, sp, dh)[:, :, :s]
    m_own = m_own.reshape(bsz, h, sp)[..., :s]

    n_sel = min(MOBA_TOPK, nb - 1)
    if n_sel == 0:
        out = o_own / l_own[..., None]
    else:
        q_blk = jnp.arange(s) // MOBA_BLOCK
        gate = jnp.einsum('bhsd,bhnd->bhsn', q, kmean)
        past = jnp.arange(nb)[None, :] < q_blk[:, None]
        gate = jnp.where(past, gate, NEG_INF)
        _, idx = lax.top_k(gate, n_sel)
        sel_ok = idx < q_blk[:, None]
        nc = s // MOBA_Q_CHUNK

        def to_chunks(t):
            return jnp.moveaxis(t.reshape(bsz, h, nc, MOBA_Q_CHUNK, *t.shape[3:]), 2, 0)

        def from_chunks(t):
            t = jnp.moveaxis(t, 0, 2)
            return t.reshape(bsz, h, s, *t.shape[4:])

        bi = jnp.arange(bsz)[:, None, None, None]
        hi = jnp.arange(h)[None, :, None, None]

        def attend(args):
            qc, ic, okc = args
            kg = kb[bi, hi, ic]
            sc = jnp.einsum('bhqd,bhqnkd->bhqnk', qc, kg) * scale
            sc = jnp.where(okc[..., None], sc, NEG_INF)
            m = jnp.max(sc, axis=(-2, -1))
            p = jnp.where(okc[..., None], jnp.exp(sc - m[..., None, None]), 0.0)
            l = jnp.sum(p, axis=(-2, -1))
            vg = vb[bi, hi, ic]
            o = jnp.einsum('bhqnk,bhqnkd->bhqd', p, vg)
            return m, l, o

        m_s, l_s, o_s = lax.map(attend, (to_chunks(q), to_chunks(idx), to_chunks(sel_ok)))
        m_s, l_s, o_s = from_chunks(m_s), from_chunks(l_s), from_chunks(o_s)
        m = jnp.maximum(m_own, m_s)
        w1 = jnp.exp(m_own - m)
        w2 = jnp.exp(m_s - m)
        out = (o_own * w1[..., None] + o_s * w2[..., None]) / (l_own * w1 + l_s * w2)[..., None]
    out = out.transpose(0, 2, 1, 3).reshape(bsz, s, MB_WIDTH).astype(x.dtype)
    return out @ w_o


def swiglu(x, w1, w3, w2):
    return (jax.nn.silu(x @ w1) * (x @ w3)) @ w2


def moe_ffn(x, w_router, w1, w3, w2):
    xt = x.reshape(-1, x.shape[-1])
    probs = jax.nn.softmax((xt @ w_router).astype(jnp.float32), axis=-1)
    top_p, top_i = lax.top_k(probs, TOP_K)
    top_p = top_p / jnp.sum(top_p, axis=-1, keepdims=True)
    gates = jnp.sum(jax.nn.one_hot(top_i, N_EXPERTS, dtype=jnp.float32) * top_p[..., None], axis=-2)
    y = jnp.zeros(xt.shape, jnp.float32)
    for e in range(N_EXPERTS):
        y = y + gates[:, e:e + 1] * swiglu(xt, w1[e], w3[e], w2[e])
    return y.reshape(x.shape).astype(x.dtype)


def setup_inputs(seed: int = 0) -> dict:
    key = jax.random.key(seed)
    ks = jax.random.split(key, 24)
    f32 = jnp.float32

    def nrm(i, shape, scale):
        return jax.random.normal(ks[i], shape, f32) * scale

    x = nrm(0, (BATCH, SEQ, D_MODEL), 1.0)
    a_w_in = nrm(1, (N_A_LAYERS, D_MODEL, DN_PROJ), D_MODEL ** -0.5)
    a_w_in = a_w_in.at[:, :, 2 * DN_WIDTH:3 * DN_WIDTH].multiply(DEEPNORM_BETA)
    a_conv_w = nrm(2, (N_A_LAYERS, DN_CONV, 3 * DN_WIDTH), DN_CONV ** -0.5)
    a_log_decay = jnp.log(jax.random.uniform(ks[3], (N_A_LAYERS, DN_HEADS), f32, 1.0, 16.0))
    dt = jnp.exp(jax.random.uniform(ks[4], (N_A_LAYERS, DN_HEADS), f32, math.log(1e-3), math.log(1e-1)))
    a_dt_bias = dt + jnp.log(-jnp.expm1(-dt))
    a_norm_w = 1.0 + nrm(5, (N_A_LAYERS, DN_HEAD_DIM), 0.02)
    a_w_out = nrm(6, (N_A_LAYERS, DN_WIDTH, D_MODEL), DN_WIDTH ** -0.5 * DEEPNORM_BETA)
    b_w_kv = nrm(7, (D_MODEL, 2 * MB_WIDTH), D_MODEL ** -0.5)
    b_w_kv = b_w_kv.at[:, MB_WIDTH:].multiply(DEEPNORM_BETA)
    b_w_q = nrm(8, (N_B_LAYERS, D_MODEL, MB_WIDTH), D_MODEL ** -0.5)
    b_w_o = nrm(9, (N_B_LAYERS, MB_WIDTH, D_MODEL), MB_WIDTH ** -0.5 * DEEPNORM_BETA)
    ffn_w1 = nrm(10, (N_EVEN, D_MODEL, FFN_HIDDEN), D_MODEL ** -0.5)
    ffn_w3 = nrm(11, (N_EVEN, D_MODEL, FFN_HIDDEN), D_MODEL ** -0.5)
    ffn_w2 = nrm(12, (N_EVEN, FFN_HIDDEN, D_MODEL), FFN_HIDDEN ** -0.5 * DEEPNORM_BETA)
    moe_router = nrm(13, (N_ODD, D_MODEL, N_EXPERTS), D_MODEL ** -0.5)
    moe_w1 = nrm(14, (N_ODD, N_EXPERTS, D_MODEL, FFN_HIDDEN), D_MODEL ** -0.5)
    moe_w3 = nrm(15, (N_ODD, N_EXPERTS, D_MODEL, FFN_HIDDEN), D_MODEL ** -0.5)
    moe_w2 = nrm(16, (N_ODD, N_EXPERTS, FFN_HIDDEN, D_MODEL), FFN_HIDDEN ** -0.5 * DEEPNORM_BETA)
    ln_g = 1.0 + nrm(17, (DEPTH, 2, D_MODEL), 0.02)
    ln_b = nrm(18, (DEPTH, 2, D_MODEL), 0.02)
    return {"x": x, "a_w_in": a_w_in, "a_conv_w": a_conv_w, "a_log_decay": a_log_decay,
            "a_dt_bias": a_dt_bias, "a_norm_w": a_norm_w, "a_w_out": a_w_out,
            "b_w_kv": b_w_kv, "b_w_q": b_w_q, "b_w_o": b_w_o,
            "ffn_w1": ffn_w1, "ffn_w3": ffn_w3, "ffn_w2": ffn_w2,
            "moe_router": moe_router, "moe_w1": moe_w1, "moe_w3": moe_w3, "moe_w2": moe_w2,
            "ln_g": ln_g, "ln_b": ln_b}


def reference(x, a_w_in, a_conv_w, a_log_decay, a_dt_bias, a_norm_w, a_w_out, b_w_kv, b_w_q, b_w_o,
              ffn_w1, ffn_w3, ffn_w2, moe_router, moe_w1, moe_w3, moe_w2, ln_g, ln_b):
    shared = None
    for layer in range(DEPTH):
        if layer < N_A_LAYERS:
            i = layer
            mix = gated_deltanet_mixer(x, a_w_in[i], a_conv_w[i], a_log_decay[i], a_dt_bias[i],
                                       a_norm_w[i], a_w_out[i])
        else:
            if shared is None:
                shared = moba_shared_kv(x, b_w_kv)
            j = layer - N_A_LAYERS
            mix = moba_mixer(x, b_w_q[j], b_w_o[j], *shared)
        x = layer_norm(DEEPNORM_ALPHA * x + mix, ln_g[layer, 0], ln_b[layer, 0])
        f = layer // 2
        if layer % 2 == 0:
            ffn = swiglu(x, ffn_w1[f], ffn_w3[f], ffn_w2[f])
        else:
            ffn = moe_ffn(x, moe_router[f], moe_w1[f], moe_w3[f], moe_w2[f])
        x = layer_norm(DEEPNORM_ALPHA * x + ffn, ln_g[layer, 1], ln_b[layer, 1])
    return x
```

```python
from contextlib import ExitStack
import numpy as np
import concourse.bass as bass
import concourse.mybir as mybir
from concourse.bass_utils import run_bass_kernel_spmd

F32 = mybir.dt.float32
BF16 = mybir.dt.bfloat16
ALU = mybir.AluOpType
AF = mybir.ActivationFunctionType
AX = mybir.AxisListType

S = 2048
D = 1024
NT = 16
NCH = 4
KC = 8
FH = 3584
NF = 28
NE = 8
ALPHA = 4.0 ** 0.25
LN_EPS = 1e-5
DN_EPS = 1e-6
SEM_ROLL = 20000


class Prog:
    ENG = ("pe", "act", "dve", "pool", "sp")

    def __init__(self, nc):
        self.nc = nc
        self.es = ExitStack()
        self.scopes = [self.es]
        self.e = {"pe": nc.tensor, "act": nc.scalar, "dve": nc.vector, "pool": nc.gpsimd, "sp": nc.sync}
        self.sem = {}
        self.cnt = {}
        self.nsem = 0
        for k in self.ENG:
            self._newsem(k)
        self.waited = {k: {} for k in self.ENG}
        self.last_w = {}
        self.readers = {}
        self.dsem = {}
        self.group_reads = []
        self.n_inst = 0

    def _newsem(self, k):
        name = f"s_{k}_{self.nsem}"
        s = self.es.enter_context(self.nc.semaphore(name))
        self.nsem += 1
        self.sem[k] = (name, s)
        self.cnt[k] = 0

    def sb(self, name, shape, dt=F32):
        return self.scopes[-1].enter_context(self.nc.sbuf_tensor(name, list(shape), dt))

    def push(self):
        self.scopes.append(ExitStack())

    def pop(self):
        self.barrier()
        self.scopes.pop().close()

    def ps(self, name, shape, dt=F32):
        return self.es.enter_context(self.nc.psum_tensor(name, list(shape), dt))

    def _deps(self, engine, reads, writes):
        need = {}

        def add(rec):
            name, s, val, e = rec
            if e == engine and engine == "pe":
                return
            if name not in need or need[name][1] < val:
                need[name] = (s, val)
        for r in list(reads) + self.group_reads:
            if r in self.last_w:
                add(self.last_w[r])
        for w in writes:
            if w in self.last_w:
                add(self.last_w[w])
            for rec in self.readers.get(w, ()):
                add(rec)
        for name, (s, val) in need.items():
            if self.waited[engine].get(name, 0) >= val:
                continue
            self.e[engine].wait_ge(s, val)
            self.waited[engine][name] = val

    def _record(self, rec, reads, writes):
        for r in reads:
            self.readers.setdefault(r, []).append(rec)
        for w in writes:
            self.last_w[w] = rec
            self.readers[w] = []

    def op(self, engine, fn, reads=(), writes=()):
        self._deps(engine, reads, writes)
        inst = fn(self.e[engine])
        name, s = self.sem[engine]
        self.cnt[engine] += 1
        val = self.cnt[engine]
        inst.then_inc(s, 1)
        self.n_inst += 1
        self._record((name, s, val, engine), reads, writes)
        if val >= SEM_ROLL:
            self._newsem(engine)
        return inst

    GROUPS = ("c0", "dnc", "mbc", "wr")

    def dma(self, queue, out, in_, reads=(), writes=(), key=None, **kw):
        if key in self.GROUPS:
            gk = ("grp", key)
            writes = list(writes) + [gk]
            if gk not in self.group_reads:
                self.group_reads.append(gk)
        self._deps(queue, reads, writes)
        if key not in self.dsem:
            name = f"d_{self.nsem}"
            s = self.es.enter_context(self.nc.semaphore(name))
            self.nsem += 1
            self.dsem[key] = [name, s, 0]
        ent = self.dsem[key]
        inst = self.e[queue].dma_start(out=out, in_=in_, **kw)
        ent[2] += 16
        inst.then_inc(ent[1], 16)
        self.n_inst += 1
        self._record((ent[0], ent[1], ent[2], "dma"), reads, writes)
        return inst

    def barrier(self):
        recs = []
        for k in self.ENG:
            name, s = self.sem[k]
            if self.cnt[k] > 0:
                recs.append((name, s, self.cnt[k], k))
        for key, ent in self.dsem.items():
            if ent[2] > 0:
                recs.append((ent[0], ent[1], ent[2], "dma"))
        for k in self.ENG:
            for name, s, val, src in recs:
                if self.waited[k].get(name, 0) >= val:
                    continue
                if src == k and k == "pe":
                    continue
                self.e[k].wait_ge(s, val)
                self.waited[k][name] = val

    def close(self):
        self.barrier()
        self.es.close()


def _consts():
    c = {}
    c["ident"] = np.eye(128, dtype=np.float32)
    c["ones"] = np.ones((128, 128), dtype=np.float32)
    half = 64
    inv_freq = (10000.0 ** (-(np.arange(half, dtype=np.float32) / np.float32(half)))).astype(np.float32)
    ang = (np.arange(S, dtype=np.float32)[:, None] * inv_freq[None, :]).astype(np.float32)
    cos = np.cos(ang.astype(np.float64)).astype(np.float32).T
    sin = np.sin(ang.astype(np.float64)).astype(np.float32).T
    c["ropec"] = np.concatenate([cos, cos], axis=0)
    c["ropes"] = np.concatenate([-sin, sin], axis=0)
    sel = np.zeros((8, 8, 128), dtype=np.float32)
    for e in range(8):
        sel[e, e, :] = 1.0
    c["sel8"] = sel.transpose(1, 0, 2).copy()
    r = np.arange(128)[:, None]
    q = np.arange(128)[None, :]
    c["tri_le"] = (r <= q).astype(np.float32)
    c["tri_gt"] = (r > q).astype(np.float32)
    c["nmask_bd"] = -((r > q) & ((r // 64) == (q // 64))).astype(np.float32)
    c["mask_off"] = ((r >= 64) & (q < 64)).astype(np.float32)
    c["perm"] = (r == ((q + 64) % 128)).astype(np.float32)
    pb = np.zeros((128, NT, 8), dtype=np.float32)
    p01 = np.zeros((128, NT, 8), dtype=np.float32)
    own = np.zeros((128, NT, 8), dtype=np.float32)
    for t in range(NT):
        qb = t // 2
        own[:, t, qb] = 1.0
        for n in range(8):
            if n < qb:
                p01[:, t, n] = 1.0
            else:
                pb[:, t, n] = -1.0e30
    c["pastb"] = pb
    c["past01"] = p01
    c["own01"] = own
    caus = np.ones((128, 4, 512), dtype=np.float32)
    j = np.arange(128)[:, None]
    ii = np.arange(512)[None, :]
    for pos in range(4):
        same = (pos // 2) == (ii // 256)
        caus[:, pos, :] = np.where(same & ((128 * pos + j) > ii), 0.0, 1.0)
    c["caus4"] = caus
    return c


CONST_SHAPES = {"ident": [128, 128], "ones": [128, 128], "ropec": [128, S], "ropes": [128, S],
                "sel8": [8, 8, 128], "tri_le": [128, 128], "tri_gt": [128, 128], "nmask_bd": [128, 128],
                "mask_off": [128, 128], "perm": [128, 128], "pastb": [128, NT, 8], "past01": [128, NT, 8],
                "own01": [128, NT, 8], "caus4": [128, 4, 512]}

IN_SHAPES = {
    "x": [S, D], "a_w_in": [D, 4112], "a_conv_w": [4, 3072], "a_log_decay": [1, 8], "a_dt_bias": [1, 8],
    "a_norm_w": [1, 128], "a_w_out": [D, D], "b_w_kv": [D, 2 * D], "b_w_q": [D, D], "b_w_o": [D, D],
    "ffn_w1": [D, FH], "ffn_w3": [D, FH], "ffn_w2": [FH, D], "moe_router": [D, 8],
    "moe_w1": [8, D, FH], "moe_w3": [8, D, FH], "moe_w2": [8, FH, D], "ln_g": [4, D], "ln_b": [4, D],
}


class K:
    def __init__(self, stages, dbg=()):
        self.stages = stages
        nc = bass.Bass("TRN2", target_bir_lowering=False)
        self.nc = nc
        self.P = Prog(nc)
        P = self.P
        self.din = {k: nc.dram_tensor(k, v, F32, kind="ExternalInput").ap() for k, v in IN_SHAPES.items()}
        self.dc = {k: nc.dram_tensor("c_" + k, v, F32, kind="ExternalInput").ap() for k, v in CONST_SHAPES.items()}
        self.out = nc.dram_tensor("out", [S, D], F32, kind="ExternalOutput").ap()
        self.dbg = {}
        for name, shape in dbg:
            self.dbg[name] = nc.dram_tensor("dbg_" + name, shape, F32, kind="ExternalOutput").ap()
        self.uid = 0
        self.XT = P.sb("XT", [128, KC, S])
        self.XB = P.sb("XB", [128, KC, S], BF16)
        self.ident = P.sb("ident", [128, 128])
        self.identb = P.sb("identb", [128, 128], BF16)
        self.ones = P.sb("ones", [128, 128])
        self.onesb = P.sb("onesb", [128, 128], BF16)
        self.lng = P.sb("lng", [128, 4, KC])
        self.lnb = P.sb("lnb", [128, 4, KC])
        self.PS = [P.ps(f"PS{i}", [128, 512]) for i in range(8)]
        self.psk = [f"PS{i}" for i in range(8)]

        P.dma("sp", self.ident[:], self.dc["ident"], writes=["ident"], key="c0")
        P.dma("sp", self.ones[:], self.dc["ones"], writes=["ones"], key="c0")
        lnraw = P.sb("lnraw", [64, 128])
        P.dma("sp", lnraw[0:32, :], self.din["ln_g"].rearrange("l (c p) -> (l c) p", p=128), writes=["lnraw"], key="c0")
        P.dma("sp", lnraw[32:64, :], self.din["ln_b"].rearrange("l (c p) -> (l c) p", p=128), writes=["lnraw"], key="c0")
        P.op("pe", lambda e: e.transpose(self.PS[0][:, 0:64], lnraw[:], self.ident[0:64, 0:64]), reads=["lnraw", "ident"], writes=["PS0"])
        P.op("dve", lambda e: e.tensor_copy(out=self.lng[:], in_=self.PS[0][:, 0:32].rearrange("p (l c) -> p l c", l=4)), reads=["PS0"], writes=["lng"])
        P.op("dve", lambda e: e.tensor_copy(out=self.lnb[:], in_=self.PS[0][:, 32:64].rearrange("p (l c) -> p l c", l=4)), reads=["PS0"], writes=["lnb"])
        P.op("dve", lambda e: e.tensor_copy(out=self.identb[:], in_=self.ident[:]), reads=["ident"], writes=["identb"])
        P.op("dve", lambda e: e.tensor_copy(out=self.onesb[:], in_=self.ones[:]), reads=["ones"], writes=["onesb"])

    def u(self, s):
        self.uid += 1
        return f"{s}{self.uid}"

    def load_x(self):
        P = self.P
        xin = [P.sb(f"xin{i}", [128, D]) for i in range(2)]
        for t in range(NT):
            b = xin[t % 2]
            bk = f"xin{t % 2}"
            P.dma("sp", b[:], self.din["x"][t * 128:(t + 1) * 128, :], writes=[bk], key=bk)
            for hlf in range(2):
                ps = self.PS[2 * (t % 2) + hlf]
                pk = self.psk[2 * (t % 2) + hlf]
                for c4 in range(4):
                    c = hlf * 4 + c4
                    P.op("pe", lambda e: e.transpose(ps[:, c4 * 128:(c4 + 1) * 128], b[:, c * 128:(c + 1) * 128], self.ident[:]),
                         reads=[bk, "ident"], writes=[pk])
                src = ps[:].rearrange("p (c n) -> p c n", c=4)
                P.op("act", lambda e: e.activation(out=self.XT[:, hlf * 4:(hlf + 1) * 4, t * 128:(t + 1) * 128], in_=src, func=AF.Copy),
                     reads=[pk], writes=[("XT", t // 4)])
                P.op("dve", lambda e: e.tensor_copy(out=self.XB[:, hlf * 4:(hlf + 1) * 4, t * 128:(t + 1) * 128],
                                                    in_=self.XT[:, hlf * 4:(hlf + 1) * 4, t * 128:(t + 1) * 128]),
                     reads=[("XT", t // 4)], writes=[("XB", t // 4)])

    def store_x(self):
        P = self.P
        xo = [P.sb(f"xo{i}", [128, D]) for i in range(2)]
        for t in range(NT):
            b = xo[t % 2]
            bk = f"xo{t % 2}"
            for hlf in range(2):
                ps = self.PS[2 * (t % 2) + hlf]
                pk = self.psk[2 * (t % 2) + hlf]
                for c4 in range(4):
                    c = hlf * 4 + c4
                    P.op("pe", lambda e: e.transpose(ps[:, c4 * 128:(c4 + 1) * 128], self.XT[:, c, t * 128:(t + 1) * 128], self.ident[:]),
                         reads=[("XT", t // 4), "ident"], writes=[pk])
                if hlf == 0:
                    P.op("act", lambda e: e.activation(out=b[:, 0:512], in_=ps[:], func=AF.Copy), reads=[pk], writes=[bk])
                else:
                    P.op("dve", lambda e: e.tensor_copy(out=b[:, 512:1024], in_=ps[:]), reads=[pk], writes=[bk])
            P.dma("sp", self.out[t * 128:(t + 1) * 128, :], b[:], reads=[bk], writes=["out"], key=bk)

    def dbg_store_xt(self, name):
        P = self.P
        for c in range(KC):
            P.dma("sp", self.dbg[name][c], self.XT[:, c, :], reads=[("XT", n) for n in range(NCH)], writes=["dbg"], key="dbg")

    def layer_norm(self, idx, tmp):
        P = self.P
        SQ, R1, R2, MEAN, RSTD = tmp["sq"], tmp["r1"], tmp["r2"], tmp["mean"], tmp["rstd"]
        for n in range(NCH):
            sl = slice(n * 512, (n + 1) * 512)
            xk = ("XT", n)
            Yv = self.XT[:, :, sl]
            P.op("act", lambda e: e.activation(out=SQ[:], in_=Yv, func=AF.Square), reads=[xk], writes=["ln_sq"])
            P.op("dve", lambda e: e.tensor_reduce(out=R1[:], in_=Yv.rearrange("p c n -> p n c"), axis=AX.X, op=ALU.add),
                 reads=[xk], writes=["ln_r1"])
            P.op("dve", lambda e: e.tensor_reduce(out=R2[:], in_=SQ[:].rearrange("p c n -> p n c"), axis=AX.X, op=ALU.add),
                 reads=["ln_sq"], writes=["ln_r2"])
            ps1, pk1 = self.PS[6], self.psk[6]
            ps2, pk2 = self.PS[7], self.psk[7]
            P.op("pe", lambda e: e.matmul(ps1[:], self.ones[:], R1[:], start=True, stop=True), reads=["ones", "ln_r1"], writes=[pk1])
            P.op("pe", lambda e: e.matmul(ps2[:], self.ones[:], R2[:], start=True, stop=True), reads=["ones", "ln_r2"], writes=[pk2])
            P.op("act", lambda e: e.activation(out=MEAN[:], in_=ps1[:], func=AF.Copy, scale=1.0 / D), reads=[pk1], writes=["ln_mean"])
            P.op("dve", lambda e: e.tensor_tensor(out=R1[:], in0=MEAN[:], in1=MEAN[:], op=ALU.mult), reads=["ln_mean"], writes=["ln_r1"])
            P.op("dve", lambda e: e.scalar_tensor_tensor(out=R2[:], in0=ps2[:], scalar=1.0 / D, in1=R1[:], op0=ALU.mult, op1=ALU.subtract),
                 reads=[pk2, "ln_r1"], writes=["ln_r2"])
            P.op("dve", lambda e: e.tensor_scalar(out=R2[:], in0=R2[:], scalar1=LN_EPS, scalar2=None, op0=ALU.add), reads=["ln_r2"], writes=["ln_r2"])
            P.op("act", lambda e: e.activation(out=R1[:], in_=R2[:], func=AF.Sqrt), reads=["ln_r2"], writes=["ln_r1"])
            P.op("dve", lambda e: e.reciprocal(out=RSTD[:], in_=R1[:]), reads=["ln_r1"], writes=["ln_rstd"])
            mb = MEAN[:].unsqueeze(1).broadcast_to([128, KC, 512])
            rb = RSTD[:].unsqueeze(1).broadcast_to([128, KC, 512])
            P.op("dve", lambda e: e.tensor_tensor(out=SQ[:], in0=Yv, in1=mb, op=ALU.subtract), reads=[xk, "ln_mean"], writes=["ln_sq"])
            P.op("pool", lambda e: e.tensor_tensor(out=SQ[:], in0=SQ[:], in1=rb, op=ALU.mult), reads=["ln_sq", "ln_rstd"], writes=["ln_sq"])
            for c in range(KC):
                P.op("act", lambda e: e.activation(out=self.XT[:, c, sl], in_=SQ[:, c, :], func=AF.Identity,
                                                   scale=self.lng[:, idx, c:c + 1], bias=self.lnb[:, idx, c:c + 1]),
                     reads=["ln_sq", "lng", "lnb"], writes=[xk])
                P.op("dve", lambda e: e.tensor_scalar(out=self.XB[:, c, sl], in0=SQ[:, c, :], scalar1=self.lng[:, idx, c:c + 1],
                                                      scalar2=self.lnb[:, idx, c:c + 1], op0=ALU.mult, op1=ALU.add),
                     reads=["ln_sq", "lng", "lnb"], writes=[("XB", n)])

    def ln_tmp(self):
        P = self.P
        return {"sq": P.sb(self.u("ln_sq"), [128, KC, 512]), "r1": P.sb(self.u("ln_r1"), [128, 512]),
                "r2": P.sb(self.u("ln_r2"), [128, 512]), "mean": P.sb(self.u("ln_mean"), [128, 512]),
                "rstd": P.sb(self.u("ln_rstd"), [128, 512])}

    def scale_xt(self):
        P = self.P
        for n in range(NCH):
            sl = slice(n * 512, (n + 1) * 512)
            eng = "act" if n % 2 == 0 else "dve"
            if eng == "act":
                P.op("act", lambda e: e.activation(out=self.XT[:, :, sl], in_=self.XT[:, :, sl], func=AF.Copy, scale=ALPHA),
                     reads=[("XT", n)], writes=[("XT", n)])
            else:
                P.op("dve", lambda e: e.tensor_scalar(out=self.XT[:, :, sl], in0=self.XT[:, :, sl], scalar1=ALPHA, scalar2=None, op0=ALU.mult),
                     reads=[("XT", n)], writes=[("XT", n)])

    def ffn(self, experts, gb=None):
        P = self.P
        G = 2
        NB = 3
        W1 = [P.sb(self.u("w1g"), [128, KC, G * 128], BF16) for _ in range(NB)]
        W3 = [P.sb(self.u("w3g"), [128, KC, G * 128], BF16) for _ in range(NB)]
        W2 = [P.sb(self.u("w2g"), [128, G, D], BF16) for _ in range(NB)]
        H = [P.sb(self.u("hg"), [128, G, S], BF16) for _ in range(2)]
        SA = [P.sb(self.u("sa"), [128, 512], BF16) for _ in range(2)]
        HT = [P.sb(self.u("ht"), [128, 512], BF16) for _ in range(2)]
        base = self.u("ffn")
        gi = 0
        si = 0
        groups = [(ei, ex, fg) for ei, ex in enumerate(experts) for fg in range(NF // G)]

        def issue_load(k):
            ei, ex, fg = groups[k]
            w1, w3, w2, _ = ex
            slot = k % NB
            f0 = fg * G * 128
            P.dma("pool", W1[slot][:], w1[:, f0:f0 + G * 128].rearrange("(c p) f -> p c f", p=128),
                  writes=[(base, "w1", slot)], key=(base, "w1", slot))
            P.dma("pool", W3[slot][:], w3[:, f0:f0 + G * 128].rearrange("(c p) f -> p c f", p=128),
                  writes=[(base, "w3", slot)], key=(base, "w3", slot))
            P.dma("pool", W2[slot][:], w2[f0:f0 + G * 128, :].rearrange("(j p) m -> p j m", p=128),
                  writes=[(base, "w2", slot)], key=(base, "w2", slot))

        for k in range(min(NB - 1, len(groups))):
            issue_load(k)
        for k, (ei, ex, fg) in enumerate(groups):
            if k + NB - 1 < len(groups):
                issue_load(k + NB - 1)
            slot = k % NB
            hb = H[k % 2]
            hk = (base, "h", k % 2)
            if ex[3] is None:
                gt = None
            elif fg == 0:
                gt, gk = gb(ex[3])
            for j in range(G):
                for n in range(NCH):
                    sl = slice(n * 512, (n + 1) * 512)
                    pa, pak = self.PS[2 * (si % 2)], self.psk[2 * (si % 2)]
                    pb, pbk = self.PS[2 * (si % 2) + 1], self.psk[2 * (si % 2) + 1]
                    sa, sak = SA[si % 2], (base, "sa", si % 2)
                    for c in range(KC):
                        P.op("pe", lambda e: e.matmul(pa[:], W1[slot][:, c, j * 128:(j + 1) * 128], self.XB[:, c, sl], start=(c == 0), stop=(c == KC - 1)),
                             reads=[(base, "w1", slot), ("XB", n)], writes=[pak])
                    for c in range(KC):
                        P.op("pe", lambda e: e.matmul(pb[:], W3[slot][:, c, j * 128:(j + 1) * 128], self.XB[:, c, sl], start=(c == 0), stop=(c == KC - 1)),
                             reads=[(base, "w3", slot), ("XB", n)], writes=[pbk])
                    P.op("act", lambda e: e.activation(out=sa[:], in_=pa[:], func=AF.Silu), reads=[pak], writes=[sak])
                    if gt is None:
                        P.op("dve", lambda e: e.tensor_tensor(out=hb[:, j, sl], in0=pb[:], in1=sa[:], op=ALU.mult),
                             reads=[pbk, sak], writes=[hk])
                    else:
                        ht, htk = HT[si % 2], (base, "ht", si % 2)
                        P.op("dve", lambda e: e.tensor_tensor(out=ht[:], in0=pb[:], in1=sa[:], op=ALU.mult),
                             reads=[pbk, sak], writes=[htk])
                        P.op("pool", lambda e: e.tensor_tensor(out=hb[:, j, sl], in0=ht[:], in1=gt[:, sl], op=ALU.mult),
                             reads=[htk, gk], writes=[hk])
                    si += 1
            for m in range(KC):
                for n in range(NCH):
                    sl = slice(n * 512, (n + 1) * 512)
                    py, pyk = self.PS[4 + gi % 2], self.psk[4 + gi % 2]
                    gi += 1
                    for j in range(G):
                        P.op("pe", lambda e: e.matmul(py[:], W2[slot][:, j, m * 128:(m + 1) * 128], hb[:, j, sl], start=(j == 0), stop=(j == G - 1)),
                             reads=[(base, "w2", slot), hk], writes=[pyk])
                    P.op("dve", lambda e: e.tensor_tensor(out=self.XT[:, m, sl], in0=py[:], in1=self.XT[:, m, sl], op=ALU.add),
                         reads=[pyk, ("XT", n)], writes=[("XT", n)])

    def router(self):
        P = self.P
        WR = P.sb("wr", [128, KC, 8])
        LG = P.sb("r_lg", [128, NT, 8])
        M8 = P.sb("r_m8", [128, NT, 8])
        EX = P.sb("r_ex", [128, NT, 8])
        SELM = P.sb("r_sel", [128, NT, 8])
        DEN = P.sb("r_den", [128, NT])
        GT = P.sb("r_gt", [8, S])
        SEL8 = P.sb("r_sel8", [8, 8, 128])
        P.dma("sp", WR[:], self.din["moe_router"].rearrange("(c p) e -> p c e", p=128), writes=["wr"], key="wr")
        P.dma("sp", SEL8[:], self.dc["sel8"], writes=["sel8"], key="wr")
        ps, pk = self.PS[0], self.psk[0]
        for t in range(NT):
            for c in range(KC):
                P.op("pe", lambda e: e.matmul(ps[:, t * 8:(t + 1) * 8], self.XT[:, c, t * 128:(t + 1) * 128], WR[:, c, :], start=(c == 0), stop=(c == KC - 1)),
                     reads=[("XT", t // 4), "wr"], writes=[pk])
        P.op("dve", lambda e: e.tensor_copy(out=LG[:], in_=ps[:, 0:NT * 8].rearrange("p (t e) -> p t e", e=8)), reads=[pk], writes=["r_lg"])
        for t in range(NT):
            P.op("dve", lambda e: e.max(out=M8[:, t, :], in_=LG[:, t, :]), reads=["r_lg"], writes=["r_m8"])
        m1 = M8[:, :, 0:1].broadcast_to([128, NT, 8])
        m2 = M8[:, :, 1:2].broadcast_to([128, NT, 8])
        P.op("dve", lambda e: e.tensor_tensor(out=EX[:], in0=LG[:], in1=m1, op=ALU.subtract), reads=["r_lg", "r_m8"], writes=["r_ex"])
        P.op("act", lambda e: e.activation(out=EX[:], in_=EX[:], func=AF.Exp), reads=["r_ex"], writes=["r_ex"])
        P.op("dve", lambda e: e.tensor_tensor(out=SELM[:], in0=LG[:], in1=m2, op=ALU.is_ge), reads=["r_lg", "r_m8"], writes=["r_sel"])
        P.op("dve", lambda e: e.tensor_tensor(out=EX[:], in0=EX[:], in1=SELM[:], op=ALU.mult), reads=["r_ex", "r_sel"], writes=["r_ex"])
        P.op("dve", lambda e: e.tensor_reduce(out=DEN[:], in_=EX[:], axis=AX.X, op=ALU.add), reads=["r_ex"], writes=["r_den"])
        P.op("dve", lambda e: e.reciprocal(out=DEN[:], in_=DEN[:]), reads=["r_den"], writes=["r_den"])
        P.op("dve", lambda e: e.tensor_tensor(out=EX[:], in0=EX[:], in1=DEN[:].unsqueeze(2).broadcast_to([128, NT, 8]), op=ALU.mult),
             reads=["r_ex", "r_den"], writes=["r_ex"])
        for q in range(4):
            pt, ptk = self.PS[1 + q % 2], self.psk[1 + q % 2]
            for tt in range(4):
                t = q * 4 + tt
                P.op("pe", lambda e: e.transpose(pt[0:8, tt * 128:(tt + 1) * 128], EX[:, t, :], self.ident[:]), reads=["r_ex", "ident"], writes=[ptk])
            P.op("act", lambda e: e.activation(out=GT[:, q * 512:(q + 1) * 512], in_=pt[0:8, :], func=AF.Copy), reads=[ptk], writes=["r_gt"])
        self.GT = GT
        self.SEL8 = SEL8
        self.GB = [P.sb(f"gb{i}", [128, S], BF16) for i in range(2)]
        if "g" in self.dbg:
            P.dma("sp", self.dbg["g"], EX[:], reads=["r_ex"], writes=["dbg"], key="dbg")

    def gate_bcast(self, eidx):
        P = self.P
        gbt = self.GB[eidx % 2]
        gk = ("gb", eidx % 2)
        for n in range(NCH):
            ps, pk = self.PS[6 + n % 2], self.psk[6 + n % 2]
            P.op("pe", lambda e: e.matmul(ps[:], self.SEL8[:, eidx, :], self.GT[:, n * 512:(n + 1) * 512], start=True, stop=True),
                 reads=["sel8", "r_gt"], writes=[pk])
            P.op("act", lambda e: e.activation(out=gbt[:, n * 512:(n + 1) * 512], in_=ps[:], func=AF.Copy), reads=[pk], writes=[gk])
        return gbt, gk


def build(stages=("load", "dn", "ffn0", "moba", "moe", "store"), dbg=()):
    k = K(stages, dbg)
    P = k.P
    din = k.din
    if "load" in stages:
        P.push()
        k.load_x()
        P.pop()
    if "dn" in stages:
        from_dn(k)
    if "ffn0" in stages:
        P.push()
        k.scale_xt()
        k.ffn([(din["ffn_w1"], din["ffn_w3"], din["ffn_w2"], None)])
        k.layer_norm(1, k.ln_tmp())
        P.pop()
    if "dbg_x1" in k.dbg:
        k.dbg_store_xt("x1")
    if "moba" in stages:
        from_moba(k)
    if "moe" in stages:
        P.push()
        k.router()
        k.scale_xt()
        k.ffn([(din["moe_w1"][e], din["moe_w3"][e], din["moe_w2"][e], e) for e in range(NE)], gb=k.gate_bcast)
        k.layer_norm(3, k.ln_tmp())
        P.pop()
    if "store" in stages:
        P.push()
        k.store_x()
        P.pop()
    P.close()
    return k.nc


def _nps(k):
    i = getattr(k, "psi", 0)
    k.psi = (i + 1) % 8
    return k.PS[i], k.psk[i]


DN_STOP = None


def from_dn(k):
    P = k.P
    din = k.din
    dc = k.dc
    TB = 2
    W = TB * 128
    QSCALE = 128.0 ** -0.5
    P.push()

    def cload(name, src, shape, dt=F32, q="sp"):
        t = P.sb(name, shape, dt)
        P.dma(q, t[:], src, writes=[name], key="dnc")
        return t

    TRI_LE = cload("tri_le", dc["tri_le"], [128, 128])
    TRI_GT = cload("tri_gt", dc["tri_gt"], [128, 128])
    NMBD = cload("nmask_bd", dc["nmask_bd"], [128, 128])
    MOFF = cload("mask_off", dc["mask_off"], [128, 128])
    ident, ones = k.ident, k.ones

    def b3(T):
        return T[:].unsqueeze(1).broadcast_to([128, TB, 128])

    def v3(ap):
        return ap.rearrange("p (t n) -> p t n", t=TB)

    cwraw = P.sb("cwraw", [96, 128])
    P.dma("sp", cwraw[:], din["a_conv_w"].rearrange("j (c p) -> (j c) p", p=128), writes=["cwraw"], key="dnc")
    CW = P.sb("CW", [128, 96])
    ps, pk = _nps(k)
    P.op("pe", lambda e: e.transpose(ps[:, 0:96], cwraw[:], ident[0:96, 0:96]), reads=["cwraw", "ident"], writes=[pk])
    P.op("dve", lambda e: e.tensor_copy(out=CW[:], in_=ps[:, 0:96]), reads=[pk], writes=["CW"])
    NW = P.sb("NW", [128, 1])
    P.dma("sp", NW[:], din["a_norm_w"].rearrange("o p -> p o"), writes=["NW"], key="dnc")

    WAB = P.sb("WAB", [128, KC, 16])
    P.dma("sp", WAB[:], din["a_w_in"][:, 4096:4112].rearrange("(c p) f -> p c f", p=128), writes=["WAB"], key="dnc")
    RW = P.sb("RW", [1, 16])
    P.dma("sp", RW[0:1, 0:8], din["a_log_decay"], writes=["RW"], key="dnc")
    P.dma("sp", RW[0:1, 8:16], din["a_dt_bias"], writes=["RW"], key="dnc")
    AB = P.sb("AB", [128, NT, 16])
    BC = P.sb("BC", [128, 16])
    ps, pk = _nps(k)
    for t in range(NT):
        for c in range(KC):
            P.op("pe", lambda e: e.matmul(ps[:, t * 16:(t + 1) * 16], k.XT[:, c, t * 128:(t + 1) * 128], WAB[:, c, :], start=(c == 0), stop=(c == KC - 1)),
                 reads=[("XT", t // 4), "WAB"], writes=[pk])
    P.op("dve", lambda e: e.tensor_copy(out=AB[:], in_=ps[:, 0:NT * 16].rearrange("p (t f) -> p t f", f=16)), reads=[pk], writes=["AB"])
    ps, pk = _nps(k)
    P.op("pe", lambda e: e.matmul(ps[:, 0:16], ones[0:1, :], RW[0:1, :], start=True, stop=True), reads=["ones", "RW"], writes=[pk])
    P.op("dve", lambda e: e.tensor_copy(out=BC[:], in_=ps[:, 0:16]), reads=[pk], writes=["BC"])

    def tm(name):
        return P.sb(name, [128, NT, 8])
    G, BETA, EKD, BEG, EGL, TM1, TM2, GCS = (tm(n) for n in ("G", "BETA", "EKD", "BEG", "EGL", "TM1", "TM2", "GCS"))
    EA = P.sb("EA", [128, 8])
    dtb = BC[:, 8:16].unsqueeze(1).broadcast_to([128, NT, 8])
    P.op("dve", lambda e: e.tensor_tensor(out=TM1[:], in0=AB[:, :, 0:8], in1=dtb, op=ALU.add), reads=["AB", "BC"], writes=["TM1"])
    P.op("act", lambda e: e.activation(out=TM1[:], in_=TM1[:], func=AF.Exp), reads=["TM1"], writes=["TM1"])
    P.op("dve", lambda e: e.tensor_scalar(out=TM1[:], in0=TM1[:], scalar1=1.0, scalar2=None, op0=ALU.add), reads=["TM1"], writes=["TM1"])
    P.op("act", lambda e: e.activation(out=TM1[:], in_=TM1[:], func=AF.Ln), reads=["TM1"], writes=["TM1"])
    P.op("act", lambda e: e.activation(out=EA[:], in_=BC[:, 0:8], func=AF.Exp), reads=["BC"], writes=["EA"])
    P.op("dve", lambda e: e.scalar_tensor_tensor(out=G[:], in0=TM1[:], scalar=-1.0, in1=EA[:].unsqueeze(1).broadcast_to([128, NT, 8]),
                                                 op0=ALU.mult, op1=ALU.mult), reads=["TM1", "EA"], writes=["G"])
    P.op("act", lambda e: e.activation(out=TM2[:], in_=AB[:, :, 8:16], func=AF.Exp, scale=-1.0), reads=["AB"], writes=["TM2"])
    P.op("dve", lambda e: e.tensor_scalar(out=TM2[:], in0=TM2[:], scalar1=1.0, scalar2=None, op0=ALU.add), reads=["TM2"], writes=["TM2"])
    P.op("dve", lambda e: e.reciprocal(out=BETA[:], in_=TM2[:]), reads=["TM2"], writes=["BETA"])
    Gf = G[:].rearrange("p t h -> p (t h)")
    psc, pkc = _nps(k)
    psl, pkl = _nps(k)
    P.op("pe", lambda e: e.matmul(psc[:, 0:128], TRI_LE[:], Gf, start=True, stop=True), reads=["tri_le", "G"], writes=[pkc])
    P.op("pe", lambda e: e.matmul(psl[:, 0:128], ones[:], Gf, start=True, stop=True), reads=["ones", "G"], writes=[pkl])
    f3 = lambda ap: ap.rearrange("p (t h) -> p t h", h=8)
    P.op("act", lambda e: e.activation(out=GCS[:], in_=f3(psc[:, 0:128]), func=AF.Copy), reads=[pkc], writes=["GCS"])
    P.op("act", lambda e: e.activation(out=EGL[:], in_=f3(psl[:, 0:128]), func=AF.Exp), reads=[pkl], writes=["EGL"])
    P.op("dve", lambda e: e.tensor_tensor(out=TM2[:], in0=f3(psl[:, 0:128]), in1=GCS[:], op=ALU.subtract), reads=[pkl, "GCS", "BETA", "EGL"], writes=["TM2"])
    P.op("act", lambda e: e.activation(out=EKD[:], in_=TM2[:], func=AF.Exp), reads=["TM2"], writes=["EKD"])
    P.op("act", lambda e: e.activation(out=TM1[:], in_=GCS[:], func=AF.Exp), reads=["GCS", "G"], writes=["TM1"])
    P.op("dve", lambda e: e.tensor_tensor(out=BEG[:], in0=TM1[:], in1=BETA[:], op=ALU.mult), reads=["TM1", "BETA"], writes=["BEG"])

    k.scale_xt()
    if DN_STOP == "tm":
        P.pop()
        return

    W4 = P.sb("W4", [128, 4, KC, 128], BF16)
    WO = P.sb("dnWO", [128, D], BF16)
    PRE = P.sb("PRE", [128, S + 3])
    QF = P.sb("QF", [128, S])
    KF = P.sb("KF", [128, S])
    VF = P.sb("VF", [128, S])
    SG = P.sb("SG", [128, S], BF16)
    OB = P.sb("dnOB", [128, S], BF16)
    RS1 = P.sb("RS1", [128, 512])
    RS2 = P.sb("RS2", [128, 512])
    Sst = P.sb("Sst", [128, 128])
    Sb = P.sb("Sb", [128, 128], BF16)
    TRIG = P.sb("TRIG", [128, TB, 128])
    ED = P.sb("ED", [128, TB, 128])
    EDT = P.sb("EDT", [128, TB, 128])
    EGB = P.sb("EGB", [128, TB, 128])
    LF = P.sb("LF", [128, TB, 128])
    Nn = [P.sb(f"Nn{i}", [128, TB, 128]) for i in range(2)]
    Mm = [P.sb(f"Mm{i}", [128, TB, 128]) for i in range(2)]
    LOFF = P.sb("LOFF", [128, TB, 128])
    LOFFT = P.sb("LOFFT", [128, TB, 128])
    Tt = P.sb("Tt", [128, TB, 128])
    R = P.sb("Rr", [128, TB, 256])
    SOL = P.sb("SOL", [128, TB, 256])
    KDEC = P.sb("KDEC", [128, TB, 128], BF16)
    WT = P.sb("WT", [128, TB, 128], BF16)
    INTRAT = P.sb("INTRAT", [128, TB, 128], BF16)
    QDT = P.sb("QDT", [128, TB, 128], BF16)
    VNEW = [P.sb(f"VNEW{i}", [128, 128], BF16) for i in range(2)]
    P.op("dve", lambda e: e.memset(PRE[:, 0:3], 0.0), writes=["PRE"])
    OT = PRE

    for h in range(8):
        for f in range(4):
            c0 = f * D + h * 128
            P.dma("pool", W4[:, f, :, :], din["a_w_in"][:, c0:c0 + 128].rearrange("(c p) f -> p c f", p=128), writes=["W4"], key="W4")
        P.dma("pool", WO[:], din["a_w_out"][h * 128:(h + 1) * 128, :], writes=["dnWO"], key="dnWO")
        for f in range(3):
            dst, dk = ((QF, "QF"), (KF, "KF"), (VF, "VF"))[f]
            for n in range(NCH):
                sl = slice(n * 512, (n + 1) * 512)
                ps, pk = _nps(k)
                for c in range(KC):
                    P.op("pe", lambda e: e.matmul(ps[:], W4[:, f, c, :], k.XB[:, c, sl], start=(c == 0), stop=(c == KC - 1)),
                         reads=["W4", ("XB", n)], writes=[pk])
                P.op("act", lambda e: e.activation(out=PRE[:, 3 + n * 512:3 + (n + 1) * 512], in_=ps[:], func=AF.Copy), reads=[pk], writes=["PRE"])
            ci = f * 8 + h
            for hf in range(2):
                eng = "dve"
                o = hf * 1024
                osl = slice(o, o + 1024)
                P.op(eng, lambda e: e.tensor_scalar(out=dst[:, osl], in0=PRE[:, 3 + o:3 + o + 1024], scalar1=CW[:, 3 * 24 + ci:3 * 24 + ci + 1], scalar2=None, op0=ALU.mult),
                     reads=["PRE", "CW"], writes=[dk])
                for j in (2, 1, 0):
                    P.op(eng, lambda e: e.scalar_tensor_tensor(out=dst[:, osl], in0=PRE[:, j + o:j + o + 1024], scalar=CW[:, j * 24 + ci:j * 24 + ci + 1], in1=dst[:, osl],
                                                               op0=ALU.mult, op1=ALU.add), reads=["PRE", "CW", dk], writes=[dk])
            for n in range(NCH):
                sl = slice(n * 512, (n + 1) * 512)
                P.op("act", lambda e: e.activation(out=dst[:, sl], in_=dst[:, sl], func=AF.Silu), reads=[dk], writes=[dk])
            if f < 2:
                for n in range(NCH):
                    sl = slice(n * 512, (n + 1) * 512)
                    P.op("act", lambda e: e.activation(out=VF[:, sl], in_=dst[:, sl], func=AF.Square), reads=[dk], writes=["VF"])
                    ps, pk = _nps(k)
                    P.op("pe", lambda e: e.matmul(ps[:], ones[:], VF[:, sl], start=True, stop=True), reads=["ones", "VF"], writes=[pk])
                    qs = 128.0 if f == 0 else 1.0
                    P.op("dve", lambda e: e.tensor_scalar(out=RS1[:], in0=ps[:], scalar1=qs, scalar2=DN_EPS * qs, op0=ALU.mult, op1=ALU.add), reads=[pk], writes=["RS1"])
                    P.op("act", lambda e: e.activation(out=RS2[:], in_=RS1[:], func=AF.Sqrt), reads=["RS1"], writes=["RS2"])
                    P.op("dve", lambda e: e.reciprocal(out=RS1[:], in_=RS2[:]), reads=["RS2"], writes=["RS1"])
                    P.op("pool", lambda e: e.tensor_tensor(out=dst[:, sl], in0=dst[:, sl], in1=RS1[:], op=ALU.mult), reads=[dk, "RS1"], writes=[dk])
        for n in range(NCH):
            sl = slice(n * 512, (n + 1) * 512)
            ps, pk = _nps(k)
            for c in range(KC):
                P.op("pe", lambda e: e.matmul(ps[:], W4[:, 3, c, :], k.XB[:, c, sl], start=(c == 0), stop=(c == KC - 1)),
                     reads=["W4", ("XB", n)], writes=[pk])
            P.op("act", lambda e: e.activation(out=SG[:, sl], in_=ps[:], func=AF.Silu), reads=[pk], writes=["SG"])
        P.op("dve", lambda e: e.memset(Sst[:], 0.0), writes=["Sst"])
        P.op("dve", lambda e: e.memset(Sb[:], 0.0), writes=["Sb"])
        if DN_STOP == "A":
            P.pop()
            return

        for b in range(NT // TB):
            t0 = b * TB

            def bc(T):
                return T[:, t0:t0 + TB, h:h + 1].broadcast_to([128, TB, 128])

            def tl(i):
                return slice((t0 + i) * 128, (t0 + i + 1) * 128)

            def cs(i):
                return slice(i * 128, (i + 1) * 128)
            psk, pkk = _nps(k)
            psv, pkv = _nps(k)
            for i in range(TB):
                P.op("pe", lambda e: e.transpose(psk[:, cs(i)], KF[:, tl(i)], ident[:]), reads=["KF", "ident"], writes=[pkk])
                P.op("pe", lambda e: e.transpose(psv[:, cs(i)], VF[:, tl(i)], ident[:]), reads=["VF", "ident"], writes=[pkv])
            P.op("dve", lambda e: e.tensor_tensor(out=KDEC[:], in0=v3(psk[:, 0:W]), in1=bc(EKD), op=ALU.mult), reads=[pkk, "EKD"], writes=["KDEC"])
            P.op("dve", lambda e: e.tensor_tensor(out=R[:, :, 128:256], in0=v3(psk[:, 0:W]), in1=bc(BEG), op=ALU.mult), reads=[pkk, "BEG"], writes=["Rr"])
            P.op("dve", lambda e: e.tensor_tensor(out=R[:, :, 0:128], in0=v3(psv[:, 0:W]), in1=bc(BETA), op=ALU.mult), reads=[pkv, "BETA"], writes=["Rr"])
            P.op("pool", lambda e: e.tensor_tensor(out=TRIG[:], in0=b3(TRI_LE), in1=bc(G), op=ALU.mult), reads=["tri_le", "G"], writes=["TRIG"])
            psd, pkd = _nps(k)
            psdt, pkdt = _nps(k)
            psg, pkg = _nps(k)
            for i in range(TB):
                P.op("pe", lambda e: e.matmul(psd[:, cs(i)], TRIG[:, i, :], TRI_GT[:], start=True, stop=True), reads=["TRIG", "tri_gt"], writes=[pkd])
                P.op("pe", lambda e: e.matmul(psdt[:, cs(i)], TRI_GT[:], TRIG[:, i, :], start=True, stop=True), reads=["TRIG", "tri_gt"], writes=[pkdt])
                P.op("pe", lambda e: e.matmul(psg[:, cs(i)], ones[:], TRIG[:, i, :], start=True, stop=True), reads=["TRIG", "ones"], writes=[pkg])
            P.op("act", lambda e: e.activation(out=ED[:], in_=v3(psd[:, 0:W]), func=AF.Exp), reads=[pkd], writes=["ED"])
            P.op("act", lambda e: e.activation(out=EDT[:], in_=v3(psdt[:, 0:W]), func=AF.Exp), reads=[pkdt], writes=["EDT"])
            P.op("act", lambda e: e.activation(out=EGB[:], in_=v3(psg[:, 0:W]), func=AF.Exp), reads=[pkg], writes=["EGB"])
            pskk, pkkk = _nps(k)
            psqk, pkqk = _nps(k)
            for i in range(TB):
                P.op("pe", lambda e: e.matmul(pskk[:, cs(i)], KF[:, tl(i)], KF[:, tl(i)], start=True, stop=True), reads=["KF"], writes=[pkkk])
                P.op("pe", lambda e: e.matmul(psqk[:, cs(i)], KF[:, tl(i)], QF[:, tl(i)], start=True, stop=True), reads=["KF", "QF"], writes=[pkqk])
            P.op("pool", lambda e: e.tensor_tensor(out=ED[:], in0=ED[:], in1=bc(BETA), op=ALU.mult), reads=["ED", "BETA"], writes=["ED"])
            P.op("dve", lambda e: e.tensor_tensor(out=LF[:], in0=v3(pskk[:, 0:W]), in1=ED[:], op=ALU.mult), reads=[pkkk, "ED"], writes=["LF"])
            P.op("pool", lambda e: e.tensor_tensor(out=Nn[0][:], in0=LF[:], in1=b3(NMBD), op=ALU.mult), reads=["LF", "nmask_bd"], writes=["Nn0"])
            P.op("pool", lambda e: e.tensor_tensor(out=LOFF[:], in0=LF[:], in1=b3(MOFF), op=ALU.mult), reads=["LF", "mask_off"], writes=["LOFF"])
            P.op("pool", lambda e: e.tensor_tensor(out=EDT[:], in0=EDT[:], in1=b3(TRI_LE), op=ALU.mult), reads=["EDT", "tri_le"], writes=["EDT"])
            P.op("dve", lambda e: e.tensor_tensor(out=INTRAT[:], in0=v3(psqk[:, 0:W]), in1=EDT[:], op=ALU.mult), reads=[pkqk, "EDT"], writes=["INTRAT"])
            P.op("pool", lambda e: e.tensor_tensor(out=QDT[:], in0=v3(QF[:, t0 * 128:(t0 + TB) * 128]), in1=EGB[:], op=ALU.mult), reads=["QF", "EGB"], writes=["QDT"])
            if DN_STOP == "B1":
                P.pop()
                return
            psa, pka = _nps(k)
            psb, pkb = _nps(k)
            for i in range(TB):
                P.op("pe", lambda e: e.transpose(psa[:, cs(i)], Nn[0][:, i, :], ident[:]), reads=["Nn0", "ident"], writes=[pka])
                P.op("pe", lambda e: e.transpose(psb[:, cs(i)], LOFF[:, i, :], ident[:]), reads=["LOFF", "ident"], writes=[pkb])
            P.op("act", lambda e: e.activation(out=Mm[0][:], in_=v3(psa[:, 0:W]), func=AF.Copy), reads=[pka], writes=["Mm0"])
            P.op("act", lambda e: e.activation(out=LOFFT[:], in_=v3(psb[:, 0:W]), func=AF.Copy), reads=[pkb], writes=["LOFFT"])
            P.op("dve", lambda e: e.tensor_tensor(out=Tt[:], in0=Mm[0][:], in1=b3(ident), op=ALU.add), reads=["Mm0", "ident"], writes=["Tt"])
            if DN_STOP == "T":
                P.pop()
                return
            for kk in range(1, 6):
                a, bb = (kk - 1) % 2, kk % 2
                if DN_STOP == f"kk{kk}":
                    P.pop()
                    return
                psn, pkn = _nps(k)
                for i in range(TB):
                    P.op("pe", lambda e: e.matmul(psn[:, cs(i)], Mm[a][:, i, :], Nn[a][:, i, :], start=True, stop=True), reads=[f"Mm{a}", f"Nn{a}"], writes=[pkn])
                if kk < 5:
                    psm, pkm = _nps(k)
                    for i in range(TB):
                        P.op("pe", lambda e: e.matmul(psm[:, cs(i)], Nn[a][:, i, :], Mm[a][:, i, :], start=True, stop=True), reads=[f"Mm{a}", f"Nn{a}"], writes=[pkm])
                P.op("act", lambda e: e.activation(out=Nn[bb][:], in_=v3(psn[:, 0:W]), func=AF.Copy), reads=[pkn], writes=[f"Nn{bb}"])
                if kk < 5:
                    P.op("dve", lambda e: e.tensor_copy(out=Mm[bb][:], in_=v3(psm[:, 0:W])), reads=[pkm], writes=[f"Mm{bb}"])
                psc, pkc = _nps(k)
                for i in range(TB):
                    P.op("pe", lambda e: e.matmul(psc[:, cs(i)], Nn[bb][:, i, :], Tt[:, i, :], start=True, stop=True), reads=[f"Nn{bb}", "Tt"], writes=[pkc])
                P.op("dve", lambda e: e.tensor_tensor(out=Tt[:], in0=v3(psc[:, 0:W]), in1=Tt[:], op=ALU.add), reads=[pkc, "Tt"], writes=["Tt"])
            if DN_STOP == "B2":
                P.pop()
                return
            v256 = lambda ap: ap.rearrange("p (t n) -> p t n", t=TB)
            pss, pks = _nps(k)
            for i in range(TB):
                P.op("pe", lambda e: e.matmul(pss[:, i * 256:(i + 1) * 256], Tt[:, i, :], R[:, i, :], start=True, stop=True), reads=["Tt", "Rr"], writes=[pks])
            P.op("act", lambda e: e.activation(out=SOL[:], in_=v256(pss[:, 0:TB * 256]), func=AF.Copy), reads=[pks], writes=["SOL"])
            psz, pkz = _nps(k)
            for i in range(TB):
                P.op("pe", lambda e: e.matmul(psz[:, i * 256:(i + 1) * 256], LOFFT[:, i, :], SOL[:, i, :], start=True, stop=True), reads=["LOFFT", "SOL"], writes=[pkz])
            P.op("act", lambda e: e.activation(out=R[:], in_=v256(psz[:, 0:TB * 256]), func=AF.Copy), reads=[pkz], writes=["Rr"])
            psc2, pkc2 = _nps(k)
            for i in range(TB):
                P.op("pe", lambda e: e.matmul(psc2[:, i * 256:(i + 1) * 256], Tt[:, i, :], R[:, i, :], start=True, stop=True), reads=["Tt", "Rr"], writes=[pkc2])
            P.op("dve", lambda e: e.scalar_tensor_tensor(out=SOL[:], in0=v256(psc2[:, 0:TB * 256]), scalar=-1.0, in1=SOL[:], op0=ALU.mult, op1=ALU.add),
                 reads=[pkc2, "SOL"], writes=["SOL"])
            psw, pkw = _nps(k)
            for i in range(TB):
                P.op("pe", lambda e: e.transpose(psw[:, cs(i)], SOL[:, i, 128:256], ident[:]), reads=["SOL", "ident"], writes=[pkw])
            P.op("act", lambda e: e.activation(out=WT[:], in_=v3(psw[:, 0:W]), func=AF.Copy), reads=[pkw], writes=["WT"])
            if DN_STOP == "B3":
                P.pop()
                return
            for i in range(TB):
                t = t0 + i
                vn = VNEW[t % 2]
                vk = f"VNEW{t % 2}"
                ps1, pk1 = _nps(k)
                P.op("pe", lambda e: e.matmul(ps1[:, 0:128], WT[:, i, :], Sb[:], start=True, stop=True), reads=["WT", "Sb"], writes=[pk1])
                P.op("dve", lambda e: e.scalar_tensor_tensor(out=vn[:], in0=ps1[:, 0:128], scalar=-1.0, in1=SOL[:, i, 0:128], op0=ALU.mult, op1=ALU.add),
                     reads=[pk1, "SOL"], writes=[vk])
                ps2, pk2 = _nps(k)
                P.op("pe", lambda e: e.matmul(ps2[:, 0:128], Sb[:], QDT[:, i, :], start=True, stop=False), reads=["Sb", "QDT"], writes=[pk2])
                P.op("pe", lambda e: e.matmul(ps2[:, 0:128], vn[:], INTRAT[:, i, :], start=False, stop=True), reads=[vk, "INTRAT"], writes=[pk2])
                P.op("act", lambda e: e.activation(out=OT[:, 3 + t * 128:3 + (t + 1) * 128], in_=ps2[:, 0:128], func=AF.Copy), reads=[pk2], writes=["PRE"])
                ps3, pk3 = _nps(k)
                P.op("pe", lambda e: e.matmul(ps3[:, 0:128], KDEC[:, i, :], vn[:], start=True, stop=True), reads=["KDEC", vk], writes=[pk3])
                P.op("dve", lambda e: e.scalar_tensor_tensor(out=Sst[:], in0=Sst[:], scalar=EGL[:, t, h:h + 1], in1=ps3[:, 0:128], op0=ALU.mult, op1=ALU.add),
                     reads=[pk3, "Sst", "EGL"], writes=["Sst"])
                P.op("act", lambda e: e.activation(out=Sb[:], in_=Sst[:], func=AF.Copy), reads=["Sst"], writes=["Sb"])
        if DN_STOP == "B4":
            P.pop()
            return
        if DN_STOP == "dump":
            for nm, T, kk_ in (("QF", QF, "QF"), ("KF", KF, "KF"), ("VF", VF, "VF")):
                P.dma("sp", k.dbg[nm], T[:], reads=[kk_], writes=["dbg"], key="dbg")
            P.dma("sp", k.dbg["OT"], OT[:, 3:3 + S], reads=["PRE"], writes=["dbg"], key="dbg")
            for nm, T in (("G", G), ("BETA", BETA), ("EKD", EKD), ("BEG", BEG), ("EGL", EGL)):
                P.dma("sp", k.dbg[nm], T[:], reads=[nm], writes=["dbg"], key="dbg")
            P.pop()
            return
        for n in range(NCH):
            sl = slice(n * 512, (n + 1) * 512)
            osl = slice(3 + n * 512, 3 + (n + 1) * 512)
            P.op("act", lambda e: e.activation(out=VF[:, sl], in_=OT[:, osl], func=AF.Square), reads=["PRE"], writes=["VF"])
            ps, pk = _nps(k)
            P.op("pe", lambda e: e.matmul(ps[:], ones[:], VF[:, sl], start=True, stop=True), reads=["ones", "VF"], writes=[pk])
            P.op("dve", lambda e: e.tensor_scalar(out=RS1[:], in0=ps[:], scalar1=1.0 / 128.0, scalar2=DN_EPS, op0=ALU.mult, op1=ALU.add), reads=[pk], writes=["RS1"])
            P.op("act", lambda e: e.activation(out=RS2[:], in_=RS1[:], func=AF.Sqrt), reads=["RS1"], writes=["RS2"])
            P.op("dve", lambda e: e.reciprocal(out=RS1[:], in_=RS2[:]), reads=["RS2"], writes=["RS1"])
            P.op("pool", lambda e: e.tensor_tensor(out=VF[:, sl], in0=OT[:, osl], in1=RS1[:], op=ALU.mult), reads=["PRE", "RS1", "VF"], writes=["VF"])
            P.op("dve", lambda e: e.scalar_tensor_tensor(out=OB[:, sl], in0=VF[:, sl], scalar=NW[:, 0:1], in1=SG[:, sl], op0=ALU.mult, op1=ALU.mult),
                 reads=["VF", "NW", "SG"], writes=["dnOB"])
        for m in range(KC):
            for n in range(NCH):
                sl = slice(n * 512, (n + 1) * 512)
                ps, pk = _nps(k)
                P.op("pe", lambda e: e.matmul(ps[:], WO[:, m * 128:(m + 1) * 128], OB[:, sl], start=True, stop=True), reads=["dnWO", "dnOB"], writes=[pk])
                P.op("dve", lambda e: e.tensor_tensor(out=k.XT[:, m, sl], in0=ps[:], in1=k.XT[:, m, sl], op=ALU.add), reads=[pk, ("XT", n)], writes=[("XT", n)])
    P.pop()
    P.push()
    k.layer_norm(0, k.ln_tmp())
    P.pop()


def from_moba(k):
    P = k.P
    din = k.din
    dc = k.dc
    ident, ones, onesb = k.ident, k.ones, k.onesb
    SCALE = 128.0 ** -0.5
    BIG = 30000.0
    P.push()

    def cload(name, src, shape, dt=F32, q="sp"):
        t = P.sb(name, shape, dt)
        P.dma(q, t[:], src, writes=[name], key="mbc")
        return t
    PERM = cload("perm", dc["perm"], [128, 128])
    ROPEC = cload("ropec", dc["ropec"], [128, S])
    ROPES = cload("ropes", dc["ropes"], [128, S])
    PASTB = cload("pastb", dc["pastb"], [128, NT, 8])
    PAST01 = cload("past01", dc["past01"], [128, NT, 8])
    OWN01 = cload("own01", dc["own01"], [128, NT, 8])
    SEL8b = cload("sel8b", dc["sel8"], [8, 8, 128], BF16, q="pool")
    CAUS = cload("caus4", dc["caus4"], [128, 4, 512], BF16, q="pool")

    k.scale_xt()

    Wq = P.sb("mWq", [128, KC, 128], BF16)
    Wk = P.sb("mWk", [128, KC, 128], BF16)
    Wv = P.sb("mWv", [128, KC, 128], BF16)
    WO = P.sb("mWO", [128, D], BF16)
    QF = P.sb("mQF", [128, S])
    KF = P.sb("mKF", [128, S])
    QB = P.sb("mQB", [128, S], BF16)
    KB = P.sb("mKB", [128, S], BF16)
    VTM = P.sb("mVTM", [128, NT, 128], BF16)
    OB = P.sb("mOB", [128, S], BF16)
    RAW = [P.sb(f"mRAW{i}", [128, 512]) for i in range(2)]
    T1 = P.sb("mT1", [128, 512])
    T2 = P.sb("mT2", [128, 512])
    PT = [P.sb(f"mPT{i}", [128, 512], BF16) for i in range(2)]
    RL = P.sb("mRL", [128, 512])
    KM = P.sb("mKM", [128, 8])
    GATE = P.sb("mGATE", [128, NT, 8])
    M8 = P.sb("mM8", [128, NT, 8])
    SEL = P.sb("mSEL", [128, NT, 8])
    BIAST = P.sb("mBIAST", [8, S], BF16)
    ri = 0
    pi = 0
    for h in range(8):
        P.dma("pool", Wq[:], din["b_w_q"][:, h * 128:(h + 1) * 128].rearrange("(c p) f -> p c f", p=128), writes=["mWq"], key="mWq")
        P.dma("pool", Wk[:], din["b_w_kv"][:, h * 128:(h + 1) * 128].rearrange("(c p) f -> p c f", p=128), writes=["mWk"], key="mWk")
        P.dma("pool", Wv[:], din["b_w_kv"][:, D + h * 128:D + (h + 1) * 128].rearrange("(c p) f -> p c f", p=128), writes=["mWv"], key="mWv")
        P.dma("pool", WO[:], din["b_w_o"][h * 128:(h + 1) * 128, :], writes=["mWO"], key="mWO")
        for (Wt, wk_, Ff, fk, Bb, bk) in ((Wq, "mWq", QF, "mQF", QB, "mQB"), (Wk, "mWk", KF, "mKF", KB, "mKB")):
            for n in range(NCH):
                sl = slice(n * 512, (n + 1) * 512)
                ps, pk = _nps(k)
                for c in range(KC):
                    P.op("pe", lambda e: e.matmul(ps[:], Wt[:, c, :], k.XB[:, c, sl], start=(c == 0), stop=(c == KC - 1)), reads=[wk_, ("XB", n)], writes=[pk])
                raw, rk = RAW[ri % 2], f"mRAW{ri % 2}"
                ri += 1
                P.op("act", lambda e: e.activation(out=raw[:], in_=ps[:], func=AF.Copy), reads=[pk], writes=[rk])
                ps2, pk2 = _nps(k)
                P.op("pe", lambda e: e.matmul(ps2[:], PERM[:], raw[:], start=True, stop=True), reads=["perm", rk], writes=[pk2])
                P.op("dve", lambda e: e.tensor_tensor(out=T1[:], in0=ps2[:], in1=ROPES[:, sl], op=ALU.mult), reads=[pk2, "ropes"], writes=["mT1"])
                P.op("pool", lambda e: e.tensor_tensor(out=T2[:], in0=raw[:], in1=ROPEC[:, sl], op=ALU.mult), reads=[rk, "ropec"], writes=["mT2"])
                P.op("pool", lambda e: e.tensor_tensor(out=Ff[:, sl], in0=T1[:], in1=T2[:], op=ALU.add), reads=["mT1", "mT2"], writes=[fk])
                P.op("act", lambda e: e.activation(out=Bb[:, sl], in_=Ff[:, sl], func=AF.Copy), reads=[fk], writes=[bk])
        P.op("dve", lambda e: e.tensor_reduce(out=KM[:], in_=KF[:].rearrange("p (b n) -> p b n", n=256), axis=AX.X, op=ALU.add), reads=["mKF"], writes=["mKM"])
        P.op("dve", lambda e: e.tensor_scalar(out=KM[:], in0=KM[:], scalar1=1.0 / 256.0, scalar2=None, op0=ALU.mult), reads=["mKM"], writes=["mKM"])
        for g in range(4):
            ps, pk = _nps(k)
            for i in range(4):
                t = 4 * g + i
                for c in range(KC):
                    P.op("pe", lambda e: e.matmul(ps[:, i * 128:(i + 1) * 128], k.XB[:, c, t * 128:(t + 1) * 128], Wv[:, c, :], start=(c == 0), stop=(c == KC - 1)),
                         reads=["mWv", ("XB", g)], writes=[pk])
            P.op("act", lambda e: e.activation(out=VTM[:, 4 * g:4 * g + 4, :], in_=ps[:].rearrange("p (t n) -> p t n", t=4), func=AF.Copy), reads=[pk], writes=["mVTM"])
        ps, pk = _nps(k)
        for t in range(NT):
            P.op("pe", lambda e: e.matmul(ps[:, t * 8:(t + 1) * 8], QF[:, t * 128:(t + 1) * 128], KM[:], start=True, stop=True), reads=["mQF", "mKM"], writes=[pk])
        P.op("dve", lambda e: e.tensor_tensor(out=GATE[:], in0=ps[:, 0:NT * 8].rearrange("p (t n) -> p t n", n=8), in1=PASTB[:], op=ALU.add), reads=[pk, "pastb"], writes=["mGATE"])
        for t in range(NT):
            P.op("dve", lambda e: e.max(out=M8[:, t, :], in_=GATE[:, t, :]), reads=["mGATE"], writes=["mM8"])
        P.op("dve", lambda e: e.tensor_tensor(out=SEL[:], in0=GATE[:], in1=M8[:, :, 2:3].broadcast_to([128, NT, 8]), op=ALU.is_ge), reads=["mGATE", "mM8"], writes=["mSEL"])
        P.op("dve", lambda e: e.tensor_tensor(out=SEL[:], in0=SEL[:], in1=PAST01[:], op=ALU.mult), reads=["mSEL", "past01"], writes=["mSEL"])
        P.op("dve", lambda e: e.tensor_tensor(out=SEL[:], in0=SEL[:], in1=OWN01[:], op=ALU.add), reads=["mSEL", "own01"], writes=["mSEL"])
        P.op("dve", lambda e: e.tensor_scalar(out=SEL[:], in0=SEL[:], scalar1=-1.0, scalar2=BIG, op0=ALU.add, op1=ALU.mult), reads=["mSEL"], writes=["mSEL"])
        for g in range(4):
            ps, pk = _nps(k)
            for i in range(4):
                t = 4 * g + i
                P.op("pe", lambda e: e.transpose(ps[0:8, i * 128:(i + 1) * 128], SEL[:, t, :], ident[:]), reads=["mSEL", "ident"], writes=[pk])
            P.op("act", lambda e: e.activation(out=BIAST[:, g * 512:(g + 1) * 512], in_=ps[0:8, :], func=AF.Copy), reads=[pk], writes=["mBIAST"])
        for qc in range(NCH):
            sl = slice(qc * 512, (qc + 1) * 512)
            pso, pko = _nps(k)
            psl, pkl = _nps(k)
            njt = 4 * qc + 4
            for jt in range(njt):
                while k.psi in (int(pko[2:]), int(pkl[2:])):
                    k.psi = (k.psi + 1) % 8
                pss, pks = _nps(k)
                P.op("pe", lambda e: e.matmul(pss[:], KB[:, jt * 128:(jt + 1) * 128], QB[:, sl], start=True, stop=False), reads=["mKB", "mQB"], writes=[pks])
                P.op("pe", lambda e: e.matmul(pss[:], SEL8b[:, jt // 2, :], BIAST[:, sl], start=False, stop=True), reads=["sel8b", "mBIAST"], writes=[pks])
                pt, ptk = PT[pi % 2], f"mPT{pi % 2}"
                pi += 1
                P.op("act", lambda e: e.activation(out=pt[:], in_=pss[:], func=AF.Exp, scale=SCALE), reads=[pks], writes=[ptk])
                if jt >= 4 * qc:
                    P.op("pool", lambda e: e.tensor_tensor(out=pt[:], in0=pt[:], in1=CAUS[:, jt - 4 * qc, :], op=ALU.mult), reads=[ptk, "caus4"], writes=[ptk])
                P.op("pe", lambda e: e.matmul(pso[:], VTM[:, jt, :], pt[:], start=(jt == 0), stop=(jt == njt - 1)), reads=["mVTM", ptk], writes=[pko])
                P.op("pe", lambda e: e.matmul(psl[:], onesb[:], pt[:], start=(jt == 0), stop=(jt == njt - 1)), reads=["onesb", ptk], writes=[pkl])
            P.op("dve", lambda e: e.reciprocal(out=RL[:], in_=psl[:]), reads=[pkl], writes=["mRL"])
            P.op("dve", lambda e: e.tensor_tensor(out=OB[:, sl], in0=pso[:], in1=RL[:], op=ALU.mult), reads=[pko, "mRL"], writes=["mOB"])
        for m in range(KC):
            for n in range(NCH):
                sl = slice(n * 512, (n + 1) * 512)
                ps, pk = _nps(k)
                P.op("pe", lambda e: e.matmul(ps[:], WO[:, m * 128:(m + 1) * 128], OB[:, sl], start=True, stop=True), reads=["mWO", "mOB"], writes=[pk])
                P.op("dve", lambda e: e.tensor_tensor(out=k.XT[:, m, sl], in0=ps[:], in1=k.XT[:, m, sl], op=ALU.add), reads=[pk, ("XT", n)], writes=[("XT", n)])
    P.pop()
    P.push()
    k.layer_norm(2, k.ln_tmp())
    P.pop()


def make_in_maps(inputs, n_cores=8):
    c = _consts()
    shared = {}
    for name in IN_SHAPES:
        if name == "x":
            continue
        a = np.ascontiguousarray(np.asarray(inputs[name], dtype=np.float32))
        shared[name] = a.reshape(IN_SHAPES[name])
    for name, v in c.items():
        shared["c_" + name] = np.ascontiguousarray(v)
    x = np.asarray(inputs["x"], dtype=np.float32)
    maps = []
    for b in range(n_cores):
        m = dict(shared)
        m["x"] = np.ascontiguousarray(x[b])
        maps.append(m)
    return maps


def kernel(**inputs):
    nc = build()
    maps = make_in_maps(inputs)
    res = run_bass_kernel_spmd(nc, maps, core_ids=list(range(8)))
    return np.stack([np.asarray(r["out"], dtype=np.float32) for r in res.results], axis=0)
```

```python
from contextlib import ExitStack
import numpy as np
import concourse.bass as bass
import concourse.mybir as mybir
from concourse.bass_utils import run_bass_kernel_spmd

F32 = mybir.dt.float32
BF16 = mybir.dt.bfloat16
ALU = mybir.AluOpType
AF = mybir.ActivationFunctionType
AX = mybir.AxisListType

S = 2048
D = 1024
NT = 16
NCH = 4
KC = 8
FH = 3584
NF = 28
NE = 8
ALPHA = 4.0 ** 0.25
LN_EPS = 1e-5
DN_EPS = 1e-6
SEM_ROLL = 20000


class Prog:
    ENG = ("pe", "act", "dve", "pool", "sp")

    def __init__(self, nc):
        self.nc = nc
        self.es = ExitStack()
        self.scopes = [self.es]
        self.e = {"pe": nc.tensor, "act": nc.scalar, "dve": nc.vector, "pool": nc.gpsimd, "sp": nc.sync}
        self.sem = {}
        self.cnt = {}
        self.nsem = 0
        for k in self.ENG:
            self._newsem(k)
        self.waited = {k: {} for k in self.ENG}
        self.last_w = {}
        self.readers = {}
        self.dsem = {}
        self.group_reads = []
        self.n_inst = 0

    def _newsem(self, k):
        name = f"s_{k}_{self.nsem}"
        s = self.es.enter_context(self.nc.semaphore(name))
        self.nsem += 1
        self.sem[k] = (name, s)
        self.cnt[k] = 0

    def sb(self, name, shape, dt=F32):
        return self.scopes[-1].enter_context(self.nc.sbuf_tensor(name, list(shape), dt))

    def push(self):
        self.scopes.append(ExitStack())

    def pop(self):
        self.barrier()
        self.scopes.pop().close()

    def ps(self, name, shape, dt=F32):
        return self.es.enter_context(self.nc.psum_tensor(name, list(shape), dt))

    def _deps(self, engine, reads, writes):
        need = {}

        def add(rec):
            name, s, val, e = rec
            if e == engine and engine == "pe":
                return
            if name not in need or need[name][1] < val:
                need[name] = (s, val)
        for r in list(reads) + self.group_reads:
            if r in self.last_w:
                add(self.last_w[r])
        for w in writes:
            if w in self.last_w:
                add(self.last_w[w])
            for rec in self.readers.get(w, ()):
                add(rec)
        for name, (s, val) in need.items():
            if self.waited[engine].get(name, 0) >= val:
                continue
            self.e[engine].wait_ge(s, val)
            self.waited[engine][name] = val

    def _record(self, rec, reads, writes):
        for r in reads:
            self.readers.setdefault(r, []).append(rec)
        for w in writes:
            self.last_w[w] = rec
            self.readers[w] = []

    def op(self, engine, fn, reads=(), writes=()):
        self._deps(engine, reads, writes)
        inst = fn(self.e[engine])
        name, s = self.sem[engine]
        self.cnt[engine] += 1
        val = self.cnt[engine]
        inst.then_inc(s, 1)
        self.n_inst += 1
        self._record((name, s, val, engine), reads, writes)
        if val >= SEM_ROLL:
            self._newsem(engine)
        return inst

    GROUPS = ("c0", "dnc", "mbc", "wr")

    def dma(self, queue, out, in_, reads=(), writes=(), key=None, **kw):
        if key in self.GROUPS:
            gk = ("grp", key)
            writes = list(writes) + [gk]
            if gk not in self.group_reads:
                self.group_reads.append(gk)
        self._deps(queue, reads, writes)
        if key not in self.dsem:
            name = f"d_{self.nsem}"
            s = self.es.enter_context(self.nc.semaphore(name))
            self.nsem += 1
            self.dsem[key] = [name, s, 0]
        ent = self.dsem[key]
        inst = self.e[queue].dma_start(out=out, in_=in_, **kw)
        ent[2] += 16
        inst.then_inc(ent[1], 16)
        self.n_inst += 1
        self._record((ent[0], ent[1], ent[2], "dma"), reads, writes)
        return inst

    def barrier(self):
        recs = []
        for k in self.ENG:
            name, s = self.sem[k]
            if self.cnt[k] > 0:
                recs.append((name, s, self.cnt[k], k))
        for key, ent in self.dsem.items():
            if ent[2] > 0:
                recs.append((ent[0], ent[1], ent[2], "dma"))
        for k in self.ENG:
            for name, s, val, src in recs:
                if self.waited[k].get(name, 0) >= val:
                    continue
                if src == k and k == "pe":
                    continue
                self.e[k].wait_ge(s, val)
                self.waited[k][name] = val

    def close(self):
        self.barrier()
        self.es.close()


def _consts():
    c = {}
    c["ident"] = np.eye(128, dtype=np.float32)
    c["ones"] = np.ones((128, 128), dtype=np.float32)
    half = 64
    inv_freq = (10000.0 ** (-(np.arange(half, dtype=np.float32) / np.float32(half)))).astype(np.float32)
    ang = (np.arange(S, dtype=np.float32)[:, None] * inv_freq[None, :]).astype(np.float32)
    cos = np.cos(ang.astype(np.float64)).astype(np.float32).T
    sin = np.sin(ang.astype(np.float64)).astype(np.float32).T
    c["ropec"] = np.concatenate([cos, cos], axis=0)
    c["ropes"] = np.concatenate([-sin, sin], axis=0)
    sel = np.zeros((8, 8, 128), dtype=np.float32)
    for e in range(8):
        sel[e, e, :] = 1.0
    c["sel8"] = sel.transpose(1, 0, 2).copy()
    r = np.arange(128)[:, None]
    q = np.arange(128)[None, :]
    c["tri_le"] = (r <= q).astype(np.float32)
    c["tri_gt"] = (r > q).astype(np.float32)
    c["nmask_bd"] = -((r > q) & ((r // 64) == (q // 64))).astype(np.float32)
    c["mask_off"] = ((r >= 64) & (q < 64)).astype(np.float32)
    c["perm"] = (r == ((q + 64) % 128)).astype(np.float32)
    pb = np.zeros((128, NT, 8), dtype=np.float32)
    p01 = np.zeros((128, NT, 8), dtype=np.float32)
    own = np.zeros((128, NT, 8), dtype=np.float32)
    for t in range(NT):
        qb = t // 2
        own[:, t, qb] = 1.0
        for n in range(8):
            if n < qb:
                p01[:, t, n] = 1.0
            else:
                pb[:, t, n] = -1.0e30
    c["pastb"] = pb
    c["past01"] = p01
    c["own01"] = own
    caus = np.ones((128, 4, 512), dtype=np.float32)
    j = np.arange(128)[:, None]
    ii = np.arange(512)[None, :]
    for pos in range(4):
        same = (pos // 2) == (ii // 256)
        caus[:, pos, :] = np.where(same & ((128 * pos + j) > ii), 0.0, 1.0)
    c["caus4"] = caus
    return c


CONST_SHAPES = {"ident": [128, 128], "ones": [128, 128], "ropec": [128, S], "ropes": [128, S],
                "sel8": [8, 8, 128], "tri_le": [128, 128], "tri_gt": [128, 128], "nmask_bd": [128, 128],
                "mask_off": [128, 128], "perm": [128, 128], "pastb": [128, NT, 8], "past01": [128, NT, 8],
                "own01": [128, NT, 8], "caus4": [128, 4, 512]}

IN_SHAPES = {
    "x": [S, D], "a_w_in": [D, 4112], "a_conv_w": [4, 3072], "a_log_decay": [1, 8], "a_dt_bias": [1, 8],
    "a_norm_w": [1, 128], "a_w_out": [D, D], "b_w_kv": [D, 2 * D], "b_w_q": [D, D], "b_w_o": [D, D],
    "ffn_w1": [D, FH], "ffn_w3": [D, FH], "ffn_w2": [FH, D], "moe_router": [D, 8],
    "moe_w1": [8, D, FH], "moe_w3": [8, D, FH], "moe_w2": [8, FH, D], "ln_g": [4, D], "ln_b": [4, D],
}


class K:
    def __init__(self, stages, dbg=()):
        self.stages = stages
        nc = bass.Bass("TRN2", target_bir_lowering=False)
        self.nc = nc
        self.P = Prog(nc)
        P = self.P
        self.din = {k: nc.dram_tensor(k, v, F32, kind="ExternalInput").ap() for k, v in IN_SHAPES.items()}
        self.dc = {k: nc.dram_tensor("c_" + k, v, F32, kind="ExternalInput").ap() for k, v in CONST_SHAPES.items()}
        self.out = nc.dram_tensor("out", [S, D], F32, kind="ExternalOutput").ap()
        self.dbg = {}
        for name, shape in dbg:
            self.dbg[name] = nc.dram_tensor("dbg_" + name, shape, F32, kind="ExternalOutput").ap()
        self.uid = 0
        self.XT = P.sb("XT", [128, KC, S])
        self.XB = P.sb("XB", [128, KC, S], BF16)
        self.ident = P.sb("ident", [128, 128])
        self.identb = P.sb("identb", [128, 128], BF16)
        self.ones = P.sb("ones", [128, 128])
        self.onesb = P.sb("onesb", [128, 128], BF16)
        self.lng = P.sb("lng", [128, 4, KC])
        self.lnb = P.sb("lnb", [128, 4, KC])
        self.PS = [P.ps(f"PS{i}", [128, 512]) for i in range(8)]
        self.psk = [f"PS{i}" for i in range(8)]

        P.dma("sp", self.ident[:], self.dc["ident"], writes=["ident"], key="c0")
        P.dma("sp", self.ones[:], self.dc["ones"], writes=["ones"], key="c0")
        lnraw = P.sb("lnraw", [64, 128])
        P.dma("sp", lnraw[0:32, :], self.din["ln_g"].rearrange("l (c p) -> (l c) p", p=128), writes=["lnraw"], key="c0")
        P.dma("sp", lnraw[32:64, :], self.din["ln_b"].rearrange("l (c p) -> (l c) p", p=128), writes=["lnraw"], key="c0")
        P.op("pe", lambda e: e.transpose(self.PS[0][:, 0:64], lnraw[:], self.ident[0:64, 0:64]), reads=["lnraw", "ident"], writes=["PS0"])
        P.op("dve", lambda e: e.tensor_copy(out=self.lng[:], in_=self.PS[0][:, 0:32].rearrange("p (l c) -> p l c", l=4)), reads=["PS0"], writes=["lng"])
        P.op("dve", lambda e: e.tensor_copy(out=self.lnb[:], in_=self.PS[0][:, 32:64].rearrange("p (l c) -> p l c", l=4)), reads=["PS0"], writes=["lnb"])
        P.op("dve", lambda e: e.tensor_copy(out=self.identb[:], in_=self.ident[:]), reads=["ident"], writes=["identb"])
        P.op("dve", lambda e: e.tensor_copy(out=self.onesb[:], in_=self.ones[:]), reads=["ones"], writes=["onesb"])

    def u(self, s):
        self.uid += 1
        return f"{s}{self.uid}"

    def load_x(self):
        P = self.P
        xin = [P.sb(f"xin{i}", [128, D]) for i in range(2)]
        for t in range(NT):
            b = xin[t % 2]
            bk = f"xin{t % 2}"
            P.dma("sp", b[:], self.din["x"][t * 128:(t + 1) * 128, :], writes=[bk], key=bk)
            for hlf in range(2):
                ps = self.PS[2 * (t % 2) + hlf]
                pk = self.psk[2 * (t % 2) + hlf]
                for c4 in range(4):
                    c = hlf * 4 + c4
                    P.op("pe", lambda e: e.transpose(ps[:, c4 * 128:(c4 + 1) * 128], b[:, c * 128:(c + 1) * 128], self.ident[:]),
                         reads=[bk, "ident"], writes=[pk])
                src = ps[:].rearrange("p (c n) -> p c n", c=4)
                P.op("act", lambda e: e.activation(out=self.XT[:, hlf * 4:(hlf + 1) * 4, t * 128:(t + 1) * 128], in_=src, func=AF.Copy),
                     reads=[pk], writes=[("XT", t // 4)])
                P.op("dve", lambda e: e.tensor_copy(out=self.XB[:, hlf * 4:(hlf + 1) * 4, t * 128:(t + 1) * 128],
                                                    in_=self.XT[:, hlf * 4:(hlf + 1) * 4, t * 128:(t + 1) * 128]),
                     reads=[("XT", t // 4)], writes=[("XB", t // 4)])

    def store_x(self):
        P = self.P
        xo = [P.sb(f"xo{i}", [128, D]) for i in range(2)]
        for t in range(NT):
            b = xo[t % 2]
            bk = f"xo{t % 2}"
            for hlf in range(2):
                ps = self.PS[2 * (t % 2) + hlf]
                pk = self.psk[2 * (t % 2) + hlf]
                for c4 in range(4):
                    c = hlf * 4 + c4
                    P.op("pe", lambda e: e.transpose(ps[:, c4 * 128:(c4 + 1) * 128], self.XT[:, c, t * 128:(t + 1) * 128], self.ident[:]),
                         reads=[("XT", t // 4), "ident"], writes=[pk])
                if hlf == 0:
                    P.op("act", lambda e: e.activation(out=b[:, 0:512], in_=ps[:], func=AF.Copy), reads=[pk], writes=[bk])
                else:
                    P.op("dve", lambda e: e.tensor_copy(out=b[:, 512:1024], in_=ps[:]), reads=[pk], writes=[bk])
            P.dma("sp", self.out[t * 128:(t + 1) * 128, :], b[:], reads=[bk], writes=["out"], key=bk)

    def dbg_store_xt(self, name):
        P = self.P
        for c in range(KC):
            P.dma("sp", self.dbg[name][c], self.XT[:, c, :], reads=[("XT", n) for n in range(NCH)], writes=["dbg"], key="dbg")

    def layer_norm(self, idx, tmp):
        P = self.P
        SQ, R1, R2, MEAN, RSTD = tmp["sq"], tmp["r1"], tmp["r2"], tmp["mean"], tmp["rstd"]
        for n in range(NCH):
            sl = slice(n * 512, (n + 1) * 512)
            xk = ("XT", n)
            Yv = self.XT[:, :, sl]
            P.op("act", lambda e: e.activation(out=SQ[:], in_=Yv, func=AF.Square), reads=[xk], writes=["ln_sq"])
            P.op("dve", lambda e: e.tensor_reduce(out=R1[:], in_=Yv.rearrange("p c n -> p n c"), axis=AX.X, op=ALU.add),
                 reads=[xk], writes=["ln_r1"])
            P.op("dve", lambda e: e.tensor_reduce(out=R2[:], in_=SQ[:].rearrange("p c n -> p n c"), axis=AX.X, op=ALU.add),
                 reads=["ln_sq"], writes=["ln_r2"])
            ps1, pk1 = self.PS[6], self.psk[6]
            ps2, pk2 = self.PS[7], self.psk[7]
            P.op("pe", lambda e: e.matmul(ps1[:], self.ones[:], R1[:], start=True, stop=True), reads=["ones", "ln_r1"], writes=[pk1])
            P.op("pe", lambda e: e.matmul(ps2[:], self.ones[:], R2[:], start=True, stop=True), reads=["ones", "ln_r2"], writes=[pk2])
            P.op("act", lambda e: e.activation(out=MEAN[:], in_=ps1[:], func=AF.Copy, scale=1.0 / D), reads=[pk1], writes=["ln_mean"])
            P.op("dve", lambda e: e.tensor_tensor(out=R1[:], in0=MEAN[:], in1=MEAN[:], op=ALU.mult), reads=["ln_mean"], writes=["ln_r1"])
            P.op("dve", lambda e: e.scalar_tensor_tensor(out=R2[:], in0=ps2[:], scalar=1.0 / D, in1=R1[:], op0=ALU.mult, op1=ALU.subtract),
                 reads=[pk2, "ln_r1"], writes=["ln_r2"])
            P.op("dve", lambda e: e.tensor_scalar(out=R2[:], in0=R2[:], scalar1=LN_EPS, scalar2=None, op0=ALU.add), reads=["ln_r2"], writes=["ln_r2"])
            P.op("act", lambda e: e.activation(out=R1[:], in_=R2[:], func=AF.Sqrt), reads=["ln_r2"], writes=["ln_r1"])
            P.op("dve", lambda e: e.reciprocal(out=RSTD[:], in_=R1[:]), reads=["ln_r1"], writes=["ln_rstd"])
            mb = MEAN[:].unsqueeze(1).broadcast_to([128, KC, 512])
            rb = RSTD[:].unsqueeze(1).broadcast_to([128, KC, 512])
            P.op("dve", lambda e: e.tensor_tensor(out=SQ[:], in0=Yv, in1=mb, op=ALU.subtract), reads=[xk, "ln_mean"], writes=["ln_sq"])
            P.op("pool", lambda e: e.tensor_tensor(out=SQ[:], in0=SQ[:], in1=rb, op=ALU.mult), reads=["ln_sq", "ln_rstd"], writes=["ln_sq"])
            for c in range(KC):
                P.op("act", lambda e: e.activation(out=self.XT[:, c, sl], in_=SQ[:, c, :], func=AF.Identity,
                                                   scale=self.lng[:, idx, c:c + 1], bias=self.lnb[:, idx, c:c + 1]),
                     reads=["ln_sq", "lng", "lnb"], writes=[xk])
                P.op("dve", lambda e: e.tensor_scalar(out=self.XB[:, c, sl], in0=SQ[:, c, :], scalar1=self.lng[:, idx, c:c + 1],
                                                      scalar2=self.lnb[:, idx, c:c + 1], op0=ALU.mult, op1=ALU.add),
                     reads=["ln_sq", "lng", "lnb"], writes=[("XB", n)])

    def ln_tmp(self):
        P = self.P
        return {"sq": P.sb(self.u("ln_sq"), [128, KC, 512]), "r1": P.sb(self.u("ln_r1"), [128, 512]),
                "r2": P.sb(self.u("ln_r2"), [128, 512]), "mean": P.sb(self.u("ln_mean"), [128, 512]),
                "rstd": P.sb(self.u("ln_rstd"), [128, 512])}

    def scale_xt(self):
        P = self.P
        for n in range(NCH):
            sl = slice(n * 512, (n + 1) * 512)
            eng = "act" if n % 2 == 0 else "dve"
            if eng == "act":
                P.op("act", lambda e: e.activation(out=self.XT[:, :, sl], in_=self.XT[:, :, sl], func=AF.Copy, scale=ALPHA),
                     reads=[("XT", n)], writes=[("XT", n)])
            else:
                P.op("dve", lambda e: e.tensor_scalar(out=self.XT[:, :, sl], in0=self.XT[:, :, sl], scalar1=ALPHA, scalar2=None, op0=ALU.mult),
                     reads=[("XT", n)], writes=[("XT", n)])

    def ffn(self, experts, gb=None):
        P = self.P
        G = 4
        NB = 2
        W1 = [P.sb(self.u("w1g"), [128, KC, G * 128], BF16) for _ in range(NB)]
        W3 = [P.sb(self.u("w3g"), [128, KC, G * 128], BF16) for _ in range(NB)]
        W2 = [P.sb(self.u("w2g"), [128, G, D], BF16) for _ in range(NB)]
        H = [P.sb(self.u("hg"), [128, G, 512], BF16) for _ in range(2)]
        SA = [P.sb(self.u("sa"), [128, 512], BF16) for _ in range(2)]
        HT = [P.sb(self.u("ht"), [128, 512], BF16) for _ in range(2)]
        base = self.u("ffn")
        gi = 0
        si = 0
        hi = 0
        groups = [(ei, ex, fg) for ei, ex in enumerate(experts) for fg in range(NF // G)]

        def issue_load(k):
            ei, ex, fg = groups[k]
            w1, w3, w2, _ = ex
            slot = k % NB
            f0 = fg * G * 128
            P.dma("pool", W1[slot][:], w1[:, f0:f0 + G * 128].rearrange("(c p) f -> p c f", p=128),
                  writes=[(base, "w1", slot)], key=(base, "w1", slot))
            P.dma("pool", W3[slot][:], w3[:, f0:f0 + G * 128].rearrange("(c p) f -> p c f", p=128),
                  writes=[(base, "w3", slot)], key=(base, "w3", slot))
            P.dma("pool", W2[slot][:], w2[f0:f0 + G * 128, :].rearrange("(j p) m -> p j m", p=128),
                  writes=[(base, "w2", slot)], key=(base, "w2", slot))

        issue_load(0)
        gt = None
        gk = None
        for k, (ei, ex, fg) in enumerate(groups):
            if k + 1 < len(groups):
                issue_load(k + 1)
            slot = k % NB
            if ex[3] is None:
                gt = None
            elif fg == 0:
                gt, gk = gb(ex[3])
            for n in range(NCH):
                sl = slice(n * 512, (n + 1) * 512)
                hb = H[hi % 2]
                hk = (base, "h", hi % 2)
                hi += 1
                for j in range(G):
                    pa, pak = self.PS[2 * (si % 2)], self.psk[2 * (si % 2)]
                    pb, pbk = self.PS[2 * (si % 2) + 1], self.psk[2 * (si % 2) + 1]
                    sa, sak = SA[si % 2], (base, "sa", si % 2)
                    for c in range(KC):
                        P.op("pe", lambda e: e.matmul(pa[:], W1[slot][:, c, j * 128:(j + 1) * 128], self.XB[:, c, sl], start=(c == 0), stop=(c == KC - 1)),
                             reads=[(base, "w1", slot), ("XB", n)], writes=[pak])
                    for c in range(KC):
                        P.op("pe", lambda e: e.matmul(pb[:], W3[slot][:, c, j * 128:(j + 1) * 128], self.XB[:, c, sl], start=(c == 0), stop=(c == KC - 1)),
                             reads=[(base, "w3", slot), ("XB", n)], writes=[pbk])
                    P.op("act", lambda e: e.activation(out=sa[:], in_=pa[:], func=AF.Silu), reads=[pak], writes=[sak])
                    if gt is None:
                        P.op("dve", lambda e: e.tensor_tensor(out=hb[:, j, :], in0=pb[:], in1=sa[:], op=ALU.mult),
                             reads=[pbk, sak], writes=[hk])
                    else:
                        ht, htk = HT[si % 2], (base, "ht", si % 2)
                        P.op("pool", lambda e: e.tensor_tensor(out=ht[:], in0=sa[:], in1=gt[:, sl], op=ALU.mult),
                             reads=[sak, gk], writes=[htk])
                        P.op("dve", lambda e: e.tensor_tensor(out=hb[:, j, :], in0=pb[:], in1=ht[:], op=ALU.mult),
                             reads=[pbk, htk], writes=[hk])
                    si += 1
                for m in range(KC):
                    py, pyk = self.PS[4 + gi % 4], self.psk[4 + gi % 4]
                    gi += 1
                    for j in range(G):
                        P.op("pe", lambda e: e.matmul(py[:], W2[slot][:, j, m * 128:(m + 1) * 128], hb[:, j, :], start=(j == 0), stop=(j == G - 1)),
                             reads=[(base, "w2", slot), hk], writes=[pyk])
                    P.op("dve", lambda e: e.tensor_tensor(out=self.XT[:, m, sl], in0=py[:], in1=self.XT[:, m, sl], op=ALU.add),
                         reads=[pyk, ("XT", n)], writes=[("XT", n)])

    def router(self):
        P = self.P
        WR = P.sb("wr", [128, KC, 8])
        LG = P.sb("r_lg", [128, NT, 8])
        M8 = P.sb("r_m8", [128, NT, 8])
        EX = P.sb("r_ex", [128, NT, 8])
        SELM = P.sb("r_sel", [128, NT, 8])
        DEN = P.sb("r_den", [128, NT])
        GT = P.sb("r_gt", [8, S])
        SEL8 = P.sb("r_sel8", [8, 8, 128])
        P.dma("sp", WR[:], self.din["moe_router"].rearrange("(c p) e -> p c e", p=128), writes=["wr"], key="wr")
        P.dma("sp", SEL8[:], self.dc["sel8"], writes=["sel8"], key="wr")
        ps, pk = self.PS[0], self.psk[0]
        for t in range(NT):
            for c in range(KC):
                P.op("pe", lambda e: e.matmul(ps[:, t * 8:(t + 1) * 8], self.XT[:, c, t * 128:(t + 1) * 128], WR[:, c, :], start=(c == 0), stop=(c == KC - 1)),
                     reads=[("XT", t // 4), "wr"], writes=[pk])
        P.op("dve", lambda e: e.tensor_copy(out=LG[:], in_=ps[:, 0:NT * 8].rearrange("p (t e) -> p t e", e=8)), reads=[pk], writes=["r_lg"])
        for t in range(NT):
            P.op("dve", lambda e: e.max(out=M8[:, t, :], in_=LG[:, t, :]), reads=["r_lg"], writes=["r_m8"])
        m1 = M8[:, :, 0:1].broadcast_to([128, NT, 8])
        m2 = M8[:, :, 1:2].broadcast_to([128, NT, 8])
        P.op("dve", lambda e: e.tensor_tensor(out=EX[:], in0=LG[:], in1=m1, op=ALU.subtract), reads=["r_lg", "r_m8"], writes=["r_ex"])
        P.op("act", lambda e: e.activation(out=EX[:], in_=EX[:], func=AF.Exp), reads=["r_ex"], writes=["r_ex"])
        P.op("dve", lambda e: e.tensor_tensor(out=SELM[:], in0=LG[:], in1=m2, op=ALU.is_ge), reads=["r_lg", "r_m8"], writes=["r_sel"])
        P.op("dve", lambda e: e.tensor_tensor(out=EX[:], in0=EX[:], in1=SELM[:], op=ALU.mult), reads=["r_ex", "r_sel"], writes=["r_ex"])
        P.op("dve", lambda e: e.tensor_reduce(out=DEN[:], in_=EX[:], axis=AX.X, op=ALU.add), reads=["r_ex"], writes=["r_den"])
        P.op("dve", lambda e: e.reciprocal(out=DEN[:], in_=DEN[:]), reads=["r_den"], writes=["r_den"])
        P.op("dve", lambda e: e.tensor_tensor(out=EX[:], in0=EX[:], in1=DEN[:].unsqueeze(2).broadcast_to([128, NT, 8]), op=ALU.mult),
             reads=["r_ex", "r_den"], writes=["r_ex"])
        for q in range(4):
            pt, ptk = self.PS[1 + q % 2], self.psk[1 + q % 2]
            for tt in range(4):
                t = q * 4 + tt
                P.op("pe", lambda e: e.transpose(pt[0:8, tt * 128:(tt + 1) * 128], EX[:, t, :], self.ident[:]), reads=["r_ex", "ident"], writes=[ptk])
            P.op("act", lambda e: e.activation(out=GT[:, q * 512:(q + 1) * 512], in_=pt[0:8, :], func=AF.Copy), reads=[ptk], writes=["r_gt"])
        self.GT = GT
        self.SEL8 = SEL8
        self.GB = [P.sb(f"gb{i}", [128, S], BF16) for i in range(2)]
        if "g" in self.dbg:
            P.dma("sp", self.dbg["g"], EX[:], reads=["r_ex"], writes=["dbg"], key="dbg")

    def gate_bcast(self, eidx):
        P = self.P
        gbt = self.GB[eidx % 2]
        gk = ("gb", eidx % 2)
        for n in range(NCH):
            ps, pk = self.PS[6 + n % 2], self.psk[6 + n % 2]
            P.op("pe", lambda e: e.matmul(ps[:], self.SEL8[:, eidx, :], self.GT[:, n * 512:(n + 1) * 512], start=True, stop=True),
                 reads=["sel8", "r_gt"], writes=[pk])
            P.op("act", lambda e: e.activation(out=gbt[:, n * 512:(n + 1) * 512], in_=ps[:], func=AF.Copy), reads=[pk], writes=[gk])
        return gbt, gk


def build(stages=("load", "dn", "ffn0", "moba", "moe", "store"), dbg=()):
    k = K(stages, dbg)
    P = k.P
    din = k.din
    if "load" in stages:
        P.push()
        k.load_x()
        P.pop()
    if "dn" in stages:
        from_dn(k)
    if "ffn0" in stages:
        P.push()
        k.scale_xt()
        k.ffn([(din["ffn_w1"], din["ffn_w3"], din["ffn_w2"], None)])
        P.pop()
        P.push()
        k.layer_norm(1, k.ln_tmp())
        P.pop()
    if "dbg_x1" in k.dbg:
        k.dbg_store_xt("x1")
    if "moba" in stages:
        from_moba(k)
    if "moe" in stages:
        P.push()
        k.router()
        k.scale_xt()
        k.ffn([(din["moe_w1"][e], din["moe_w3"][e], din["moe_w2"][e], e) for e in range(NE)], gb=k.gate_bcast)
        P.pop()
        P.push()
        k.layer_norm(3, k.ln_tmp())
        P.pop()
    if "store" in stages:
        P.push()
        k.store_x()
        P.pop()
    P.close()
    return k.nc


def _nps(k):
    i = getattr(k, "psi", 0)
    k.psi = (i + 1) % 8
    return k.PS[i], k.psk[i]


DN_STOP = None


def from_dn(k):
    P = k.P
    din = k.din
    dc = k.dc
    TB = 4
    W = TB * 128
    QSCALE = 128.0 ** -0.5
    P.push()

    def cload(name, src, shape, dt=F32, q="sp"):
        t = P.sb(name, shape, dt)
        P.dma(q, t[:], src, writes=[name], key="dnc")
        return t

    TRI_LE = cload("tri_le", dc["tri_le"], [128, 128])
    TRI_GT = cload("tri_gt", dc["tri_gt"], [128, 128])
    NMBD = cload("nmask_bd", dc["nmask_bd"], [128, 128])
    MOFF = cload("mask_off", dc["mask_off"], [128, 128])
    ident, ones = k.ident, k.ones

    def b3(T):
        return T[:].unsqueeze(1).broadcast_to([128, TB, 128])

    def v3(ap):
        return ap.rearrange("p (t n) -> p t n", t=TB)

    cwraw = P.sb("cwraw", [96, 128])
    P.dma("sp", cwraw[:], din["a_conv_w"].rearrange("j (c p) -> (j c) p", p=128), writes=["cwraw"], key="dnc")
    CW = P.sb("CW", [128, 96])
    ps, pk = _nps(k)
    P.op("pe", lambda e: e.transpose(ps[:, 0:96], cwraw[:], ident[0:96, 0:96]), reads=["cwraw", "ident"], writes=[pk])
    P.op("dve", lambda e: e.tensor_copy(out=CW[:], in_=ps[:, 0:96]), reads=[pk], writes=["CW"])
    NW = P.sb("NW", [128, 1])
    P.dma("sp", NW[:], din["a_norm_w"].rearrange("o p -> p o"), writes=["NW"], key="dnc")

    WAB = P.sb("WAB", [128, KC, 16])
    P.dma("sp", WAB[:], din["a_w_in"][:, 4096:4112].rearrange("(c p) f -> p c f", p=128), writes=["WAB"], key="dnc")
    RW = P.sb("RW", [1, 16])
    P.dma("sp", RW[0:1, 0:8], din["a_log_decay"], writes=["RW"], key="dnc")
    P.dma("sp", RW[0:1, 8:16], din["a_dt_bias"], writes=["RW"], key="dnc")
    AB = P.sb("AB", [128, NT, 16])
    BC = P.sb("BC", [128, 16])
    ps, pk = _nps(k)
    for t in range(NT):
        for c in range(KC):
            P.op("pe", lambda e: e.matmul(ps[:, t * 16:(t + 1) * 16], k.XT[:, c, t * 128:(t + 1) * 128], WAB[:, c, :], start=(c == 0), stop=(c == KC - 1)),
                 reads=[("XT", t // 4), "WAB"], writes=[pk])
    P.op("dve", lambda e: e.tensor_copy(out=AB[:], in_=ps[:, 0:NT * 16].rearrange("p (t f) -> p t f", f=16)), reads=[pk], writes=["AB"])
    ps, pk = _nps(k)
    P.op("pe", lambda e: e.matmul(ps[:, 0:16], ones[0:1, :], RW[0:1, :], start=True, stop=True), reads=["ones", "RW"], writes=[pk])
    P.op("dve", lambda e: e.tensor_copy(out=BC[:], in_=ps[:, 0:16]), reads=[pk], writes=["BC"])

    def tm(name):
        return P.sb(name, [128, NT, 8])
    G, BETA, EKD, BEG, EGL, TM1, TM2, GCS = (tm(n) for n in ("G", "BETA", "EKD", "BEG", "EGL", "TM1", "TM2", "GCS"))
    EA = P.sb("EA", [128, 8])
    dtb = BC[:, 8:16].unsqueeze(1).broadcast_to([128, NT, 8])
    P.op("dve", lambda e: e.tensor_tensor(out=TM1[:], in0=AB[:, :, 0:8], in1=dtb, op=ALU.add), reads=["AB", "BC"], writes=["TM1"])
    P.op("act", lambda e: e.activation(out=TM1[:], in_=TM1[:], func=AF.Exp), reads=["TM1"], writes=["TM1"])
    P.op("dve", lambda e: e.tensor_scalar(out=TM1[:], in0=TM1[:], scalar1=1.0, scalar2=None, op0=ALU.add), reads=["TM1"], writes=["TM1"])
    P.op("act", lambda e: e.activation(out=TM1[:], in_=TM1[:], func=AF.Ln), reads=["TM1"], writes=["TM1"])
    P.op("act", lambda e: e.activation(out=EA[:], in_=BC[:, 0:8], func=AF.Exp), reads=["BC"], writes=["EA"])
    P.op("dve", lambda e: e.scalar_tensor_tensor(out=G[:], in0=TM1[:], scalar=-1.0, in1=EA[:].unsqueeze(1).broadcast_to([128, NT, 8]),
                                                 op0=ALU.mult, op1=ALU.mult), reads=["TM1", "EA"], writes=["G"])
    P.op("act", lambda e: e.activation(out=TM2[:], in_=AB[:, :, 8:16], func=AF.Exp, scale=-1.0), reads=["AB"], writes=["TM2"])
    P.op("dve", lambda e: e.tensor_scalar(out=TM2[:], in0=TM2[:], scalar1=1.0, scalar2=None, op0=ALU.add), reads=["TM2"], writes=["TM2"])
    P.op("dve", lambda e: e.reciprocal(out=BETA[:], in_=TM2[:]), reads=["TM2"], writes=["BETA"])
    Gf = G[:].rearrange("p t h -> p (t h)")
    psc, pkc = _nps(k)
    psl, pkl = _nps(k)
    P.op("pe", lambda e: e.matmul(psc[:, 0:128], TRI_LE[:], Gf, start=True, stop=True), reads=["tri_le", "G"], writes=[pkc])
    P.op("pe", lambda e: e.matmul(psl[:, 0:128], ones[:], Gf, start=True, stop=True), reads=["ones", "G"], writes=[pkl])
    f3 = lambda ap: ap.rearrange("p (t h) -> p t h", h=8)
    P.op("act", lambda e: e.activation(out=GCS[:], in_=f3(psc[:, 0:128]), func=AF.Copy), reads=[pkc], writes=["GCS"])
    P.op("act", lambda e: e.activation(out=EGL[:], in_=f3(psl[:, 0:128]), func=AF.Exp), reads=[pkl], writes=["EGL"])
    P.op("dve", lambda e: e.tensor_tensor(out=TM2[:], in0=f3(psl[:, 0:128]), in1=GCS[:], op=ALU.subtract), reads=[pkl, "GCS", "BETA", "EGL"], writes=["TM2"])
    P.op("act", lambda e: e.activation(out=EKD[:], in_=TM2[:], func=AF.Exp), reads=["TM2"], writes=["EKD"])
    P.op("act", lambda e: e.activation(out=TM1[:], in_=GCS[:], func=AF.Exp), reads=["GCS", "G"], writes=["TM1"])
    P.op("dve", lambda e: e.tensor_tensor(out=BEG[:], in0=TM1[:], in1=BETA[:], op=ALU.mult), reads=["TM1", "BETA"], writes=["BEG"])

    k.scale_xt()
    if DN_STOP == "tm":
        P.pop()
        return

    W4 = P.sb("W4", [128, 4, KC, 128], BF16)
    WO = P.sb("dnWO", [128, D], BF16)
    PRE = P.sb("PRE", [128, S + 3])
    QF = P.sb("QF", [128, S])
    KF = P.sb("KF", [128, S])
    VF = P.sb("VF", [128, S])
    SG = P.sb("SG", [128, S], BF16)
    OB = P.sb("dnOB", [128, S], BF16)
    RS1 = P.sb("RS1", [128, 512])
    RS2 = P.sb("RS2", [128, 512])
    Sst = P.sb("Sst", [128, 128])
    Sb = P.sb("Sb", [128, 128], BF16)
    TRIG = P.sb("TRIG", [128, TB, 128])
    ED = P.sb("ED", [128, TB, 128])
    EDT = P.sb("EDT", [128, TB, 128])
    EGB = P.sb("EGB", [128, TB, 128])
    LF = P.sb("LF", [128, TB, 128])
    Nn = [P.sb(f"Nn{i}", [128, TB, 128]) for i in range(2)]
    Mm = [P.sb(f"Mm{i}", [128, TB, 128]) for i in range(2)]
    LOFF = P.sb("LOFF", [128, TB, 128])
    LOFFT = P.sb("LOFFT", [128, TB, 128])
    Tt = P.sb("Tt", [128, TB, 128])
    R = P.sb("Rr", [128, TB, 256])
    SOL = P.sb("SOL", [128, TB, 256])
    KDEC = P.sb("KDEC", [128, TB, 128], BF16)
    WT = P.sb("WT", [128, TB, 128], BF16)
    INTRAT = P.sb("INTRAT", [128, TB, 128], BF16)
    QDT = P.sb("QDT", [128, TB, 128], BF16)
    VNEW = [P.sb(f"VNEW{i}", [128, 128], BF16) for i in range(2)]
    P.op("dve", lambda e: e.memset(PRE[:, 0:3], 0.0), writes=["PRE"])
    OT = PRE

    for h in range(8):
        for f in range(4):
            c0 = f * D + h * 128
            P.dma("pool", W4[:, f, :, :], din["a_w_in"][:, c0:c0 + 128].rearrange("(c p) f -> p c f", p=128), writes=["W4"], key="W4")
        P.dma("pool", WO[:], din["a_w_out"][h * 128:(h + 1) * 128, :], writes=["dnWO"], key="dnWO")
        for f in range(3):
            dst, dk = ((QF, "QF"), (KF, "KF"), (VF, "VF"))[f]
            for n in range(NCH):
                sl = slice(n * 512, (n + 1) * 512)
                ps, pk = _nps(k)
                for c in range(KC):
                    P.op("pe", lambda e: e.matmul(ps[:], W4[:, f, c, :], k.XB[:, c, sl], start=(c == 0), stop=(c == KC - 1)),
                         reads=["W4", ("XB", n)], writes=[pk])
                P.op("act", lambda e: e.activation(out=PRE[:, 3 + n * 512:3 + (n + 1) * 512], in_=ps[:], func=AF.Copy), reads=[pk], writes=["PRE"])
            ci = f * 8 + h
            for hf in range(2):
                eng = "dve"
                o = hf * 1024
                osl = slice(o, o + 1024)
                P.op(eng, lambda e: e.tensor_scalar(out=dst[:, osl], in0=PRE[:, 3 + o:3 + o + 1024], scalar1=CW[:, 3 * 24 + ci:3 * 24 + ci + 1], scalar2=None, op0=ALU.mult),
                     reads=["PRE", "CW"], writes=[dk])
                for j in (2, 1, 0):
                    P.op(eng, lambda e: e.scalar_tensor_tensor(out=dst[:, osl], in0=PRE[:, j + o:j + o + 1024], scalar=CW[:, j * 24 + ci:j * 24 + ci + 1], in1=dst[:, osl],
                                                               op0=ALU.mult, op1=ALU.add), reads=["PRE", "CW", dk], writes=[dk])
            for n in range(NCH):
                sl = slice(n * 512, (n + 1) * 512)
                P.op("act", lambda e: e.activation(out=dst[:, sl], in_=dst[:, sl], func=AF.Silu), reads=[dk], writes=[dk])
            if f < 2:
                for n in range(NCH):
                    sl = slice(n * 512, (n + 1) * 512)
                    P.op("act", lambda e: e.activation(out=VF[:, sl], in_=dst[:, sl], func=AF.Square), reads=[dk], writes=["VF"])
                    ps, pk = _nps(k)
                    P.op("pe", lambda e: e.matmul(ps[:], ones[:], VF[:, sl], start=True, stop=True), reads=["ones", "VF"], writes=[pk])
                    qs = 128.0 if f == 0 else 1.0
                    P.op("dve", lambda e: e.tensor_scalar(out=RS1[:], in0=ps[:], scalar1=qs, scalar2=DN_EPS * qs, op0=ALU.mult, op1=ALU.add), reads=[pk], writes=["RS1"])
                    P.op("act", lambda e: e.activation(out=RS2[:], in_=RS1[:], func=AF.Sqrt), reads=["RS1"], writes=["RS2"])
                    P.op("dve", lambda e: e.reciprocal(out=RS1[:], in_=RS2[:]), reads=["RS2"], writes=["RS1"])
                    P.op("pool", lambda e: e.tensor_tensor(out=dst[:, sl], in0=dst[:, sl], in1=RS1[:], op=ALU.mult), reads=[dk, "RS1"], writes=[dk])
        for n in range(NCH):
            sl = slice(n * 512, (n + 1) * 512)
            ps, pk = _nps(k)
            for c in range(KC):
                P.op("pe", lambda e: e.matmul(ps[:], W4[:, 3, c, :], k.XB[:, c, sl], start=(c == 0), stop=(c == KC - 1)),
                     reads=["W4", ("XB", n)], writes=[pk])
            P.op("act", lambda e: e.activation(out=SG[:, sl], in_=ps[:], func=AF.Silu), reads=[pk], writes=["SG"])
        P.op("dve", lambda e: e.memset(Sst[:], 0.0), writes=["Sst"])
        P.op("dve", lambda e: e.memset(Sb[:], 0.0), writes=["Sb"])
        if DN_STOP == "A":
            P.pop()
            return

        for b in range(NT // TB):
            t0 = b * TB

            def bc(T):
                return T[:, t0:t0 + TB, h:h + 1].broadcast_to([128, TB, 128])

            def tl(i):
                return slice((t0 + i) * 128, (t0 + i + 1) * 128)

            def cs(i):
                return slice(i * 128, (i + 1) * 128)
            psk, pkk = _nps(k)
            psv, pkv = _nps(k)
            for i in range(TB):
                P.op("pe", lambda e: e.transpose(psk[:, cs(i)], KF[:, tl(i)], ident[:]), reads=["KF", "ident"], writes=[pkk])
                P.op("pe", lambda e: e.transpose(psv[:, cs(i)], VF[:, tl(i)], ident[:]), reads=["VF", "ident"], writes=[pkv])
            P.op("dve", lambda e: e.tensor_tensor(out=KDEC[:], in0=v3(psk[:, 0:W]), in1=bc(EKD), op=ALU.mult), reads=[pkk, "EKD"], writes=["KDEC"])
            P.op("dve", lambda e: e.tensor_tensor(out=R[:, :, 128:256], in0=v3(psk[:, 0:W]), in1=bc(BEG), op=ALU.mult), reads=[pkk, "BEG"], writes=["Rr"])
            P.op("dve", lambda e: e.tensor_tensor(out=R[:, :, 0:128], in0=v3(psv[:, 0:W]), in1=bc(BETA), op=ALU.mult), reads=[pkv, "BETA"], writes=["Rr"])
            P.op("pool", lambda e: e.tensor_tensor(out=TRIG[:], in0=b3(TRI_LE), in1=bc(G), op=ALU.mult), reads=["tri_le", "G"], writes=["TRIG"])
            psd, pkd = _nps(k)
            psdt, pkdt = _nps(k)
            psg, pkg = _nps(k)
            for i in range(TB):
                P.op("pe", lambda e: e.matmul(psd[:, cs(i)], TRIG[:, i, :], TRI_GT[:], start=True, stop=True), reads=["TRIG", "tri_gt"], writes=[pkd])
                P.op("pe", lambda e: e.matmul(psdt[:, cs(i)], TRI_GT[:], TRIG[:, i, :], start=True, stop=True), reads=["TRIG", "tri_gt"], writes=[pkdt])
                P.op("pe", lambda e: e.matmul(psg[:, cs(i)], ones[:], TRIG[:, i, :], start=True, stop=True), reads=["TRIG", "ones"], writes=[pkg])
            P.op("act", lambda e: e.activation(out=ED[:], in_=v3(psd[:, 0:W]), func=AF.Exp), reads=[pkd], writes=["ED"])
            P.op("act", lambda e: e.activation(out=EDT[:], in_=v3(psdt[:, 0:W]), func=AF.Exp), reads=[pkdt], writes=["EDT"])
            P.op("act", lambda e: e.activation(out=EGB[:], in_=v3(psg[:, 0:W]), func=AF.Exp), reads=[pkg], writes=["EGB"])
            pskk, pkkk = _nps(k)
            psqk, pkqk = _nps(k)
            for i in range(TB):
                P.op("pe", lambda e: e.matmul(pskk[:, cs(i)], KF[:, tl(i)], KF[:, tl(i)], start=True, stop=True), reads=["KF"], writes=[pkkk])
                P.op("pe", lambda e: e.matmul(psqk[:, cs(i)], KF[:, tl(i)], QF[:, tl(i)], start=True, stop=True), reads=["KF", "QF"], writes=[pkqk])
            P.op("pool", lambda e: e.tensor_tensor(out=ED[:], in0=ED[:], in1=bc(BETA), op=ALU.mult), reads=["ED", "BETA"], writes=["ED"])
            P.op("dve", lambda e: e.tensor_tensor(out=LF[:], in0=v3(pskk[:, 0:W]), in1=ED[:], op=ALU.mult), reads=[pkkk, "ED"], writes=["LF"])
            P.op("pool", lambda e: e.tensor_tensor(out=Nn[0][:], in0=LF[:], in1=b3(NMBD), op=ALU.mult), reads=["LF", "nmask_bd"], writes=["Nn0"])
            P.op("pool", lambda e: e.tensor_tensor(out=LOFF[:], in0=LF[:], in1=b3(MOFF), op=ALU.mult), reads=["LF", "mask_off"], writes=["LOFF"])
            P.op("pool", lambda e: e.tensor_tensor(out=EDT[:], in0=EDT[:], in1=b3(TRI_LE), op=ALU.mult), reads=["EDT", "tri_le"], writes=["EDT"])
            P.op("dve", lambda e: e.tensor_tensor(out=INTRAT[:], in0=v3(psqk[:, 0:W]), in1=EDT[:], op=ALU.mult), reads=[pkqk, "EDT"], writes=["INTRAT"])
            P.op("pool", lambda e: e.tensor_tensor(out=QDT[:], in0=v3(QF[:, t0 * 128:(t0 + TB) * 128]), in1=EGB[:], op=ALU.mult), reads=["QF", "EGB"], writes=["QDT"])
            if DN_STOP == "B1":
                P.pop()
                return
            psa, pka = _nps(k)
            psb, pkb = _nps(k)
            for i in range(TB):
                P.op("pe", lambda e: e.transpose(psa[:, cs(i)], Nn[0][:, i, :], ident[:]), reads=["Nn0", "ident"], writes=[pka])
                P.op("pe", lambda e: e.transpose(psb[:, cs(i)], LOFF[:, i, :], ident[:]), reads=["LOFF", "ident"], writes=[pkb])
            P.op("act", lambda e: e.activation(out=Mm[0][:], in_=v3(psa[:, 0:W]), func=AF.Copy), reads=[pka], writes=["Mm0"])
            P.op("act", lambda e: e.activation(out=LOFFT[:], in_=v3(psb[:, 0:W]), func=AF.Copy), reads=[pkb], writes=["LOFFT"])
            P.op("dve", lambda e: e.tensor_tensor(out=Tt[:], in0=Mm[0][:], in1=b3(ident), op=ALU.add), reads=["Mm0", "ident"], writes=["Tt"])
            if DN_STOP == "T":
                P.pop()
                return
            for kk in range(1, 6):
                a, bb = (kk - 1) % 2, kk % 2
                if DN_STOP == f"kk{kk}":
                    P.pop()
                    return
                psn, pkn = _nps(k)
                for i in range(TB):
                    P.op("pe", lambda e: e.matmul(psn[:, cs(i)], Mm[a][:, i, :], Nn[a][:, i, :], start=True, stop=True), reads=[f"Mm{a}", f"Nn{a}"], writes=[pkn])
                if kk < 5:
                    psm, pkm = _nps(k)
                    for i in range(TB):
                        P.op("pe", lambda e: e.matmul(psm[:, cs(i)], Nn[a][:, i, :], Mm[a][:, i, :], start=True, stop=True), reads=[f"Mm{a}", f"Nn{a}"], writes=[pkm])
                P.op("act", lambda e: e.activation(out=Nn[bb][:], in_=v3(psn[:, 0:W]), func=AF.Copy), reads=[pkn], writes=[f"Nn{bb}"])
                if kk < 5:
                    P.op("dve", lambda e: e.tensor_copy(out=Mm[bb][:], in_=v3(psm[:, 0:W])), reads=[pkm], writes=[f"Mm{bb}"])
                psc, pkc = _nps(k)
                for i in range(TB):
                    P.op("pe", lambda e: e.matmul(psc[:, cs(i)], Nn[bb][:, i, :], Tt[:, i, :], start=True, stop=True), reads=[f"Nn{bb}", "Tt"], writes=[pkc])
                P.op("dve", lambda e: e.tensor_tensor(out=Tt[:], in0=v3(psc[:, 0:W]), in1=Tt[:], op=ALU.add), reads=[pkc, "Tt"], writes=["Tt"])
            if DN_STOP == "B2":
                P.pop()
                return
            v256 = lambda ap: ap.rearrange("p (t n) -> p t n", t=2)
            for hp in range(TB // 2):
                pss, pks = _nps(k)
                for i2 in range(2):
                    i = 2 * hp + i2
                    P.op("pe", lambda e: e.matmul(pss[:, i2 * 256:(i2 + 1) * 256], Tt[:, i, :], R[:, i, :], start=True, stop=True), reads=["Tt", "Rr"], writes=[pks])
                P.op("act", lambda e: e.activation(out=SOL[:, 2 * hp:2 * hp + 2, :], in_=v256(pss[:]), func=AF.Copy), reads=[pks], writes=["SOL"])
            for hp in range(TB // 2):
                psz, pkz = _nps(k)
                for i2 in range(2):
                    i = 2 * hp + i2
                    P.op("pe", lambda e: e.matmul(psz[:, i2 * 256:(i2 + 1) * 256], LOFFT[:, i, :], SOL[:, i, :], start=True, stop=True), reads=["LOFFT", "SOL"], writes=[pkz])
                P.op("act", lambda e: e.activation(out=R[:, 2 * hp:2 * hp + 2, :], in_=v256(psz[:]), func=AF.Copy), reads=[pkz], writes=["Rr"])
            for hp in range(TB // 2):
                psc2, pkc2 = _nps(k)
                for i2 in range(2):
                    i = 2 * hp + i2
                    P.op("pe", lambda e: e.matmul(psc2[:, i2 * 256:(i2 + 1) * 256], Tt[:, i, :], R[:, i, :], start=True, stop=True), reads=["Tt", "Rr"], writes=[pkc2])
                P.op("dve", lambda e: e.scalar_tensor_tensor(out=SOL[:, 2 * hp:2 * hp + 2, :], in0=v256(psc2[:]), scalar=-1.0, in1=SOL[:, 2 * hp:2 * hp + 2, :], op0=ALU.mult, op1=ALU.add),
                     reads=[pkc2, "SOL"], writes=["SOL"])
            psw, pkw = _nps(k)
            for i in range(TB):
                P.op("pe", lambda e: e.transpose(psw[:, cs(i)], SOL[:, i, 128:256], ident[:]), reads=["SOL", "ident"], writes=[pkw])
            P.op("act", lambda e: e.activation(out=WT[:], in_=v3(psw[:, 0:W]), func=AF.Copy), reads=[pkw], writes=["WT"])
            if DN_STOP == "B3":
                P.pop()
                return
            for i in range(TB):
                t = t0 + i
                vn = VNEW[t % 2]
                vk = f"VNEW{t % 2}"
                ps1, pk1 = _nps(k)
                P.op("pe", lambda e: e.matmul(ps1[:, 0:128], WT[:, i, :], Sb[:], start=True, stop=True), reads=["WT", "Sb"], writes=[pk1])
                P.op("dve", lambda e: e.scalar_tensor_tensor(out=vn[:], in0=ps1[:, 0:128], scalar=-1.0, in1=SOL[:, i, 0:128], op0=ALU.mult, op1=ALU.add),
                     reads=[pk1, "SOL"], writes=[vk])
                ps2, pk2 = _nps(k)
                P.op("pe", lambda e: e.matmul(ps2[:, 0:128], Sb[:], QDT[:, i, :], start=True, stop=False), reads=["Sb", "QDT"], writes=[pk2])
                P.op("pe", lambda e: e.matmul(ps2[:, 0:128], vn[:], INTRAT[:, i, :], start=False, stop=True), reads=[vk, "INTRAT"], writes=[pk2])
                P.op("act", lambda e: e.activation(out=OT[:, 3 + t * 128:3 + (t + 1) * 128], in_=ps2[:, 0:128], func=AF.Copy), reads=[pk2], writes=["PRE"])
                ps3, pk3 = _nps(k)
                P.op("pe", lambda e: e.matmul(ps3[:, 0:128], KDEC[:, i, :], vn[:], start=True, stop=True), reads=["KDEC", vk], writes=[pk3])
                P.op("dve", lambda e: e.scalar_tensor_tensor(out=Sst[:], in0=Sst[:], scalar=EGL[:, t, h:h + 1], in1=ps3[:, 0:128], op0=ALU.mult, op1=ALU.add),
                     reads=[pk3, "Sst", "EGL"], writes=["Sst"])
                P.op("act", lambda e: e.activation(out=Sb[:], in_=Sst[:], func=AF.Copy), reads=["Sst"], writes=["Sb"])
        if DN_STOP == "B4":
            P.pop()
            return
        if DN_STOP == "dump":
            for nm, T, kk_ in (("QF", QF, "QF"), ("KF", KF, "KF"), ("VF", VF, "VF")):
                P.dma("sp", k.dbg[nm], T[:], reads=[kk_], writes=["dbg"], key="dbg")
            P.dma("sp", k.dbg["OT"], OT[:, 3:3 + S], reads=["PRE"], writes=["dbg"], key="dbg")
            for nm, T in (("G", G), ("BETA", BETA), ("EKD", EKD), ("BEG", BEG), ("EGL", EGL)):
                P.dma("sp", k.dbg[nm], T[:], reads=[nm], writes=["dbg"], key="dbg")
            P.pop()
            return
        for n in range(NCH):
            sl = slice(n * 512, (n + 1) * 512)
            osl = slice(3 + n * 512, 3 + (n + 1) * 512)
            P.op("act", lambda e: e.activation(out=VF[:, sl], in_=OT[:, osl], func=AF.Square), reads=["PRE"], writes=["VF"])
            ps, pk = _nps(k)
            P.op("pe", lambda e: e.matmul(ps[:], ones[:], VF[:, sl], start=True, stop=True), reads=["ones", "VF"], writes=[pk])
            P.op("dve", lambda e: e.tensor_scalar(out=RS1[:], in0=ps[:], scalar1=1.0 / 128.0, scalar2=DN_EPS, op0=ALU.mult, op1=ALU.add), reads=[pk], writes=["RS1"])
            P.op("act", lambda e: e.activation(out=RS2[:], in_=RS1[:], func=AF.Sqrt), reads=["RS1"], writes=["RS2"])
            P.op("dve", lambda e: e.reciprocal(out=RS1[:], in_=RS2[:]), reads=["RS2"], writes=["RS1"])
            P.op("pool", lambda e: e.tensor_tensor(out=VF[:, sl], in0=OT[:, osl], in1=RS1[:], op=ALU.mult), reads=["PRE", "RS1", "VF"], writes=["VF"])
            P.op("dve", lambda e: e.scalar_tensor_tensor(out=OB[:, sl], in0=VF[:, sl], scalar=NW[:, 0:1], in1=SG[:, sl], op0=ALU.mult, op1=ALU.mult),
                 reads=["VF", "NW", "SG"], writes=["dnOB"])
        for m in range(KC):
            for n in range(NCH):
                sl = slice(n * 512, (n + 1) * 512)
                ps, pk = _nps(k)
                P.op("pe", lambda e: e.matmul(ps[:], WO[:, m * 128:(m + 1) * 128], OB[:, sl], start=True, stop=True), reads=["dnWO", "dnOB"], writes=[pk])
                P.op("dve", lambda e: e.tensor_tensor(out=k.XT[:, m, sl], in0=ps[:], in1=k.XT[:, m, sl], op=ALU.add), reads=[pk, ("XT", n)], writes=[("XT", n)])
    P.pop()
    P.push()
    k.layer_norm(0, k.ln_tmp())
    P.pop()


def from_moba(k):
    P = k.P
    din = k.din
    dc = k.dc
    ident, ones, onesb = k.ident, k.ones, k.onesb
    SCALE = 128.0 ** -0.5
    BIG = 30000.0
    P.push()

    def cload(name, src, shape, dt=F32, q="sp"):
        t = P.sb(name, shape, dt)
        P.dma(q, t[:], src, writes=[name], key="mbc")
        return t
    PERM = cload("perm", dc["perm"], [128, 128])
    ROPEC = cload("ropec", dc["ropec"], [128, S])
    ROPES = cload("ropes", dc["ropes"], [128, S])
    PASTB = cload("pastb", dc["pastb"], [128, NT, 8])
    PAST01 = cload("past01", dc["past01"], [128, NT, 8])
    OWN01 = cload("own01", dc["own01"], [128, NT, 8])
    SEL8b = cload("sel8b", dc["sel8"], [8, 8, 128], BF16, q="pool")
    CAUS = cload("caus4", dc["caus4"], [128, 4, 512], BF16, q="pool")

    k.scale_xt()

    Wq = P.sb("mWq", [128, KC, 128], BF16)
    Wk = P.sb("mWk", [128, KC, 128], BF16)
    Wv = P.sb("mWv", [128, KC, 128], BF16)
    WO = P.sb("mWO", [128, D], BF16)
    QF = P.sb("mQF", [128, S])
    KF = P.sb("mKF", [128, S])
    QB = P.sb("mQB", [128, S], BF16)
    KB = P.sb("mKB", [128, S], BF16)
    VTM = P.sb("mVTM", [128, NT, 128], BF16)
    OB = P.sb("mOB", [128, S], BF16)
    RAW = [P.sb(f"mRAW{i}", [128, 512]) for i in range(2)]
    T1 = P.sb("mT1", [128, 512])
    T2 = P.sb("mT2", [128, 512])
    PT = [P.sb(f"mPT{i}", [128, 512], BF16) for i in range(2)]
    RL = P.sb("mRL", [128, 512])
    KM = P.sb("mKM", [128, 8])
    GATE = P.sb("mGATE", [128, NT, 8])
    M8 = P.sb("mM8", [128, NT, 8])
    SEL = P.sb("mSEL", [128, NT, 8])
    BIAST = P.sb("mBIAST", [8, S], BF16)
    ri = 0
    pi = 0
    for h in range(8):
        P.dma("pool", Wq[:], din["b_w_q"][:, h * 128:(h + 1) * 128].rearrange("(c p) f -> p c f", p=128), writes=["mWq"], key="mWq")
        P.dma("pool", Wk[:], din["b_w_kv"][:, h * 128:(h + 1) * 128].rearrange("(c p) f -> p c f", p=128), writes=["mWk"], key="mWk")
        P.dma("pool", Wv[:], din["b_w_kv"][:, D + h * 128:D + (h + 1) * 128].rearrange("(c p) f -> p c f", p=128), writes=["mWv"], key="mWv")
        P.dma("pool", WO[:], din["b_w_o"][h * 128:(h + 1) * 128, :], writes=["mWO"], key="mWO")
        for (Wt, wk_, Ff, fk, Bb, bk) in ((Wq, "mWq", QF, "mQF", QB, "mQB"), (Wk, "mWk", KF, "mKF", KB, "mKB")):
            for n in range(NCH):
                sl = slice(n * 512, (n + 1) * 512)
                ps, pk = _nps(k)
                for c in range(KC):
                    P.op("pe", lambda e: e.matmul(ps[:], Wt[:, c, :], k.XB[:, c, sl], start=(c == 0), stop=(c == KC - 1)), reads=[wk_, ("XB", n)], writes=[pk])
                raw, rk = RAW[ri % 2], f"mRAW{ri % 2}"
                ri += 1
                P.op("act", lambda e: e.activation(out=raw[:], in_=ps[:], func=AF.Copy), reads=[pk], writes=[rk])
                ps2, pk2 = _nps(k)
                P.op("pe", lambda e: e.matmul(ps2[:], PERM[:], raw[:], start=True, stop=True), reads=["perm", rk], writes=[pk2])
                P.op("dve", lambda e: e.tensor_tensor(out=T1[:], in0=ps2[:], in1=ROPES[:, sl], op=ALU.mult), reads=[pk2, "ropes"], writes=["mT1"])
                P.op("pool", lambda e: e.tensor_tensor(out=T2[:], in0=raw[:], in1=ROPEC[:, sl], op=ALU.mult), reads=[rk, "ropec"], writes=["mT2"])
                P.op("pool", lambda e: e.tensor_tensor(out=Ff[:, sl], in0=T1[:], in1=T2[:], op=ALU.add), reads=["mT1", "mT2"], writes=[fk])
                P.op("act", lambda e: e.activation(out=Bb[:, sl], in_=Ff[:, sl], func=AF.Copy), reads=[fk], writes=[bk])
        P.op("dve", lambda e: e.tensor_reduce(out=KM[:], in_=KF[:].rearrange("p (b n) -> p b n", n=256), axis=AX.X, op=ALU.add), reads=["mKF"], writes=["mKM"])
        P.op("dve", lambda e: e.tensor_scalar(out=KM[:], in0=KM[:], scalar1=1.0 / 256.0, scalar2=None, op0=ALU.mult), reads=["mKM"], writes=["mKM"])
        for g in range(4):
            ps, pk = _nps(k)
            for i in range(4):
                t = 4 * g + i
                for c in range(KC):
                    P.op("pe", lambda e: e.matmul(ps[:, i * 128:(i + 1) * 128], k.XB[:, c, t * 128:(t + 1) * 128], Wv[:, c, :], start=(c == 0), stop=(c == KC - 1)),
                         reads=["mWv", ("XB", g)], writes=[pk])
            P.op("act", lambda e: e.activation(out=VTM[:, 4 * g:4 * g + 4, :], in_=ps[:].rearrange("p (t n) -> p t n", t=4), func=AF.Copy), reads=[pk], writes=["mVTM"])
        ps, pk = _nps(k)
        for t in range(NT):
            P.op("pe", lambda e: e.matmul(ps[:, t * 8:(t + 1) * 8], QF[:, t * 128:(t + 1) * 128], KM[:], start=True, stop=True), reads=["mQF", "mKM"], writes=[pk])
        P.op("dve", lambda e: e.tensor_tensor(out=GATE[:], in0=ps[:, 0:NT * 8].rearrange("p (t n) -> p t n", n=8), in1=PASTB[:], op=ALU.add), reads=[pk, "pastb"], writes=["mGATE"])
        for t in range(NT):
            P.op("dve", lambda e: e.max(out=M8[:, t, :], in_=GATE[:, t, :]), reads=["mGATE"], writes=["mM8"])
        P.op("dve", lambda e: e.tensor_tensor(out=SEL[:], in0=GATE[:], in1=M8[:, :, 2:3].broadcast_to([128, NT, 8]), op=ALU.is_ge), reads=["mGATE", "mM8"], writes=["mSEL"])
        P.op("dve", lambda e: e.tensor_tensor(out=SEL[:], in0=SEL[:], in1=PAST01[:], op=ALU.mult), reads=["mSEL", "past01"], writes=["mSEL"])
        P.op("dve", lambda e: e.tensor_tensor(out=SEL[:], in0=SEL[:], in1=OWN01[:], op=ALU.add), reads=["mSEL", "own01"], writes=["mSEL"])
        P.op("dve", lambda e: e.tensor_scalar(out=SEL[:], in0=SEL[:], scalar1=-1.0, scalar2=BIG, op0=ALU.add, op1=ALU.mult), reads=["mSEL"], writes=["mSEL"])
        for g in range(4):
            ps, pk = _nps(k)
            for i in range(4):
                t = 4 * g + i
                P.op("pe", lambda e: e.transpose(ps[0:8, i * 128:(i + 1) * 128], SEL[:, t, :], ident[:]), reads=["mSEL", "ident"], writes=[pk])
            P.op("act", lambda e: e.activation(out=BIAST[:, g * 512:(g + 1) * 512], in_=ps[0:8, :], func=AF.Copy), reads=[pk], writes=["mBIAST"])
        for qc in range(NCH):
            sl = slice(qc * 512, (qc + 1) * 512)
            pso, pko = _nps(k)
            psl, pkl = _nps(k)
            njt = 4 * qc + 4
            for jt in range(njt):
                while k.psi in (int(pko[2:]), int(pkl[2:])):
                    k.psi = (k.psi + 1) % 8
                pss, pks = _nps(k)
                P.op("pe", lambda e: e.matmul(pss[:], KB[:, jt * 128:(jt + 1) * 128], QB[:, sl], start=True, stop=False), reads=["mKB", "mQB"], writes=[pks])
                P.op("pe", lambda e: e.matmul(pss[:], SEL8b[:, jt // 2, :], BIAST[:, sl], start=False, stop=True), reads=["sel8b", "mBIAST"], writes=[pks])
                pt, ptk = PT[pi % 2], f"mPT{pi % 2}"
                pi += 1
                P.op("act", lambda e: e.activation(out=pt[:], in_=pss[:], func=AF.Exp, scale=SCALE), reads=[pks], writes=[ptk])
                if jt >= 4 * qc:
                    P.op("pool", lambda e: e.tensor_tensor(out=pt[:], in0=pt[:], in1=CAUS[:, jt - 4 * qc, :], op=ALU.mult), reads=[ptk, "caus4"], writes=[ptk])
                P.op("pe", lambda e: e.matmul(pso[:], VTM[:, jt, :], pt[:], start=(jt == 0), stop=(jt == njt - 1)), reads=["mVTM", ptk], writes=[pko])
                P.op("pe", lambda e: e.matmul(psl[:], onesb[:], pt[:], start=(jt == 0), stop=(jt == njt - 1)), reads=["onesb", ptk], writes=[pkl])
            P.op("dve", lambda e: e.reciprocal(out=RL[:], in_=psl[:]), reads=[pkl], writes=["mRL"])
            P.op("dve", lambda e: e.tensor_tensor(out=OB[:, sl], in0=pso[:], in1=RL[:], op=ALU.mult), reads=[pko, "mRL"], writes=["mOB"])
        for m in range(KC):
            for n in range(NCH):
                sl = slice(n * 512, (n + 1) * 512)
                ps, pk = _nps(k)
                P.op("pe", lambda e: e.matmul(ps[:], WO[:, m * 128:(m + 1) * 128], OB[:, sl], start=True, stop=True), reads=["mWO", "mOB"], writes=[pk])
                P.op("dve", lambda e: e.tensor_tensor(out=k.XT[:, m, sl], in0=ps[:], in1=k.XT[:, m, sl], op=ALU.add), reads=[pk, ("XT", n)], writes=[("XT", n)])
    P.pop()
    P.push()
    k.layer_norm(2, k.ln_tmp())
    P.pop()


def make_in_maps(inputs, n_cores=8):
    c = _consts()
    shared = {}
    for name in IN_SHAPES:
        if name == "x":
            continue
        a = np.ascontiguousarray(np.asarray(inputs[name], dtype=np.float32))
        shared[name] = a.reshape(IN_SHAPES[name])
    for name, v in c.items():
        shared["c_" + name] = np.ascontiguousarray(v)
    x = np.asarray(inputs["x"], dtype=np.float32)
    maps = []
    for b in range(n_cores):
        m = dict(shared)
        m["x"] = np.ascontiguousarray(x[b])
        maps.append(m)
    return maps


def kernel(**inputs):
    nc = build()
    maps = make_in_maps(inputs)
    res = run_bass_kernel_spmd(nc, maps, core_ids=list(range(8)))
    return np.stack([np.asarray(r["out"], dtype=np.float32) for r in res.results], axis=0)
```

```python
from contextlib import ExitStack
import numpy as np
import concourse.bass as bass
import concourse.mybir as mybir
from concourse.bass_utils import run_bass_kernel_spmd

F32 = mybir.dt.float32
BF16 = mybir.dt.bfloat16
ALU = mybir.AluOpType
AF = mybir.ActivationFunctionType
AX = mybir.AxisListType

S = 2048
D = 1024
NT = 16
NCH = 4
KC = 8
FH = 3584
NF = 28
NE = 8
ALPHA = 4.0 ** 0.25
LN_EPS = 1e-5
DN_EPS = 1e-6
SEM_ROLL = 20000


class Prog:
    ENG = ("pe", "act", "dve", "pool", "sp")

    def __init__(self, nc):
        self.nc = nc
        self.es = ExitStack()
        self.scopes = [self.es]
        self.e = {"pe": nc.tensor, "act": nc.scalar, "dve": nc.vector, "pool": nc.gpsimd, "sp": nc.sync}
        self.sem = {}
        self.cnt = {}
        self.nsem = 0
        for k in self.ENG:
            self._newsem(k)
        self.waited = {k: {} for k in self.ENG}
        self.last_w = {}
        self.readers = {}
        self.dsem = {}
        self.group_reads = []
        self.n_inst = 0

    def _newsem(self, k):
        name = f"s_{k}_{self.nsem}"
        s = self.es.enter_context(self.nc.semaphore(name))
        self.nsem += 1
        self.sem[k] = (name, s)
        self.cnt[k] = 0

    def sb(self, name, shape, dt=F32):
        return self.scopes[-1].enter_context(self.nc.sbuf_tensor(name, list(shape), dt))

    def push(self):
        self.scopes.append(ExitStack())

    def pop(self):
        self.barrier()
        self.scopes.pop().close()

    def ps(self, name, shape, dt=F32):
        return self.es.enter_context(self.nc.psum_tensor(name, list(shape), dt))

    def _deps(self, engine, reads, writes):
        need = {}

        def add(rec):
            name, s, val, e = rec
            if e == engine and engine == "pe":
                return
            if name not in need or need[name][1] < val:
                need[name] = (s, val)
        for r in list(reads) + self.group_reads:
            if r in self.last_w:
                add(self.last_w[r])
        for w in writes:
            if w in self.last_w:
                add(self.last_w[w])
            for rec in self.readers.get(w, ()):
                add(rec)
        for name, (s, val) in need.items():
            if self.waited[engine].get(name, 0) >= val:
                continue
            self.e[engine].wait_ge(s, val)
            self.waited[engine][name] = val

    def _record(self, rec, reads, writes):
        for r in reads:
            self.readers.setdefault(r, []).append(rec)
        for w in writes:
            self.last_w[w] = rec
            self.readers[w] = []

    def op(self, engine, fn, reads=(), writes=()):
        self._deps(engine, reads, writes)
        inst = fn(self.e[engine])
        name, s = self.sem[engine]
        self.cnt[engine] += 1
        val = self.cnt[engine]
        inst.then_inc(s, 1)
        self.n_inst += 1
        self._record((name, s, val, engine), reads, writes)
        if val >= SEM_ROLL:
            self._newsem(engine)
        return inst

    GROUPS = ("c0", "dnc", "mbc", "wr")

    def dma(self, queue, out, in_, reads=(), writes=(), key=None, **kw):
        if key in self.GROUPS:
            gk = ("grp", key)
            writes = list(writes) + [gk]
            if gk not in self.group_reads:
                self.group_reads.append(gk)
        self._deps(queue, reads, writes)
        if key not in self.dsem:
            name = f"d_{self.nsem}"
            s = self.es.enter_context(self.nc.semaphore(name))
            self.nsem += 1
            self.dsem[key] = [name, s, 0]
        ent = self.dsem[key]
        inst = self.e[queue].dma_start(out=out, in_=in_, **kw)
        ent[2] += 16
        inst.then_inc(ent[1], 16)
        self.n_inst += 1
        self._record((ent[0], ent[1], ent[2], "dma"), reads, writes)
        return inst

    def barrier(self):
        recs = []
        for k in self.ENG:
            name, s = self.sem[k]
            if self.cnt[k] > 0:
                recs.append((name, s, self.cnt[k], k))
        for key, ent in self.dsem.items():
            if ent[2] > 0:
                recs.append((ent[0], ent[1], ent[2], "dma"))
        for k in self.ENG:
            for name, s, val, src in recs:
                if self.waited[k].get(name, 0) >= val:
                    continue
                if src == k and k == "pe":
                    continue
                self.e[k].wait_ge(s, val)
                self.waited[k][name] = val

    def close(self):
        self.barrier()
        self.es.close()


def _consts():
    c = {}
    c["ident"] = np.eye(128, dtype=np.float32)
    c["ones"] = np.ones((128, 128), dtype=np.float32)
    half = 64
    inv_freq = (10000.0 ** (-(np.arange(half, dtype=np.float32) / np.float32(half)))).astype(np.float32)
    ang = (np.arange(S, dtype=np.float32)[:, None] * inv_freq[None, :]).astype(np.float32)
    cos = np.cos(ang.astype(np.float64)).astype(np.float32).T
    sin = np.sin(ang.astype(np.float64)).astype(np.float32).T
    c["ropec"] = np.concatenate([cos, cos], axis=0)
    c["ropes"] = np.concatenate([-sin, sin], axis=0)
    sel = np.zeros((8, 8, 128), dtype=np.float32)
    for e in range(8):
        sel[e, e, :] = 1.0
    c["sel8"] = sel.transpose(1, 0, 2).copy()
    r = np.arange(128)[:, None]
    q = np.arange(128)[None, :]
    c["tri_le"] = (r <= q).astype(np.float32)
    c["tri_gt"] = (r > q).astype(np.float32)
    c["nmask_bd"] = -((r > q) & ((r // 64) == (q // 64))).astype(np.float32)
    c["mask_off"] = ((r >= 64) & (q < 64)).astype(np.float32)
    c["perm"] = (r == ((q + 64) % 128)).astype(np.float32)
    pb = np.zeros((128, NT, 8), dtype=np.float32)
    p01 = np.zeros((128, NT, 8), dtype=np.float32)
    own = np.zeros((128, NT, 8), dtype=np.float32)
    for t in range(NT):
        qb = t // 2
        own[:, t, qb] = 1.0
        for n in range(8):
            if n < qb:
                p01[:, t, n] = 1.0
            else:
                pb[:, t, n] = -1.0e30
    c["pastb"] = pb
    c["past01"] = p01
    c["own01"] = own
    caus = np.ones((128, 4, 512), dtype=np.float32)
    j = np.arange(128)[:, None]
    ii = np.arange(512)[None, :]
    for pos in range(4):
        same = (pos // 2) == (ii // 256)
        caus[:, pos, :] = np.where(same & ((128 * pos + j) > ii), 0.0, 1.0)
    c["caus4"] = caus
    return c


CONST_SHAPES = {"ident": [128, 128], "ones": [128, 128], "ropec": [128, S], "ropes": [128, S],
                "sel8": [8, 8, 128], "tri_le": [128, 128], "tri_gt": [128, 128], "nmask_bd": [128, 128],
                "mask_off": [128, 128], "perm": [128, 128], "pastb": [128, NT, 8], "past01": [128, NT, 8],
                "own01": [128, NT, 8], "caus4": [128, 4, 512]}

IN_SHAPES = {
    "x": [S, D], "a_w_in": [D, 4112], "a_conv_w": [4, 3072], "a_log_decay": [1, 8], "a_dt_bias": [1, 8],
    "a_norm_w": [1, 128], "a_w_out": [D, D], "b_w_kv": [D, 2 * D], "b_w_q": [D, D], "b_w_o": [D, D],
    "ffn_w1": [D, FH], "ffn_w3": [D, FH], "ffn_w2": [FH, D], "moe_router": [D, 8],
    "moe_w1": [8, D, FH], "moe_w3": [8, D, FH], "moe_w2": [8, FH, D], "ln_g": [4, D], "ln_b": [4, D],
}


class K:
    def __init__(self, stages, dbg=()):
        self.stages = stages
        nc = bass.Bass("TRN2", target_bir_lowering=False)
        self.nc = nc
        self.P = Prog(nc)
        P = self.P
        self.din = {k: nc.dram_tensor(k, v, F32, kind="ExternalInput").ap() for k, v in IN_SHAPES.items()}
        self.dc = {k: nc.dram_tensor("c_" + k, v, F32, kind="ExternalInput").ap() for k, v in CONST_SHAPES.items()}
        self.out = nc.dram_tensor("out", [S, D], F32, kind="ExternalOutput").ap()
        self.dbg = {}
        for name, shape in dbg:
            self.dbg[name] = nc.dram_tensor("dbg_" + name, shape, F32, kind="ExternalOutput").ap()
        self.uid = 0
        self.XT = P.sb("XT", [128, KC, S])
        self.XB = P.sb("XB", [128, KC, S], BF16)
        self.ident = P.sb("ident", [128, 128])
        self.identb = P.sb("identb", [128, 128], BF16)
        self.ones = P.sb("ones", [128, 128])
        self.onesb = P.sb("onesb", [128, 128], BF16)
        self.lng = P.sb("lng", [128, 4, KC])
        self.lnb = P.sb("lnb", [128, 4, KC])
        self.PS = [P.ps(f"PS{i}", [128, 512]) for i in range(8)]
        self.psk = [f"PS{i}" for i in range(8)]

        P.dma("sp", self.ident[:], self.dc["ident"], writes=["ident"], key="c0")
        P.dma("sp", self.ones[:], self.dc["ones"], writes=["ones"], key="c0")
        lnraw = P.sb("lnraw", [64, 128])
        P.dma("sp", lnraw[0:32, :], self.din["ln_g"].rearrange("l (c p) -> (l c) p", p=128), writes=["lnraw"], key="c0")
        P.dma("sp", lnraw[32:64, :], self.din["ln_b"].rearrange("l (c p) -> (l c) p", p=128), writes=["lnraw"], key="c0")
        P.op("pe", lambda e: e.transpose(self.PS[0][:, 0:64], lnraw[:], self.ident[0:64, 0:64]), reads=["lnraw", "ident"], writes=["PS0"])
        P.op("dve", lambda e: e.tensor_copy(out=self.lng[:], in_=self.PS[0][:, 0:32].rearrange("p (l c) -> p l c", l=4)), reads=["PS0"], writes=["lng"])
        P.op("dve", lambda e: e.tensor_copy(out=self.lnb[:], in_=self.PS[0][:, 32:64].rearrange("p (l c) -> p l c", l=4)), reads=["PS0"], writes=["lnb"])
        P.op("dve", lambda e: e.tensor_copy(out=self.identb[:], in_=self.ident[:]), reads=["ident"], writes=["identb"])
        P.op("dve", lambda e: e.tensor_copy(out=self.onesb[:], in_=self.ones[:]), reads=["ones"], writes=["onesb"])

    def u(self, s):
        self.uid += 1
        return f"{s}{self.uid}"

    def load_x(self):
        P = self.P
        xin = [P.sb(f"xin{i}", [128, D]) for i in range(2)]
        for t in range(NT):
            b = xin[t % 2]
            bk = f"xin{t % 2}"
            P.dma("sp", b[:], self.din["x"][t * 128:(t + 1) * 128, :], writes=[bk], key=bk)
            for hlf in range(2):
                ps = self.PS[2 * (t % 2) + hlf]
                pk = self.psk[2 * (t % 2) + hlf]
                for c4 in range(4):
                    c = hlf * 4 + c4
                    P.op("pe", lambda e: e.transpose(ps[:, c4 * 128:(c4 + 1) * 128], b[:, c * 128:(c + 1) * 128], self.ident[:]),
                         reads=[bk, "ident"], writes=[pk])
                src = ps[:].rearrange("p (c n) -> p c n", c=4)
                P.op("act", lambda e: e.activation(out=self.XT[:, hlf * 4:(hlf + 1) * 4, t * 128:(t + 1) * 128], in_=src, func=AF.Copy),
                     reads=[pk], writes=[("XT", t // 4)])
                P.op("dve", lambda e: e.tensor_copy(out=self.XB[:, hlf * 4:(hlf + 1) * 4, t * 128:(t + 1) * 128],
                                                    in_=self.XT[:, hlf * 4:(hlf + 1) * 4, t * 128:(t + 1) * 128]),
                     reads=[("XT", t // 4)], writes=[("XB", t // 4)])

    def store_x(self):
        P = self.P
        xo = [P.sb(f"xo{i}", [128, D]) for i in range(2)]
        for t in range(NT):
            b = xo[t % 2]
            bk = f"xo{t % 2}"
            for hlf in range(2):
                ps = self.PS[2 * (t % 2) + hlf]
                pk = self.psk[2 * (t % 2) + hlf]
                for c4 in range(4):
                    c = hlf * 4 + c4
                    P.op("pe", lambda e: e.transpose(ps[:, c4 * 128:(c4 + 1) * 128], self.XT[:, c, t * 128:(t + 1) * 128], self.ident[:]),
                         reads=[("XT", t // 4), "ident"], writes=[pk])
                if hlf == 0:
                    P.op("act", lambda e: e.activation(out=b[:, 0:512], in_=ps[:], func=AF.Copy), reads=[pk], writes=[bk])
                else:
                    P.op("dve", lambda e: e.tensor_copy(out=b[:, 512:1024], in_=ps[:]), reads=[pk], writes=[bk])
            P.dma("sp", self.out[t * 128:(t + 1) * 128, :], b[:], reads=[bk], writes=["out"], key=bk)

    def dbg_store_xt(self, name):
        P = self.P
        for c in range(KC):
            P.dma("sp", self.dbg[name][c], self.XT[:, c, :], reads=[("XT", n) for n in range(NCH)], writes=["dbg"], key="dbg")

    def layer_norm(self, idx, tmp):
        P = self.P
        SQ, R1, R2, MEAN, RSTD = tmp["sq"], tmp["r1"], tmp["r2"], tmp["mean"], tmp["rstd"]
        for n in range(NCH):
            sl = slice(n * 512, (n + 1) * 512)
            xk = ("XT", n)
            Yv = self.XT[:, :, sl]
            P.op("act", lambda e: e.activation(out=SQ[:], in_=Yv, func=AF.Square), reads=[xk], writes=["ln_sq"])
            P.op("dve", lambda e: e.tensor_reduce(out=R1[:], in_=Yv.rearrange("p c n -> p n c"), axis=AX.X, op=ALU.add),
                 reads=[xk], writes=["ln_r1"])
            P.op("dve", lambda e: e.tensor_reduce(out=R2[:], in_=SQ[:].rearrange("p c n -> p n c"), axis=AX.X, op=ALU.add),
                 reads=["ln_sq"], writes=["ln_r2"])
            ps1, pk1 = self.PS[6], self.psk[6]
            ps2, pk2 = self.PS[7], self.psk[7]
            P.op("pe", lambda e: e.matmul(ps1[:], self.ones[:], R1[:], start=True, stop=True), reads=["ones", "ln_r1"], writes=[pk1])
            P.op("pe", lambda e: e.matmul(ps2[:], self.ones[:], R2[:], start=True, stop=True), reads=["ones", "ln_r2"], writes=[pk2])
            P.op("act", lambda e: e.activation(out=MEAN[:], in_=ps1[:], func=AF.Copy, scale=1.0 / D), reads=[pk1], writes=["ln_mean"])
            P.op("dve", lambda e: e.tensor_tensor(out=R1[:], in0=MEAN[:], in1=MEAN[:], op=ALU.mult), reads=["ln_mean"], writes=["ln_r1"])
            P.op("dve", lambda e: e.scalar_tensor_tensor(out=R2[:], in0=ps2[:], scalar=1.0 / D, in1=R1[:], op0=ALU.mult, op1=ALU.subtract),
                 reads=[pk2, "ln_r1"], writes=["ln_r2"])
            P.op("dve", lambda e: e.tensor_scalar(out=R2[:], in0=R2[:], scalar1=LN_EPS, scalar2=None, op0=ALU.add), reads=["ln_r2"], writes=["ln_r2"])
            P.op("act", lambda e: e.activation(out=R1[:], in_=R2[:], func=AF.Sqrt), reads=["ln_r2"], writes=["ln_r1"])
            P.op("dve", lambda e: e.reciprocal(out=RSTD[:], in_=R1[:]), reads=["ln_r1"], writes=["ln_rstd"])
            mb = MEAN[:].unsqueeze(1).broadcast_to([128, KC, 512])
            rb = RSTD[:].unsqueeze(1).broadcast_to([128, KC, 512])
            P.op("dve", lambda e: e.tensor_tensor(out=SQ[:], in0=Yv, in1=mb, op=ALU.subtract), reads=[xk, "ln_mean"], writes=["ln_sq"])
            P.op("pool", lambda e: e.tensor_tensor(out=SQ[:], in0=SQ[:], in1=rb, op=ALU.mult), reads=["ln_sq", "ln_rstd"], writes=["ln_sq"])
            for c in range(KC):
                P.op("act", lambda e: e.activation(out=self.XT[:, c, sl], in_=SQ[:, c, :], func=AF.Identity,
                                                   scale=self.lng[:, idx, c:c + 1], bias=self.lnb[:, idx, c:c + 1]),
                     reads=["ln_sq", "lng", "lnb"], writes=[xk])
                P.op("dve", lambda e: e.tensor_scalar(out=self.XB[:, c, sl], in0=SQ[:, c, :], scalar1=self.lng[:, idx, c:c + 1],
                                                      scalar2=self.lnb[:, idx, c:c + 1], op0=ALU.mult, op1=ALU.add),
                     reads=["ln_sq", "lng", "lnb"], writes=[("XB", n)])

    def ln_tmp(self):
        P = self.P
        return {"sq": P.sb(self.u("ln_sq"), [128, KC, 512]), "r1": P.sb(self.u("ln_r1"), [128, 512]),
                "r2": P.sb(self.u("ln_r2"), [128, 512]), "mean": P.sb(self.u("ln_mean"), [128, 512]),
                "rstd": P.sb(self.u("ln_rstd"), [128, 512])}

    def scale_xt(self):
        P = self.P
        for n in range(NCH):
            sl = slice(n * 512, (n + 1) * 512)
            eng = "act" if n % 2 == 0 else "dve"
            if eng == "act":
                P.op("act", lambda e: e.activation(out=self.XT[:, :, sl], in_=self.XT[:, :, sl], func=AF.Copy, scale=ALPHA),
                     reads=[("XT", n)], writes=[("XT", n)])
            else:
                P.op("dve", lambda e: e.tensor_scalar(out=self.XT[:, :, sl], in0=self.XT[:, :, sl], scalar1=ALPHA, scalar2=None, op0=ALU.mult),
                     reads=[("XT", n)], writes=[("XT", n)])

    def ffn(self, experts, gb=None):
        P = self.P
        G = 4
        NB = 2
        W1 = [P.sb(self.u("w1g"), [128, KC, G * 128], BF16) for _ in range(NB)]
        W3 = [P.sb(self.u("w3g"), [128, KC, G * 128], BF16) for _ in range(NB)]
        W2 = [P.sb(self.u("w2g"), [128, G, D], BF16) for _ in range(NB)]
        H = [P.sb(self.u("hg"), [128, G, 512], BF16) for _ in range(2)]
        SA = [P.sb(self.u("sa"), [128, 512], BF16) for _ in range(2)]
        HT = [P.sb(self.u("ht"), [128, 512], BF16) for _ in range(2)]
        base = self.u("ffn")
        gi = 0
        si = 0
        hi = 0
        groups = [(ei, ex, fg) for ei, ex in enumerate(experts) for fg in range(NF // G)]

        def issue_load(k):
            ei, ex, fg = groups[k]
            w1, w3, w2, _ = ex
            slot = k % NB
            f0 = fg * G * 128
            P.dma("pool", W1[slot][:], w1[:, f0:f0 + G * 128].rearrange("(c p) f -> p c f", p=128),
                  writes=[(base, "w1", slot)], key=(base, "w1", slot))
            P.dma("pool", W3[slot][:], w3[:, f0:f0 + G * 128].rearrange("(c p) f -> p c f", p=128),
                  writes=[(base, "w3", slot)], key=(base, "w3", slot))
            P.dma("pool", W2[slot][:], w2[f0:f0 + G * 128, :].rearrange("(j p) m -> p j m", p=128),
                  writes=[(base, "w2", slot)], key=(base, "w2", slot))

        issue_load(0)
        st = {"gt": None, "gk": None, "si": 0, "gi": 0}
        steps = [(k, n) for k in range(len(groups)) for n in range(NCH)]

        def phase1(s):
            k, n = steps[s]
            ei, ex, fg = groups[k]
            slot = k % NB
            if n == 0:
                if ex[3] is None:
                    st["gt"] = None
                elif fg == 0:
                    st["gt"], st["gk"] = gb(ex[3])
            gt, gk = st["gt"], st["gk"]
            sl = slice(n * 512, (n + 1) * 512)
            hb = H[s % 2]
            hk = (base, "h", s % 2)
            for j in range(G):
                si = st["si"]
                pa, pak = self.PS[2 * (si % 2)], self.psk[2 * (si % 2)]
                pb, pbk = self.PS[2 * (si % 2) + 1], self.psk[2 * (si % 2) + 1]
                sa, sak = SA[si % 2], (base, "sa", si % 2)
                for c in range(KC):
                    P.op("pe", lambda e: e.matmul(pa[:], W1[slot][:, c, j * 128:(j + 1) * 128], self.XB[:, c, sl], start=(c == 0), stop=(c == KC - 1)),
                         reads=[(base, "w1", slot), ("XB", n)], writes=[pak])
                for c in range(KC):
                    P.op("pe", lambda e: e.matmul(pb[:], W3[slot][:, c, j * 128:(j + 1) * 128], self.XB[:, c, sl], start=(c == 0), stop=(c == KC - 1)),
                         reads=[(base, "w3", slot), ("XB", n)], writes=[pbk])
                P.op("act", lambda e: e.activation(out=sa[:], in_=pa[:], func=AF.Silu), reads=[pak], writes=[sak])
                if gt is None:
                    P.op("dve", lambda e: e.tensor_tensor(out=hb[:, j, :], in0=pb[:], in1=sa[:], op=ALU.mult),
                         reads=[pbk, sak], writes=[hk])
                else:
                    ht, htk = HT[si % 2], (base, "ht", si % 2)
                    P.op("pool", lambda e: e.tensor_tensor(out=ht[:], in0=sa[:], in1=gt[:, sl], op=ALU.mult),
                         reads=[sak, gk], writes=[htk])
                    P.op("dve", lambda e: e.tensor_tensor(out=hb[:, j, :], in0=pb[:], in1=ht[:], op=ALU.mult),
                         reads=[pbk, htk], writes=[hk])
                st["si"] += 1

        def phase2(s):
            k, n = steps[s]
            slot = k % NB
            sl = slice(n * 512, (n + 1) * 512)
            hb = H[s % 2]
            hk = (base, "h", s % 2)
            for m in range(KC):
                gi = st["gi"]
                py, pyk = self.PS[4 + gi % 4], self.psk[4 + gi % 4]
                st["gi"] += 1
                for j in range(G):
                    P.op("pe", lambda e: e.matmul(py[:], W2[slot][:, j, m * 128:(m + 1) * 128], hb[:, j, :], start=(j == 0), stop=(j == G - 1)),
                         reads=[(base, "w2", slot), hk], writes=[pyk])
                P.op("dve", lambda e: e.tensor_tensor(out=self.XT[:, m, sl], in0=py[:], in1=self.XT[:, m, sl], op=ALU.add),
                     reads=[pyk, ("XT", n)], writes=[("XT", n)])

        if len(groups) > 1:
            issue_load(1)
        phase1(0)
        for s in range(len(steps)):
            if s + 1 < len(steps):
                phase1(s + 1)
            phase2(s)
            kk_, nn_ = steps[s]
            if nn_ == NCH - 1 and kk_ + 2 < len(groups):
                issue_load(kk_ + 2)

    def router(self):
        P = self.P
        WR = P.sb("wr", [128, KC, 8])
        LG = P.sb("r_lg", [128, NT, 8])
        M8 = P.sb("r_m8", [128, NT, 8])
        EX = P.sb("r_ex", [128, NT, 8])
        SELM = P.sb("r_sel", [128, NT, 8])
        DEN = P.sb("r_den", [128, NT])
        GT = P.sb("r_gt", [8, S])
        SEL8 = P.sb("r_sel8", [8, 8, 128])
        P.dma("sp", WR[:], self.din["moe_router"].rearrange("(c p) e -> p c e", p=128), writes=["wr"], key="wr")
        P.dma("sp", SEL8[:], self.dc["sel8"], writes=["sel8"], key="wr")
        ps, pk = self.PS[0], self.psk[0]
        for t in range(NT):
            for c in range(KC):
                P.op("pe", lambda e: e.matmul(ps[:, t * 8:(t + 1) * 8], self.XT[:, c, t * 128:(t + 1) * 128], WR[:, c, :], start=(c == 0), stop=(c == KC - 1)),
                     reads=[("XT", t // 4), "wr"], writes=[pk])
        P.op("dve", lambda e: e.tensor_copy(out=LG[:], in_=ps[:, 0:NT * 8].rearrange("p (t e) -> p t e", e=8)), reads=[pk], writes=["r_lg"])
        for t in range(NT):
            P.op("dve", lambda e: e.max(out=M8[:, t, :], in_=LG[:, t, :]), reads=["r_lg"], writes=["r_m8"])
        m1 = M8[:, :, 0:1].broadcast_to([128, NT, 8])
        m2 = M8[:, :, 1:2].broadcast_to([128, NT, 8])
        P.op("dve", lambda e: e.tensor_tensor(out=EX[:], in0=LG[:], in1=m1, op=ALU.subtract), reads=["r_lg", "r_m8"], writes=["r_ex"])
        P.op("act", lambda e: e.activation(out=EX[:], in_=EX[:], func=AF.Exp), reads=["r_ex"], writes=["r_ex"])
        P.op("dve", lambda e: e.tensor_tensor(out=SELM[:], in0=LG[:], in1=m2, op=ALU.is_ge), reads=["r_lg", "r_m8"], writes=["r_sel"])
        P.op("dve", lambda e: e.tensor_tensor(out=EX[:], in0=EX[:], in1=SELM[:], op=ALU.mult), reads=["r_ex", "r_sel"], writes=["r_ex"])
        P.op("dve", lambda e: e.tensor_reduce(out=DEN[:], in_=EX[:], axis=AX.X, op=ALU.add), reads=["r_ex"], writes=["r_den"])
        P.op("dve", lambda e: e.reciprocal(out=DEN[:], in_=DEN[:]), reads=["r_den"], writes=["r_den"])
        P.op("dve", lambda e: e.tensor_tensor(out=EX[:], in0=EX[:], in1=DEN[:].unsqueeze(2).broadcast_to([128, NT, 8]), op=ALU.mult),
             reads=["r_ex", "r_den"], writes=["r_ex"])
        for q in range(4):
            pt, ptk = self.PS[1 + q % 2], self.psk[1 + q % 2]
            for tt in range(4):
                t = q * 4 + tt
                P.op("pe", lambda e: e.transpose(pt[0:8, tt * 128:(tt + 1) * 128], EX[:, t, :], self.ident[:]), reads=["r_ex", "ident"], writes=[ptk])
            P.op("act", lambda e: e.activation(out=GT[:, q * 512:(q + 1) * 512], in_=pt[0:8, :], func=AF.Copy), reads=[ptk], writes=["r_gt"])
        self.GT = GT
        self.SEL8 = SEL8
        self.GB = [P.sb(f"gb{i}", [128, S], BF16) for i in range(2)]
        if "g" in self.dbg:
            P.dma("sp", self.dbg["g"], EX[:], reads=["r_ex"], writes=["dbg"], key="dbg")

    def gate_bcast(self, eidx):
        P = self.P
        gbt = self.GB[eidx % 2]
        gk = ("gb", eidx % 2)
        for n in range(NCH):
            ps, pk = self.PS[6 + n % 2], self.psk[6 + n % 2]
            P.op("pe", lambda e: e.matmul(ps[:], self.SEL8[:, eidx, :], self.GT[:, n * 512:(n + 1) * 512], start=True, stop=True),
                 reads=["sel8", "r_gt"], writes=[pk])
            P.op("act", lambda e: e.activation(out=gbt[:, n * 512:(n + 1) * 512], in_=ps[:], func=AF.Copy), reads=[pk], writes=[gk])
        return gbt, gk


def build(stages=("load", "dn", "ffn0", "moba", "moe", "store"), dbg=()):
    k = K(stages, dbg)
    P = k.P
    din = k.din
    if "load" in stages:
        P.push()
        k.load_x()
        P.pop()
    if "dn" in stages:
        from_dn(k)
    if "ffn0" in stages:
        P.push()
        k.scale_xt()
        k.ffn([(din["ffn_w1"], din["ffn_w3"], din["ffn_w2"], None)])
        P.pop()
        P.push()
        k.layer_norm(1, k.ln_tmp())
        P.pop()
    if "dbg_x1" in k.dbg:
        k.dbg_store_xt("x1")
    if "moba" in stages:
        from_moba(k)
    if "moe" in stages:
        P.push()
        k.router()
        k.scale_xt()
        k.ffn([(din["moe_w1"][e], din["moe_w3"][e], din["moe_w2"][e], e) for e in range(NE)], gb=k.gate_bcast)
        P.pop()
        P.push()
        k.layer_norm(3, k.ln_tmp())
        P.pop()
    if "store" in stages:
        P.push()
        k.store_x()
        P.pop()
    P.close()
    return k.nc


def _nps(k):
    i = getattr(k, "psi", 0)
    k.psi = (i + 1) % 8
    return k.PS[i], k.psk[i]


DN_STOP = None


def from_dn(k):
    P = k.P
    din = k.din
    dc = k.dc
    TB = 4
    W = TB * 128
    QSCALE = 128.0 ** -0.5
    P.push()

    def cload(name, src, shape, dt=F32, q="sp"):
        t = P.sb(name, shape, dt)
        P.dma(q, t[:], src, writes=[name], key="dnc")
        return t

    TRI_LE = cload("tri_le", dc["tri_le"], [128, 128])
    TRI_GT = cload("tri_gt", dc["tri_gt"], [128, 128])
    NMBD = cload("nmask_bd", dc["nmask_bd"], [128, 128])
    MOFF = cload("mask_off", dc["mask_off"], [128, 128])
    ident, ones = k.ident, k.ones

    def b3(T):
        return T[:].unsqueeze(1).broadcast_to([128, TB, 128])

    def v3(ap):
        return ap.rearrange("p (t n) -> p t n", t=TB)

    cwraw = P.sb("cwraw", [96, 128])
    P.dma("sp", cwraw[:], din["a_conv_w"].rearrange("j (c p) -> (j c) p", p=128), writes=["cwraw"], key="dnc")
    CW = P.sb("CW", [128, 96])
    ps, pk = _nps(k)
    P.op("pe", lambda e: e.transpose(ps[:, 0:96], cwraw[:], ident[0:96, 0:96]), reads=["cwraw", "ident"], writes=[pk])
    P.op("dve", lambda e: e.tensor_copy(out=CW[:], in_=ps[:, 0:96]), reads=[pk], writes=["CW"])
    NW = P.sb("NW", [128, 1])
    P.dma("sp", NW[:], din["a_norm_w"].rearrange("o p -> p o"), writes=["NW"], key="dnc")

    WAB = P.sb("WAB", [128, KC, 16])
    P.dma("sp", WAB[:], din["a_w_in"][:, 4096:4112].rearrange("(c p) f -> p c f", p=128), writes=["WAB"], key="dnc")
    RW = P.sb("RW", [1, 16])
    P.dma("sp", RW[0:1, 0:8], din["a_log_decay"], writes=["RW"], key="dnc")
    P.dma("sp", RW[0:1, 8:16], din["a_dt_bias"], writes=["RW"], key="dnc")
    AB = P.sb("AB", [128, NT, 16])
    BC = P.sb("BC", [128, 16])
    ps, pk = _nps(k)
    for t in range(NT):
        for c in range(KC):
            P.op("pe", lambda e: e.matmul(ps[:, t * 16:(t + 1) * 16], k.XT[:, c, t * 128:(t + 1) * 128], WAB[:, c, :], start=(c == 0), stop=(c == KC - 1)),
                 reads=[("XT", t // 4), "WAB"], writes=[pk])
    P.op("dve", lambda e: e.tensor_copy(out=AB[:], in_=ps[:, 0:NT * 16].rearrange("p (t f) -> p t f", f=16)), reads=[pk], writes=["AB"])
    ps, pk = _nps(k)
    P.op("pe", lambda e: e.matmul(ps[:, 0:16], ones[0:1, :], RW[0:1, :], start=True, stop=True), reads=["ones", "RW"], writes=[pk])
    P.op("dve", lambda e: e.tensor_copy(out=BC[:], in_=ps[:, 0:16]), reads=[pk], writes=["BC"])

    def tm(name):
        return P.sb(name, [128, NT, 8])
    G, BETA, EKD, BEG, EGL, TM1, TM2, GCS = (tm(n) for n in ("G", "BETA", "EKD", "BEG", "EGL", "TM1", "TM2", "GCS"))
    EA = P.sb("EA", [128, 8])
    dtb = BC[:, 8:16].unsqueeze(1).broadcast_to([128, NT, 8])
    P.op("dve", lambda e: e.tensor_tensor(out=TM1[:], in0=AB[:, :, 0:8], in1=dtb, op=ALU.add), reads=["AB", "BC"], writes=["TM1"])
    P.op("act", lambda e: e.activation(out=TM1[:], in_=TM1[:], func=AF.Exp), reads=["TM1"], writes=["TM1"])
    P.op("dve", lambda e: e.tensor_scalar(out=TM1[:], in0=TM1[:], scalar1=1.0, scalar2=None, op0=ALU.add), reads=["TM1"], writes=["TM1"])
    P.op("act", lambda e: e.activation(out=TM1[:], in_=TM1[:], func=AF.Ln), reads=["TM1"], writes=["TM1"])
    P.op("act", lambda e: e.activation(out=EA[:], in_=BC[:, 0:8], func=AF.Exp), reads=["BC"], writes=["EA"])
    P.op("dve", lambda e: e.scalar_tensor_tensor(out=G[:], in0=TM1[:], scalar=-1.0, in1=EA[:].unsqueeze(1).broadcast_to([128, NT, 8]),
                                                 op0=ALU.mult, op1=ALU.mult), reads=["TM1", "EA"], writes=["G"])
    P.op("act", lambda e: e.activation(out=TM2[:], in_=AB[:, :, 8:16], func=AF.Exp, scale=-1.0), reads=["AB"], writes=["TM2"])
    P.op("dve", lambda e: e.tensor_scalar(out=TM2[:], in0=TM2[:], scalar1=1.0, scalar2=None, op0=ALU.add), reads=["TM2"], writes=["TM2"])
    P.op("dve", lambda e: e.reciprocal(out=BETA[:], in_=TM2[:]), reads=["TM2"], writes=["BETA"])
    Gf = G[:].rearrange("p t h -> p (t h)")
    psc, pkc = _nps(k)
    psl, pkl = _nps(k)
    P.op("pe", lambda e: e.matmul(psc[:, 0:128], TRI_LE[:], Gf, start=True, stop=True), reads=["tri_le", "G"], writes=[pkc])
    P.op("pe", lambda e: e.matmul(psl[:, 0:128], ones[:], Gf, start=True, stop=True), reads=["ones", "G"], writes=[pkl])
    f3 = lambda ap: ap.rearrange("p (t h) -> p t h", h=8)
    P.op("act", lambda e: e.activation(out=GCS[:], in_=f3(psc[:, 0:128]), func=AF.Copy), reads=[pkc], writes=["GCS"])
    P.op("act", lambda e: e.activation(out=EGL[:], in_=f3(psl[:, 0:128]), func=AF.Exp), reads=[pkl], writes=["EGL"])
    P.op("dve", lambda e: e.tensor_tensor(out=TM2[:], in0=f3(psl[:, 0:128]), in1=GCS[:], op=ALU.subtract), reads=[pkl, "GCS", "BETA", "EGL"], writes=["TM2"])
    P.op("act", lambda e: e.activation(out=EKD[:], in_=TM2[:], func=AF.Exp), reads=["TM2"], writes=["EKD"])
    P.op("act", lambda e: e.activation(out=TM1[:], in_=GCS[:], func=AF.Exp), reads=["GCS", "G"], writes=["TM1"])
    P.op("dve", lambda e: e.tensor_tensor(out=BEG[:], in0=TM1[:], in1=BETA[:], op=ALU.mult), reads=["TM1", "BETA"], writes=["BEG"])

    k.scale_xt()
    if DN_STOP == "tm":
        P.pop()
        return

    W4 = P.sb("W4", [128, 4, KC, 128], BF16)
    WO = P.sb("dnWO", [128, D], BF16)
    PRE = P.sb("PRE", [128, S + 3])
    QF = P.sb("QF", [128, S])
    KF = P.sb("KF", [128, S])
    VF = P.sb("VF", [128, S])
    SG = P.sb("SG", [128, S], BF16)
    OB = P.sb("dnOB", [128, S], BF16)
    RS1 = P.sb("RS1", [128, 512])
    RS2 = P.sb("RS2", [128, 512])
    Sst = P.sb("Sst", [128, 128])
    Sb = P.sb("Sb", [128, 128], BF16)
    TRIG = P.sb("TRIG", [128, TB, 128])
    ED = P.sb("ED", [128, TB, 128])
    EDT = P.sb("EDT", [128, TB, 128])
    EGB = P.sb("EGB", [128, TB, 128])
    LF = P.sb("LF", [128, TB, 128])
    Nn = [P.sb(f"Nn{i}", [128, TB, 128]) for i in range(2)]
    Mm = [P.sb(f"Mm{i}", [128, TB, 128]) for i in range(2)]
    LOFF = P.sb("LOFF", [128, TB, 128])
    LOFFT = P.sb("LOFFT", [128, TB, 128])
    Tt = P.sb("Tt", [128, TB, 128])
    R = P.sb("Rr", [128, TB, 256])
    SOL = P.sb("SOL", [128, TB, 256])
    KDEC = P.sb("KDEC", [128, TB, 128], BF16)
    WT = P.sb("WT", [128, TB, 128], BF16)
    INTRAT = P.sb("INTRAT", [128, TB, 128], BF16)
    QDT = P.sb("QDT", [128, TB, 128], BF16)
    VNEW = [P.sb(f"VNEW{i}", [128, 128], BF16) for i in range(2)]
    P.op("dve", lambda e: e.memset(PRE[:, 0:3], 0.0), writes=["PRE"])
    OT = PRE

    def load_w4(hh):
        for f in range(4):
            c0 = f * D + hh * 128
            P.dma("pool", W4[:, f, :, :], din["a_w_in"][:, c0:c0 + 128].rearrange("(c p) f -> p c f", p=128), writes=["W4"], key="W4")

    load_w4(0)
    for h in range(8):
        P.dma("pool", WO[:], din["a_w_out"][h * 128:(h + 1) * 128, :], writes=["dnWO"], key="dnWO")
        for f in range(3):
            dst, dk = ((QF, "QF"), (KF, "KF"), (VF, "VF"))[f]
            for n in range(NCH):
                sl = slice(n * 512, (n + 1) * 512)
                ps, pk = _nps(k)
                for c in range(KC):
                    P.op("pe", lambda e: e.matmul(ps[:], W4[:, f, c, :], k.XB[:, c, sl], start=(c == 0), stop=(c == KC - 1)),
                         reads=["W4", ("XB", n)], writes=[pk])
                P.op("act", lambda e: e.activation(out=PRE[:, 3 + n * 512:3 + (n + 1) * 512], in_=ps[:], func=AF.Copy), reads=[pk], writes=["PRE"])
            ci = f * 8 + h
            for hf in range(2):
                eng = "dve"
                o = hf * 1024
                osl = slice(o, o + 1024)
                P.op(eng, lambda e: e.tensor_scalar(out=dst[:, osl], in0=PRE[:, 3 + o:3 + o + 1024], scalar1=CW[:, 3 * 24 + ci:3 * 24 + ci + 1], scalar2=None, op0=ALU.mult),
                     reads=["PRE", "CW"], writes=[dk])
                for j in (2, 1, 0):
                    P.op(eng, lambda e: e.scalar_tensor_tensor(out=dst[:, osl], in0=PRE[:, j + o:j + o + 1024], scalar=CW[:, j * 24 + ci:j * 24 + ci + 1], in1=dst[:, osl],
                                                               op0=ALU.mult, op1=ALU.add), reads=["PRE", "CW", dk], writes=[dk])
            for n in range(NCH):
                sl = slice(n * 512, (n + 1) * 512)
                P.op("act", lambda e: e.activation(out=dst[:, sl], in_=dst[:, sl], func=AF.Silu), reads=[dk], writes=[dk])
            if f < 2:
                for n in range(NCH):
                    sl = slice(n * 512, (n + 1) * 512)
                    P.op("act", lambda e: e.activation(out=VF[:, sl], in_=dst[:, sl], func=AF.Square), reads=[dk], writes=["VF"])
                    ps, pk = _nps(k)
                    P.op("pe", lambda e: e.matmul(ps[:], ones[:], VF[:, sl], start=True, stop=True), reads=["ones", "VF"], writes=[pk])
                    qs = 128.0 if f == 0 else 1.0
                    P.op("dve", lambda e: e.tensor_scalar(out=RS1[:], in0=ps[:], scalar1=qs, scalar2=DN_EPS * qs, op0=ALU.mult, op1=ALU.add), reads=[pk], writes=["RS1"])
                    P.op("act", lambda e: e.activation(out=RS2[:], in_=RS1[:], func=AF.Sqrt), reads=["RS1"], writes=["RS2"])
                    P.op("dve", lambda e: e.reciprocal(out=RS1[:], in_=RS2[:]), reads=["RS2"], writes=["RS1"])
                    P.op("pool", lambda e: e.tensor_tensor(out=dst[:, sl], in0=dst[:, sl], in1=RS1[:], op=ALU.mult), reads=[dk, "RS1"], writes=[dk])
        for n in range(NCH):
            sl = slice(n * 512, (n + 1) * 512)
            ps, pk = _nps(k)
            for c in range(KC):
                P.op("pe", lambda e: e.matmul(ps[:], W4[:, 3, c, :], k.XB[:, c, sl], start=(c == 0), stop=(c == KC - 1)),
                     reads=["W4", ("XB", n)], writes=[pk])
            P.op("act", lambda e: e.activation(out=SG[:, sl], in_=ps[:], func=AF.Silu), reads=[pk], writes=["SG"])
        P.op("dve", lambda e: e.memset(Sst[:], 0.0), writes=["Sst"])
        P.op("dve", lambda e: e.memset(Sb[:], 0.0), writes=["Sb"])
        if h + 1 < 8:
            load_w4(h + 1)
        if DN_STOP == "A":
            P.pop()
            return

        for b in range(NT // TB):
            t0 = b * TB

            def bc(T):
                return T[:, t0:t0 + TB, h:h + 1].broadcast_to([128, TB, 128])

            def tl(i):
                return slice((t0 + i) * 128, (t0 + i + 1) * 128)

            def cs(i):
                return slice(i * 128, (i + 1) * 128)
            psk, pkk = _nps(k)
            psv, pkv = _nps(k)
            for i in range(TB):
                P.op("pe", lambda e: e.transpose(psk[:, cs(i)], KF[:, tl(i)], ident[:]), reads=["KF", "ident"], writes=[pkk])
                P.op("pe", lambda e: e.transpose(psv[:, cs(i)], VF[:, tl(i)], ident[:]), reads=["VF", "ident"], writes=[pkv])
            P.op("dve", lambda e: e.tensor_tensor(out=KDEC[:], in0=v3(psk[:, 0:W]), in1=bc(EKD), op=ALU.mult), reads=[pkk, "EKD"], writes=["KDEC"])
            P.op("dve", lambda e: e.tensor_tensor(out=R[:, :, 128:256], in0=v3(psk[:, 0:W]), in1=bc(BEG), op=ALU.mult), reads=[pkk, "BEG"], writes=["Rr"])
            P.op("dve", lambda e: e.tensor_tensor(out=R[:, :, 0:128], in0=v3(psv[:, 0:W]), in1=bc(BETA), op=ALU.mult), reads=[pkv, "BETA"], writes=["Rr"])
            P.op("pool", lambda e: e.tensor_tensor(out=TRIG[:], in0=b3(TRI_LE), in1=bc(G), op=ALU.mult), reads=["tri_le", "G"], writes=["TRIG"])
            psd, pkd = _nps(k)
            psdt, pkdt = _nps(k)
            psg, pkg = _nps(k)
            for i in range(TB):
                P.op("pe", lambda e: e.matmul(psd[:, cs(i)], TRIG[:, i, :], TRI_GT[:], start=True, stop=True), reads=["TRIG", "tri_gt"], writes=[pkd])
                P.op("pe", lambda e: e.matmul(psdt[:, cs(i)], TRI_GT[:], TRIG[:, i, :], start=True, stop=True), reads=["TRIG", "tri_gt"], writes=[pkdt])
                P.op("pe", lambda e: e.matmul(psg[:, cs(i)], ones[:], TRIG[:, i, :], start=True, stop=True), reads=["TRIG", "ones"], writes=[pkg])
            P.op("act", lambda e: e.activation(out=ED[:], in_=v3(psd[:, 0:W]), func=AF.Exp), reads=[pkd], writes=["ED"])
            P.op("act", lambda e: e.activation(out=EDT[:], in_=v3(psdt[:, 0:W]), func=AF.Exp), reads=[pkdt], writes=["EDT"])
            P.op("act", lambda e: e.activation(out=EGB[:], in_=v3(psg[:, 0:W]), func=AF.Exp), reads=[pkg], writes=["EGB"])
            pskk, pkkk = _nps(k)
            psqk, pkqk = _nps(k)
            for i in range(TB):
                P.op("pe", lambda e: e.matmul(pskk[:, cs(i)], KF[:, tl(i)], KF[:, tl(i)], start=True, stop=True), reads=["KF"], writes=[pkkk])
                P.op("pe", lambda e: e.matmul(psqk[:, cs(i)], KF[:, tl(i)], QF[:, tl(i)], start=True, stop=True), reads=["KF", "QF"], writes=[pkqk])
            P.op("pool", lambda e: e.tensor_tensor(out=ED[:], in0=ED[:], in1=bc(BETA), op=ALU.mult), reads=["ED", "BETA"], writes=["ED"])
            P.op("dve", lambda e: e.tensor_tensor(out=LF[:], in0=v3(pskk[:, 0:W]), in1=ED[:], op=ALU.mult), reads=[pkkk, "ED"], writes=["LF"])
            P.op("pool", lambda e: e.tensor_tensor(out=Nn[0][:], in0=LF[:], in1=b3(NMBD), op=ALU.mult), reads=["LF", "nmask_bd"], writes=["Nn0"])
            P.op("pool", lambda e: e.tensor_tensor(out=LOFF[:], in0=LF[:], in1=b3(MOFF), op=ALU.mult), reads=["LF", "mask_off"], writes=["LOFF"])
            P.op("pool", lambda e: e.tensor_tensor(out=EDT[:], in0=EDT[:], in1=b3(TRI_LE), op=ALU.mult), reads=["EDT", "tri_le"], writes=["EDT"])
            P.op("dve", lambda e: e.tensor_tensor(out=INTRAT[:], in0=v3(psqk[:, 0:W]), in1=EDT[:], op=ALU.mult), reads=[pkqk, "EDT"], writes=["INTRAT"])
            P.op("pool", lambda e: e.tensor_tensor(out=QDT[:], in0=v3(QF[:, t0 * 128:(t0 + TB) * 128]), in1=EGB[:], op=ALU.mult), reads=["QF", "EGB"], writes=["QDT"])
            if DN_STOP == "B1":
                P.pop()
                return
            psa, pka = _nps(k)
            psb, pkb = _nps(k)
            for i in range(TB):
                P.op("pe", lambda e: e.transpose(psa[:, cs(i)], Nn[0][:, i, :], ident[:]), reads=["Nn0", "ident"], writes=[pka])
                P.op("pe", lambda e: e.transpose(psb[:, cs(i)], LOFF[:, i, :], ident[:]), reads=["LOFF", "ident"], writes=[pkb])
            P.op("act", lambda e: e.activation(out=Mm[0][:], in_=v3(psa[:, 0:W]), func=AF.Copy), reads=[pka], writes=["Mm0"])
            P.op("act", lambda e: e.activation(out=LOFFT[:], in_=v3(psb[:, 0:W]), func=AF.Copy), reads=[pkb], writes=["LOFFT"])
            P.op("dve", lambda e: e.tensor_tensor(out=Tt[:], in0=Mm[0][:], in1=b3(ident), op=ALU.add), reads=["Mm0", "ident"], writes=["Tt"])
            if DN_STOP == "T":
                P.pop()
                return
            for kk in range(1, 6):
                a, bb = (kk - 1) % 2, kk % 2
                if DN_STOP == f"kk{kk}":
                    P.pop()
                    return
                psn, pkn = _nps(k)
                for i in range(TB):
                    P.op("pe", lambda e: e.matmul(psn[:, cs(i)], Mm[a][:, i, :], Nn[a][:, i, :], start=True, stop=True), reads=[f"Mm{a}", f"Nn{a}"], writes=[pkn])
                if kk < 5:
                    psm, pkm = _nps(k)
                    for i in range(TB):
                        P.op("pe", lambda e: e.matmul(psm[:, cs(i)], Nn[a][:, i, :], Mm[a][:, i, :], start=True, stop=True), reads=[f"Mm{a}", f"Nn{a}"], writes=[pkm])
                P.op("act", lambda e: e.activation(out=Nn[bb][:], in_=v3(psn[:, 0:W]), func=AF.Copy), reads=[pkn], writes=[f"Nn{bb}"])
                if kk < 5:
                    P.op("dve", lambda e: e.tensor_copy(out=Mm[bb][:], in_=v3(psm[:, 0:W])), reads=[pkm], writes=[f"Mm{bb}"])
                psc, pkc = _nps(k)
                for i in range(TB):
                    P.op("pe", lambda e: e.matmul(psc[:, cs(i)], Nn[bb][:, i, :], Tt[:, i, :], start=True, stop=True), reads=[f"Nn{bb}", "Tt"], writes=[pkc])
                P.op("dve", lambda e: e.tensor_tensor(out=Tt[:], in0=v3(psc[:, 0:W]), in1=Tt[:], op=ALU.add), reads=[pkc, "Tt"], writes=["Tt"])
            if DN_STOP == "B2":
                P.pop()
                return
            v256 = lambda ap: ap.rearrange("p (t n) -> p t n", t=2)
            for hp in range(TB // 2):
                pss, pks = _nps(k)
                for i2 in range(2):
                    i = 2 * hp + i2
                    P.op("pe", lambda e: e.matmul(pss[:, i2 * 256:(i2 + 1) * 256], Tt[:, i, :], R[:, i, :], start=True, stop=True), reads=["Tt", "Rr"], writes=[pks])
                P.op("act", lambda e: e.activation(out=SOL[:, 2 * hp:2 * hp + 2, :], in_=v256(pss[:]), func=AF.Copy), reads=[pks], writes=["SOL"])
            for hp in range(TB // 2):
                psz, pkz = _nps(k)
                for i2 in range(2):
                    i = 2 * hp + i2
                    P.op("pe", lambda e: e.matmul(psz[:, i2 * 256:(i2 + 1) * 256], LOFFT[:, i, :], SOL[:, i, :], start=True, stop=True), reads=["LOFFT", "SOL"], writes=[pkz])
                P.op("act", lambda e: e.activation(out=R[:, 2 * hp:2 * hp + 2, :], in_=v256(psz[:]), func=AF.Copy), reads=[pkz], writes=["Rr"])
            for hp in range(TB // 2):
                psc2, pkc2 = _nps(k)
                for i2 in range(2):
                    i = 2 * hp + i2
                    P.op("pe", lambda e: e.matmul(psc2[:, i2 * 256:(i2 + 1) * 256], Tt[:, i, :], R[:, i, :], start=True, stop=True), reads=["Tt", "Rr"], writes=[pkc2])
                P.op("dve", lambda e: e.scalar_tensor_tensor(out=SOL[:, 2 * hp:2 * hp + 2, :], in0=v256(psc2[:]), scalar=-1.0, in1=SOL[:, 2 * hp:2 * hp + 2, :], op0=ALU.mult, op1=ALU.add),
                     reads=[pkc2, "SOL"], writes=["SOL"])
            psw, pkw = _nps(k)
            for i in range(TB):
                P.op("pe", lambda e: e.transpose(psw[:, cs(i)], SOL[:, i, 128:256], ident[:]), reads=["SOL", "ident"], writes=[pkw])
            P.op("act", lambda e: e.activation(out=WT[:], in_=v3(psw[:, 0:W]), func=AF.Copy), reads=[pkw], writes=["WT"])
            if DN_STOP == "B3":
                P.pop()
                return
            for i in range(TB):
                t = t0 + i
                vn = VNEW[t % 2]
                vk = f"VNEW{t % 2}"
                ps1, pk1 = _nps(k)
                P.op("pe", lambda e: e.matmul(ps1[:, 0:128], WT[:, i, :], Sb[:], start=True, stop=True), reads=["WT", "Sb"], writes=[pk1])
                P.op("dve", lambda e: e.scalar_tensor_tensor(out=vn[:], in0=ps1[:, 0:128], scalar=-1.0, in1=SOL[:, i, 0:128], op0=ALU.mult, op1=ALU.add),
                     reads=[pk1, "SOL"], writes=[vk])
                ps2, pk2 = _nps(k)
                P.op("pe", lambda e: e.matmul(ps2[:, 0:128], Sb[:], QDT[:, i, :], start=True, stop=False), reads=["Sb", "QDT"], writes=[pk2])
                P.op("pe", lambda e: e.matmul(ps2[:, 0:128], vn[:], INTRAT[:, i, :], start=False, stop=True), reads=[vk, "INTRAT"], writes=[pk2])
                P.op("act", lambda e: e.activation(out=OT[:, 3 + t * 128:3 + (t + 1) * 128], in_=ps2[:, 0:128], func=AF.Copy), reads=[pk2], writes=["PRE"])
                ps3, pk3 = _nps(k)
                P.op("pe", lambda e: e.matmul(ps3[:, 0:128], KDEC[:, i, :], vn[:], start=True, stop=True), reads=["KDEC", vk], writes=[pk3])
                P.op("dve", lambda e: e.scalar_tensor_tensor(out=Sst[:], in0=Sst[:], scalar=EGL[:, t, h:h + 1], in1=ps3[:, 0:128], op0=ALU.mult, op1=ALU.add),
                     reads=[pk3, "Sst", "EGL"], writes=["Sst"])
                P.op("act", lambda e: e.activation(out=Sb[:], in_=Sst[:], func=AF.Copy), reads=["Sst"], writes=["Sb"])
        if DN_STOP == "B4":
            P.pop()
            return
        if DN_STOP == "dump":
            for nm, T, kk_ in (("QF", QF, "QF"), ("KF", KF, "KF"), ("VF", VF, "VF")):
                P.dma("sp", k.dbg[nm], T[:], reads=[kk_], writes=["dbg"], key="dbg")
            P.dma("sp", k.dbg["OT"], OT[:, 3:3 + S], reads=["PRE"], writes=["dbg"], key="dbg")
            for nm, T in (("G", G), ("BETA", BETA), ("EKD", EKD), ("BEG", BEG), ("EGL", EGL)):
                P.dma("sp", k.dbg[nm], T[:], reads=[nm], writes=["dbg"], key="dbg")
            P.pop()
            return
        for n in range(NCH):
            sl = slice(n * 512, (n + 1) * 512)
            osl = slice(3 + n * 512, 3 + (n + 1) * 512)
            P.op("act", lambda e: e.activation(out=VF[:, sl], in_=OT[:, osl], func=AF.Square), reads=["PRE"], writes=["VF"])
            ps, pk = _nps(k)
            P.op("pe", lambda e: e.matmul(ps[:], ones[:], VF[:, sl], start=True, stop=True), reads=["ones", "VF"], writes=[pk])
            P.op("dve", lambda e: e.tensor_scalar(out=RS1[:], in0=ps[:], scalar1=1.0 / 128.0, scalar2=DN_EPS, op0=ALU.mult, op1=ALU.add), reads=[pk], writes=["RS1"])
            P.op("act", lambda e: e.activation(out=RS2[:], in_=RS1[:], func=AF.Sqrt), reads=["RS1"], writes=["RS2"])
            P.op("dve", lambda e: e.reciprocal(out=RS1[:], in_=RS2[:]), reads=["RS2"], writes=["RS1"])
            P.op("pool", lambda e: e.tensor_tensor(out=VF[:, sl], in0=OT[:, osl], in1=RS1[:], op=ALU.mult), reads=["PRE", "RS1", "VF"], writes=["VF"])
            P.op("dve", lambda e: e.scalar_tensor_tensor(out=OB[:, sl], in0=VF[:, sl], scalar=NW[:, 0:1], in1=SG[:, sl], op0=ALU.mult, op1=ALU.mult),
                 reads=["VF", "NW", "SG"], writes=["dnOB"])
        for m in range(KC):
            for n in range(NCH):
                sl = slice(n * 512, (n + 1) * 512)
                ps, pk = _nps(k)
                P.op("pe", lambda e: e.matmul(ps[:], WO[:, m * 128:(m + 1) * 128], OB[:, sl], start=True, stop=True), reads=["dnWO", "dnOB"], writes=[pk])
                P.op("dve", lambda e: e.tensor_tensor(out=k.XT[:, m, sl], in0=ps[:], in1=k.XT[:, m, sl], op=ALU.add), reads=[pk, ("XT", n)], writes=[("XT", n)])
    P.pop()
    P.push()
    k.layer_norm(0, k.ln_tmp())
    P.pop()


def from_moba(k):
    P = k.P
    din = k.din
    dc = k.dc
    ident, ones, onesb = k.ident, k.ones, k.onesb
    SCALE = 128.0 ** -0.5
    BIG = 30000.0
    P.push()

    def cload(name, src, shape, dt=F32, q="sp"):
        t = P.sb(name, shape, dt)
        P.dma(q, t[:], src, writes=[name], key="mbc")
        return t
    PERM = cload("perm", dc["perm"], [128, 128])
    ROPEC = cload("ropec", dc["ropec"], [128, S])
    ROPES = cload("ropes", dc["ropes"], [128, S])
    PASTB = cload("pastb", dc["pastb"], [128, NT, 8])
    PAST01 = cload("past01", dc["past01"], [128, NT, 8])
    OWN01 = cload("own01", dc["own01"], [128, NT, 8])
    SEL8b = cload("sel8b", dc["sel8"], [8, 8, 128], BF16, q="pool")
    CAUS = cload("caus4", dc["caus4"], [128, 4, 512], BF16, q="pool")

    k.scale_xt()

    Wq = P.sb("mWq", [128, KC, 128], BF16)
    Wk = P.sb("mWk", [128, KC, 128], BF16)
    Wv = P.sb("mWv", [128, KC, 128], BF16)
    WO = P.sb("mWO", [128, D], BF16)
    QF = P.sb("mQF", [128, S])
    KF = P.sb("mKF", [128, S])
    QB = P.sb("mQB", [128, S], BF16)
    KB = P.sb("mKB", [128, S], BF16)
    VTM = P.sb("mVTM", [128, NT, 128], BF16)
    OB = P.sb("mOB", [128, S], BF16)
    RAW = [P.sb(f"mRAW{i}", [128, 512]) for i in range(2)]
    T1 = P.sb("mT1", [128, 512])
    T2 = P.sb("mT2", [128, 512])
    PT = [P.sb(f"mPT{i}", [128, 512], BF16) for i in range(2)]
    RL = P.sb("mRL", [128, 512])
    KM = P.sb("mKM", [128, 8])
    GATE = P.sb("mGATE", [128, NT, 8])
    M8 = P.sb("mM8", [128, NT, 8])
    SEL = P.sb("mSEL", [128, NT, 8])
    BIAST = P.sb("mBIAST", [8, S], BF16)
    ri = 0
    pi = 0
    def load_w(hh):
        P.dma("pool", Wq[:], din["b_w_q"][:, hh * 128:(hh + 1) * 128].rearrange("(c p) f -> p c f", p=128), writes=["mWq"], key="mWq")
        P.dma("pool", Wk[:], din["b_w_kv"][:, hh * 128:(hh + 1) * 128].rearrange("(c p) f -> p c f", p=128), writes=["mWk"], key="mWk")
        P.dma("pool", Wv[:], din["b_w_kv"][:, D + hh * 128:D + (hh + 1) * 128].rearrange("(c p) f -> p c f", p=128), writes=["mWv"], key="mWv")

    load_w(0)
    for h in range(8):
        P.dma("pool", WO[:], din["b_w_o"][h * 128:(h + 1) * 128, :], writes=["mWO"], key="mWO")
        jobs = [(Wt, wk_, Ff, fk, Bb, bk, n) for (Wt, wk_, Ff, fk, Bb, bk) in ((Wq, "mWq", QF, "mQF", QB, "mQB"), (Wk, "mWk", KF, "mKF", KB, "mKB"))
                for n in range(NCH)]

        def r1(job):
            Wt, wk_, Ff, fk, Bb, bk, n = job
            nonlocal ri
            sl = slice(n * 512, (n + 1) * 512)
            ps, pk = _nps(k)
            for c in range(KC):
                P.op("pe", lambda e: e.matmul(ps[:], Wt[:, c, :], k.XB[:, c, sl], start=(c == 0), stop=(c == KC - 1)), reads=[wk_, ("XB", n)], writes=[pk])
            raw, rk = RAW[ri % 2], f"mRAW{ri % 2}"
            ri += 1
            P.op("act", lambda e: e.activation(out=raw[:], in_=ps[:], func=AF.Copy), reads=[pk], writes=[rk])
            return raw, rk

        def r2(job, raw, rk):
            Wt, wk_, Ff, fk, Bb, bk, n = job
            sl = slice(n * 512, (n + 1) * 512)
            ps2, pk2 = _nps(k)
            P.op("pe", lambda e: e.matmul(ps2[:], PERM[:], raw[:], start=True, stop=True), reads=["perm", rk], writes=[pk2])
            P.op("dve", lambda e: e.tensor_tensor(out=T1[:], in0=ps2[:], in1=ROPES[:, sl], op=ALU.mult), reads=[pk2, "ropes"], writes=["mT1"])
            P.op("pool", lambda e: e.tensor_tensor(out=T2[:], in0=raw[:], in1=ROPEC[:, sl], op=ALU.mult), reads=[rk, "ropec"], writes=["mT2"])
            P.op("pool", lambda e: e.tensor_tensor(out=Ff[:, sl], in0=T1[:], in1=T2[:], op=ALU.add), reads=["mT1", "mT2"], writes=[fk])
            P.op("act", lambda e: e.activation(out=Bb[:, sl], in_=Ff[:, sl], func=AF.Copy), reads=[fk], writes=[bk])

        cur = r1(jobs[0])
        for ji in range(len(jobs)):
            nxt = r1(jobs[ji + 1]) if ji + 1 < len(jobs) else None
            r2(jobs[ji], *cur)
            cur = nxt
        P.op("dve", lambda e: e.tensor_reduce(out=KM[:], in_=KF[:].rearrange("p (b n) -> p b n", n=256), axis=AX.X, op=ALU.add), reads=["mKF"], writes=["mKM"])
        P.op("dve", lambda e: e.tensor_scalar(out=KM[:], in0=KM[:], scalar1=1.0 / 256.0, scalar2=None, op0=ALU.mult), reads=["mKM"], writes=["mKM"])
        for g in range(4):
            ps, pk = _nps(k)
            for i in range(4):
                t = 4 * g + i
                for c in range(KC):
                    P.op("pe", lambda e: e.matmul(ps[:, i * 128:(i + 1) * 128], k.XB[:, c, t * 128:(t + 1) * 128], Wv[:, c, :], start=(c == 0), stop=(c == KC - 1)),
                         reads=["mWv", ("XB", g)], writes=[pk])
            P.op("act", lambda e: e.activation(out=VTM[:, 4 * g:4 * g + 4, :], in_=ps[:].rearrange("p (t n) -> p t n", t=4), func=AF.Copy), reads=[pk], writes=["mVTM"])
        if h + 1 < 8:
            load_w(h + 1)
        ps, pk = _nps(k)
        for t in range(NT):
            P.op("pe", lambda e: e.matmul(ps[:, t * 8:(t + 1) * 8], QF[:, t * 128:(t + 1) * 128], KM[:], start=True, stop=True), reads=["mQF", "mKM"], writes=[pk])
        P.op("dve", lambda e: e.tensor_tensor(out=GATE[:], in0=ps[:, 0:NT * 8].rearrange("p (t n) -> p t n", n=8), in1=PASTB[:], op=ALU.add), reads=[pk, "pastb"], writes=["mGATE"])
        for t in range(NT):
            P.op("dve", lambda e: e.max(out=M8[:, t, :], in_=GATE[:, t, :]), reads=["mGATE"], writes=["mM8"])
        P.op("dve", lambda e: e.tensor_tensor(out=SEL[:], in0=GATE[:], in1=M8[:, :, 2:3].broadcast_to([128, NT, 8]), op=ALU.is_ge), reads=["mGATE", "mM8"], writes=["mSEL"])
        P.op("dve", lambda e: e.tensor_tensor(out=SEL[:], in0=SEL[:], in1=PAST01[:], op=ALU.mult), reads=["mSEL", "past01"], writes=["mSEL"])
        P.op("dve", lambda e: e.tensor_tensor(out=SEL[:], in0=SEL[:], in1=OWN01[:], op=ALU.add), reads=["mSEL", "own01"], writes=["mSEL"])
        P.op("dve", lambda e: e.tensor_scalar(out=SEL[:], in0=SEL[:], scalar1=-1.0, scalar2=BIG, op0=ALU.add, op1=ALU.mult), reads=["mSEL"], writes=["mSEL"])
        for g in range(4):
            ps, pk = _nps(k)
            for i in range(4):
                t = 4 * g + i
                P.op("pe", lambda e: e.transpose(ps[0:8, i * 128:(i + 1) * 128], SEL[:, t, :], ident[:]), reads=["mSEL", "ident"], writes=[pk])
            P.op("act", lambda e: e.activation(out=BIAST[:, g * 512:(g + 1) * 512], in_=ps[0:8, :], func=AF.Copy), reads=[pk], writes=["mBIAST"])
        for qc in range(NCH):
            sl = slice(qc * 512, (qc + 1) * 512)
            pso, pko = _nps(k)
            psl, pkl = _nps(k)
            njt = 4 * qc + 4

            def s_stage(jt):
                while k.psi in (int(pko[2:]), int(pkl[2:])):
                    k.psi = (k.psi + 1) % 8
                pss, pks = _nps(k)
                P.op("pe", lambda e: e.matmul(pss[:], KB[:, jt * 128:(jt + 1) * 128], QB[:, sl], start=True, stop=False), reads=["mKB", "mQB"], writes=[pks])
                P.op("pe", lambda e: e.matmul(pss[:], SEL8b[:, jt // 2, :], BIAST[:, sl], start=False, stop=True), reads=["sel8b", "mBIAST"], writes=[pks])
                return pss, pks

            cur = s_stage(0)
            for jt in range(njt):
                nxt = s_stage(jt + 1) if jt + 1 < njt else None
                pss, pks = cur
                pt, ptk = PT[pi % 2], f"mPT{pi % 2}"
                pi += 1
                P.op("act", lambda e: e.activation(out=pt[:], in_=pss[:], func=AF.Exp, scale=SCALE), reads=[pks], writes=[ptk])
                if jt >= 4 * qc:
                    P.op("pool", lambda e: e.tensor_tensor(out=pt[:], in0=pt[:], in1=CAUS[:, jt - 4 * qc, :], op=ALU.mult), reads=[ptk, "caus4"], writes=[ptk])
                P.op("pe", lambda e: e.matmul(pso[:], VTM[:, jt, :], pt[:], start=(jt == 0), stop=(jt == njt - 1)), reads=["mVTM", ptk], writes=[pko])
                P.op("pe", lambda e: e.matmul(psl[:], onesb[:], pt[:], start=(jt == 0), stop=(jt == njt - 1)), reads=["onesb", ptk], writes=[pkl])
                cur = nxt
            P.op("dve", lambda e: e.reciprocal(out=RL[:], in_=psl[:]), reads=[pkl], writes=["mRL"])
            P.op("dve", lambda e: e.tensor_tensor(out=OB[:, sl], in0=pso[:], in1=RL[:], op=ALU.mult), reads=[pko, "mRL"], writes=["mOB"])
        for m in range(KC):
            for n in range(NCH):
                sl = slice(n * 512, (n + 1) * 512)
                ps, pk = _nps(k)
                P.op("pe", lambda e: e.matmul(ps[:], WO[:, m * 128:(m + 1) * 128], OB[:, sl], start=True, stop=True), reads=["mWO", "mOB"], writes=[pk])
                P.op("dve", lambda e: e.tensor_tensor(out=k.XT[:, m, sl], in0=ps[:], in1=k.XT[:, m, sl], op=ALU.add), reads=[pk, ("XT", n)], writes=[("XT", n)])
    P.pop()
    P.push()
    k.layer_norm(2, k.ln_tmp())
    P.pop()


def make_in_maps(inputs, n_cores=8):
    c = _consts()
    shared = {}
    for name in IN_SHAPES:
        if name == "x":
            continue
        a = np.ascontiguousarray(np.asarray(inputs[name], dtype=np.float32))
        shared[name] = a.reshape(IN_SHAPES[name])
    for name, v in c.items():
        shared["c_" + name] = np.ascontiguousarray(v)
    x = np.asarray(inputs["x"], dtype=np.float32)
    maps = []
    for b in range(n_cores):
        m = dict(shared)
        m["x"] = np.ascontiguousarray(x[b])
        maps.append(m)
    return maps


def kernel(**inputs):
    nc = build()
    maps = make_in_maps(inputs)
    res = run_bass_kernel_spmd(nc, maps, core_ids=list(range(8)))
    return np.stack([np.asarray(r["out"], dtype=np.float32) for r in res.results], axis=0)
```

```python
from contextlib import ExitStack
import numpy as np
import concourse.bass as bass
import concourse.mybir as mybir
from concourse.bass_utils import run_bass_kernel_spmd

F32 = mybir.dt.float32
BF16 = mybir.dt.bfloat16
ALU = mybir.AluOpType
AF = mybir.ActivationFunctionType
AX = mybir.AxisListType

S = 2048
D = 1024
NT = 16
NCH = 4
KC = 8
FH = 3584
NF = 28
NE = 8
ALPHA = 4.0 ** 0.25
LN_EPS = 1e-5
DN_EPS = 1e-6
SEM_ROLL = 20000


class Prog:
    ENG = ("pe", "act", "dve", "pool", "sp")

    def __init__(self, nc):
        self.nc = nc
        self.es = ExitStack()
        self.scopes = [self.es]
        self.e = {"pe": nc.tensor, "act": nc.scalar, "dve": nc.vector, "pool": nc.gpsimd, "sp": nc.sync}
        self.sem = {}
        self.cnt = {}
        self.nsem = 0
        for k in self.ENG:
            self._newsem(k)
        self.waited = {k: {} for k in self.ENG}
        self.last_w = {}
        self.readers = {}
        self.dsem = {}
        self.group_reads = []
        self.n_inst = 0

    def _newsem(self, k):
        name = f"s_{k}_{self.nsem}"
        s = self.es.enter_context(self.nc.semaphore(name))
        self.nsem += 1
        self.sem[k] = (name, s)
        self.cnt[k] = 0

    def sb(self, name, shape, dt=F32):
        return self.scopes[-1].enter_context(self.nc.sbuf_tensor(name, list(shape), dt))

    def push(self):
        self.scopes.append(ExitStack())

    def pop(self):
        self.barrier()
        self.scopes.pop().close()

    def ps(self, name, shape, dt=F32):
        return self.es.enter_context(self.nc.psum_tensor(name, list(shape), dt))

    def _deps(self, engine, reads, writes):
        need = {}

        def add(rec):
            name, s, val, e = rec
            if e == engine and engine == "pe":
                return
            if name not in need or need[name][1] < val:
                need[name] = (s, val)
        for r in list(reads) + self.group_reads:
            if r in self.last_w:
                add(self.last_w[r])
        for w in writes:
            if w in self.last_w:
                add(self.last_w[w])
            for rec in self.readers.get(w, ()):
                add(rec)
        for name, (s, val) in need.items():
            if self.waited[engine].get(name, 0) >= val:
                continue
            self.e[engine].wait_ge(s, val)
            self.waited[engine][name] = val

    def _record(self, rec, reads, writes):
        for r in reads:
            self.readers.setdefault(r, []).append(rec)
        for w in writes:
            self.last_w[w] = rec
            self.readers[w] = []

    def op(self, engine, fn, reads=(), writes=()):
        self._deps(engine, reads, writes)
        inst = fn(self.e[engine])
        name, s = self.sem[engine]
        self.cnt[engine] += 1
        val = self.cnt[engine]
        inst.then_inc(s, 1)
        self.n_inst += 1
        self._record((name, s, val, engine), reads, writes)
        if val >= SEM_ROLL:
            self._newsem(engine)
        return inst

    GROUPS = ("c0", "dnc", "mbc", "wr")

    def dma(self, queue, out, in_, reads=(), writes=(), key=None, **kw):
        if key in self.GROUPS:
            gk = ("grp", key)
            writes = list(writes) + [gk]
            if gk not in self.group_reads:
                self.group_reads.append(gk)
        self._deps(queue, reads, writes)
        if key not in self.dsem:
            name = f"d_{self.nsem}"
            s = self.es.enter_context(self.nc.semaphore(name))
            self.nsem += 1
            self.dsem[key] = [name, s, 0]
        ent = self.dsem[key]
        inst = self.e[queue].dma_start(out=out, in_=in_, **kw)
        ent[2] += 16
        inst.then_inc(ent[1], 16)
        self.n_inst += 1
        self._record((ent[0], ent[1], ent[2], "dma"), reads, writes)
        return inst

    def barrier(self):
        recs = []
        for k in self.ENG:
            name, s = self.sem[k]
            if self.cnt[k] > 0:
                recs.append((name, s, self.cnt[k], k))
        for key, ent in self.dsem.items():
            if ent[2] > 0:
                recs.append((ent[0], ent[1], ent[2], "dma"))
        for k in self.ENG:
            for name, s, val, src in recs:
                if self.waited[k].get(name, 0) >= val:
                    continue
                if src == k and k == "pe":
                    continue
                self.e[k].wait_ge(s, val)
                self.waited[k][name] = val

    def close(self):
        self.barrier()
        self.es.close()


def _consts():
    c = {}
    c["ident"] = np.eye(128, dtype=np.float32)
    c["ones"] = np.ones((128, 128), dtype=np.float32)
    half = 64
    inv_freq = (10000.0 ** (-(np.arange(half, dtype=np.float32) / np.float32(half)))).astype(np.float32)
    ang = (np.arange(S, dtype=np.float32)[:, None] * inv_freq[None, :]).astype(np.float32)
    cos = np.cos(ang.astype(np.float64)).astype(np.float32).T
    sin = np.sin(ang.astype(np.float64)).astype(np.float32).T
    c["ropec"] = np.concatenate([cos, cos], axis=0)
    c["ropes"] = np.concatenate([-sin, sin], axis=0)
    sel = np.zeros((8, 8, 128), dtype=np.float32)
    for e in range(8):
        sel[e, e, :] = 1.0
    c["sel8"] = sel.transpose(1, 0, 2).copy()
    r = np.arange(128)[:, None]
    q = np.arange(128)[None, :]
    c["tri_le"] = (r <= q).astype(np.float32)
    c["tri_gt"] = (r > q).astype(np.float32)
    c["nmask_bd"] = -((r > q) & ((r // 64) == (q // 64))).astype(np.float32)
    c["mask_off"] = ((r >= 64) & (q < 64)).astype(np.float32)
    c["perm"] = (r == ((q + 64) % 128)).astype(np.float32)
    pb = np.zeros((128, NT, 8), dtype=np.float32)
    p01 = np.zeros((128, NT, 8), dtype=np.float32)
    own = np.zeros((128, NT, 8), dtype=np.float32)
    for t in range(NT):
        qb = t // 2
        own[:, t, qb] = 1.0
        for n in range(8):
            if n < qb:
                p01[:, t, n] = 1.0
            else:
                pb[:, t, n] = -1.0e30
    c["pastb"] = pb
    c["past01"] = p01
    c["own01"] = own
    caus = np.ones((128, 4, 512), dtype=np.float32)
    j = np.arange(128)[:, None]
    ii = np.arange(512)[None, :]
    for pos in range(4):
        same = (pos // 2) == (ii // 256)
        caus[:, pos, :] = np.where(same & ((128 * pos + j) > ii), 0.0, 1.0)
    c["caus4"] = caus
    return c


CONST_SHAPES = {"ident": [128, 128], "ones": [128, 128], "ropec": [128, S], "ropes": [128, S],
                "sel8": [8, 8, 128], "tri_le": [128, 128], "tri_gt": [128, 128], "nmask_bd": [128, 128],
                "mask_off": [128, 128], "perm": [128, 128], "pastb": [128, NT, 8], "past01": [128, NT, 8],
                "own01": [128, NT, 8], "caus4": [128, 4, 512]}

IN_SHAPES = {
    "x": [S, D], "a_w_in": [D, 4112], "a_conv_w": [4, 3072], "a_log_decay": [1, 8], "a_dt_bias": [1, 8],
    "a_norm_w": [1, 128], "a_w_out": [D, D], "b_w_kv": [D, 2 * D], "b_w_q": [D, D], "b_w_o": [D, D],
    "ffn_w1": [D, FH], "ffn_w3": [D, FH], "ffn_w2": [FH, D], "moe_router": [D, 8],
    "moe_w1": [8, D, FH], "moe_w3": [8, D, FH], "moe_w2": [8, FH, D], "ln_g": [4, D], "ln_b": [4, D],
}


class K:
    def __init__(self, stages, dbg=()):
        self.stages = stages
        nc = bass.Bass("TRN2", target_bir_lowering=False)
        self.nc = nc
        self.P = Prog(nc)
        P = self.P
        self.din = {k: nc.dram_tensor(k, v, F32, kind="ExternalInput").ap() for k, v in IN_SHAPES.items()}
        self.dc = {k: nc.dram_tensor("c_" + k, v, F32, kind="ExternalInput").ap() for k, v in CONST_SHAPES.items()}
        self.out = nc.dram_tensor("out", [S, D], F32, kind="ExternalOutput").ap()
        self.dbg = {}
        for name, shape in dbg:
            self.dbg[name] = nc.dram_tensor("dbg_" + name, shape, F32, kind="ExternalOutput").ap()
        self.uid = 0
        self.XT = P.sb("XT", [128, KC, S])
        self.XB = P.sb("XB", [128, KC, S], BF16)
        self.ident = P.sb("ident", [128, 128])
        self.identb = P.sb("identb", [128, 128], BF16)
        self.ones = P.sb("ones", [128, 128])
        self.onesb = P.sb("onesb", [128, 128], BF16)
        self.lng = P.sb("lng", [128, 4, KC])
        self.lnb = P.sb("lnb", [128, 4, KC])
        self.PS = [P.ps(f"PS{i}", [128, 512]) for i in range(8)]
        self.psk = [f"PS{i}" for i in range(8)]

        P.dma("sp", self.ident[:], self.dc["ident"], writes=["ident"], key="c0")
        P.dma("sp", self.ones[:], self.dc["ones"], writes=["ones"], key="c0")
        lnraw = P.sb("lnraw", [64, 128])
        P.dma("sp", lnraw[0:32, :], self.din["ln_g"].rearrange("l (c p) -> (l c) p", p=128), writes=["lnraw"], key="c0")
        P.dma("sp", lnraw[32:64, :], self.din["ln_b"].rearrange("l (c p) -> (l c) p", p=128), writes=["lnraw"], key="c0")
        P.op("pe", lambda e: e.transpose(self.PS[0][:, 0:64], lnraw[:], self.ident[0:64, 0:64]), reads=["lnraw", "ident"], writes=["PS0"])
        P.op("dve", lambda e: e.tensor_copy(out=self.lng[:], in_=self.PS[0][:, 0:32].rearrange("p (l c) -> p l c", l=4)), reads=["PS0"], writes=["lng"])
        P.op("dve", lambda e: e.tensor_copy(out=self.lnb[:], in_=self.PS[0][:, 32:64].rearrange("p (l c) -> p l c", l=4)), reads=["PS0"], writes=["lnb"])
        P.op("dve", lambda e: e.tensor_copy(out=self.identb[:], in_=self.ident[:]), reads=["ident"], writes=["identb"])
        P.op("dve", lambda e: e.tensor_copy(out=self.onesb[:], in_=self.ones[:]), reads=["ones"], writes=["onesb"])

    def u(self, s):
        self.uid += 1
        return f"{s}{self.uid}"

    def load_x(self):
        P = self.P
        xin = [P.sb(f"xin{i}", [128, D]) for i in range(2)]
        for t in range(NT):
            b = xin[t % 2]
            bk = f"xin{t % 2}"
            P.dma("sp", b[:], self.din["x"][t * 128:(t + 1) * 128, :], writes=[bk], key=bk)
            for hlf in range(2):
                ps = self.PS[2 * (t % 2) + hlf]
                pk = self.psk[2 * (t % 2) + hlf]
                for c4 in range(4):
                    c = hlf * 4 + c4
                    P.op("pe", lambda e: e.transpose(ps[:, c4 * 128:(c4 + 1) * 128], b[:, c * 128:(c + 1) * 128], self.ident[:]),
                         reads=[bk, "ident"], writes=[pk])
                src = ps[:].rearrange("p (c n) -> p c n", c=4)
                P.op("act", lambda e: e.activation(out=self.XT[:, hlf * 4:(hlf + 1) * 4, t * 128:(t + 1) * 128], in_=src, func=AF.Copy),
                     reads=[pk], writes=[("XT", t // 4)])
                P.op("dve", lambda e: e.tensor_copy(out=self.XB[:, hlf * 4:(hlf + 1) * 4, t * 128:(t + 1) * 128],
                                                    in_=self.XT[:, hlf * 4:(hlf + 1) * 4, t * 128:(t + 1) * 128]),
                     reads=[("XT", t // 4)], writes=[("XB", t // 4)])

    def store_x(self):
        P = self.P
        xo = [P.sb(f"xo{i}", [128, D]) for i in range(2)]
        for t in range(NT):
            b = xo[t % 2]
            bk = f"xo{t % 2}"
            for hlf in range(2):
                ps = self.PS[2 * (t % 2) + hlf]
                pk = self.psk[2 * (t % 2) + hlf]
                for c4 in range(4):
                    c = hlf * 4 + c4
                    P.op("pe", lambda e: e.transpose(ps[:, c4 * 128:(c4 + 1) * 128], self.XT[:, c, t * 128:(t + 1) * 128], self.ident[:]),
                         reads=[("XT", t // 4), "ident"], writes=[pk])
                if hlf == 0:
                    P.op("act", lambda e: e.activation(out=b[:, 0:512], in_=ps[:], func=AF.Copy), reads=[pk], writes=[bk])
                else:
                    P.op("dve", lambda e: e.tensor_copy(out=b[:, 512:1024], in_=ps[:]), reads=[pk], writes=[bk])
            P.dma("sp", self.out[t * 128:(t + 1) * 128, :], b[:], reads=[bk], writes=["out"], key=bk)

    def dbg_store_xt(self, name):
        P = self.P
        for c in range(KC):
            P.dma("sp", self.dbg[name][c], self.XT[:, c, :], reads=[("XT", n) for n in range(NCH)], writes=["dbg"], key="dbg")

    def layer_norm(self, idx, tmp):
        P = self.P
        SQ, R1, R2, MEAN, RSTD = tmp["sq"], tmp["r1"], tmp["r2"], tmp["mean"], tmp["rstd"]
        for n in range(NCH):
            sl = slice(n * 512, (n + 1) * 512)
            xk = ("XT", n)
            Yv = self.XT[:, :, sl]
            P.op("act", lambda e: e.activation(out=SQ[:], in_=Yv, func=AF.Square), reads=[xk], writes=["ln_sq"])
            P.op("dve", lambda e: e.tensor_reduce(out=R1[:], in_=Yv.rearrange("p c n -> p n c"), axis=AX.X, op=ALU.add),
                 reads=[xk], writes=["ln_r1"])
            P.op("dve", lambda e: e.tensor_reduce(out=R2[:], in_=SQ[:].rearrange("p c n -> p n c"), axis=AX.X, op=ALU.add),
                 reads=["ln_sq"], writes=["ln_r2"])
            ps1, pk1 = self.PS[6], self.psk[6]
            ps2, pk2 = self.PS[7], self.psk[7]
            P.op("pe", lambda e: e.matmul(ps1[:], self.ones[:], R1[:], start=True, stop=True), reads=["ones", "ln_r1"], writes=[pk1])
            P.op("pe", lambda e: e.matmul(ps2[:], self.ones[:], R2[:], start=True, stop=True), reads=["ones", "ln_r2"], writes=[pk2])
            P.op("act", lambda e: e.activation(out=MEAN[:], in_=ps1[:], func=AF.Copy, scale=1.0 / D), reads=[pk1], writes=["ln_mean"])
            P.op("dve", lambda e: e.tensor_tensor(out=R1[:], in0=MEAN[:], in1=MEAN[:], op=ALU.mult), reads=["ln_mean"], writes=["ln_r1"])
            P.op("dve", lambda e: e.scalar_tensor_tensor(out=R2[:], in0=ps2[:], scalar=1.0 / D, in1=R1[:], op0=ALU.mult, op1=ALU.subtract),
                 reads=[pk2, "ln_r1"], writes=["ln_r2"])
            P.op("dve", lambda e: e.tensor_scalar(out=R2[:], in0=R2[:], scalar1=LN_EPS, scalar2=None, op0=ALU.add), reads=["ln_r2"], writes=["ln_r2"])
            P.op("act", lambda e: e.activation(out=R1[:], in_=R2[:], func=AF.Sqrt), reads=["ln_r2"], writes=["ln_r1"])
            P.op("dve", lambda e: e.reciprocal(out=RSTD[:], in_=R1[:]), reads=["ln_r1"], writes=["ln_rstd"])
            mb = MEAN[:].unsqueeze(1).broadcast_to([128, KC, 512])
            rb = RSTD[:].unsqueeze(1).broadcast_to([128, KC, 512])
            P.op("dve", lambda e: e.tensor_tensor(out=SQ[:], in0=Yv, in1=mb, op=ALU.subtract), reads=[xk, "ln_mean"], writes=["ln_sq"])
            P.op("pool", lambda e: e.tensor_tensor(out=SQ[:], in0=SQ[:], in1=rb, op=ALU.mult), reads=["ln_sq", "ln_rstd"], writes=["ln_sq"])
            for c in range(KC):
                P.op("act", lambda e: e.activation(out=self.XT[:, c, sl], in_=SQ[:, c, :], func=AF.Identity,
                                                   scale=self.lng[:, idx, c:c + 1], bias=self.lnb[:, idx, c:c + 1]),
                     reads=["ln_sq", "lng", "lnb"], writes=[xk])
                P.op("dve", lambda e: e.tensor_scalar(out=self.XB[:, c, sl], in0=SQ[:, c, :], scalar1=self.lng[:, idx, c:c + 1],
                                                      scalar2=self.lnb[:, idx, c:c + 1], op0=ALU.mult, op1=ALU.add),
                     reads=["ln_sq", "lng", "lnb"], writes=[("XB", n)])

    def ln_tmp(self):
        P = self.P
        return {"sq": P.sb(self.u("ln_sq"), [128, KC, 512]), "r1": P.sb(self.u("ln_r1"), [128, 512]),
                "r2": P.sb(self.u("ln_r2"), [128, 512]), "mean": P.sb(self.u("ln_mean"), [128, 512]),
                "rstd": P.sb(self.u("ln_rstd"), [128, 512])}

    def scale_xt(self):
        P = self.P
        for n in range(NCH):
            sl = slice(n * 512, (n + 1) * 512)
            eng = "act" if n % 2 == 0 else "dve"
            if eng == "act":
                P.op("act", lambda e: e.activation(out=self.XT[:, :, sl], in_=self.XT[:, :, sl], func=AF.Copy, scale=ALPHA),
                     reads=[("XT", n)], writes=[("XT", n)])
            else:
                P.op("dve", lambda e: e.tensor_scalar(out=self.XT[:, :, sl], in0=self.XT[:, :, sl], scalar1=ALPHA, scalar2=None, op0=ALU.mult),
                     reads=[("XT", n)], writes=[("XT", n)])

    def ffn(self, experts, gb=None):
        P = self.P
        G = 4
        NB = 2
        W1 = [P.sb(self.u("w1g"), [128, KC, G * 128], BF16) for _ in range(NB)]
        W3 = [P.sb(self.u("w3g"), [128, KC, G * 128], BF16) for _ in range(NB)]
        W2 = [P.sb(self.u("w2g"), [128, G, D], BF16) for _ in range(NB)]
        H = [P.sb(self.u("hg"), [128, G, 512], BF16) for _ in range(2)]
        SA = [P.sb(self.u("sa"), [128, 512], BF16) for _ in range(2)]
        HT = [P.sb(self.u("ht"), [128, 512], BF16) for _ in range(2)]
        base = self.u("ffn")
        gi = 0
        si = 0
        hi = 0
        groups = [(ei, ex, fg) for ei, ex in enumerate(experts) for fg in range(NF // G)]

        def issue_load(k):
            ei, ex, fg = groups[k]
            w1, w3, w2, _ = ex
            slot = k % NB
            f0 = fg * G * 128
            P.dma("pool", W1[slot][:], w1[:, f0:f0 + G * 128].rearrange("(c p) f -> p c f", p=128),
                  writes=[(base, "w1", slot)], key=(base, "w1", slot))
            P.dma("pool", W3[slot][:], w3[:, f0:f0 + G * 128].rearrange("(c p) f -> p c f", p=128),
                  writes=[(base, "w3", slot)], key=(base, "w3", slot))
            P.dma("pool", W2[slot][:], w2[f0:f0 + G * 128, :].rearrange("(j p) m -> p j m", p=128),
                  writes=[(base, "w2", slot)], key=(base, "w2", slot))

        issue_load(0)
        st = {"gt": None, "gk": None, "si": 0, "gi": 0}
        steps = [(k, n) for k in range(len(groups)) for n in range(NCH)]

        def phase1(s):
            k, n = steps[s]
            ei, ex, fg = groups[k]
            slot = k % NB
            if n == 0:
                if ex[3] is None:
                    st["gt"] = None
                elif fg == 0:
                    st["gt"], st["gk"] = gb(ex[3])
            gt, gk = st["gt"], st["gk"]
            sl = slice(n * 512, (n + 1) * 512)
            hb = H[s % 2]
            hk = (base, "h", s % 2)
            for j in range(G):
                si = st["si"]
                pa, pak = self.PS[2 * (si % 2)], self.psk[2 * (si % 2)]
                pb, pbk = self.PS[2 * (si % 2) + 1], self.psk[2 * (si % 2) + 1]
                sa, sak = SA[si % 2], (base, "sa", si % 2)
                for c in range(KC):
                    P.op("pe", lambda e: e.matmul(pa[:], W1[slot][:, c, j * 128:(j + 1) * 128], self.XB[:, c, sl], start=(c == 0), stop=(c == KC - 1)),
                         reads=[(base, "w1", slot), ("XB", n)], writes=[pak])
                for c in range(KC):
                    P.op("pe", lambda e: e.matmul(pb[:], W3[slot][:, c, j * 128:(j + 1) * 128], self.XB[:, c, sl], start=(c == 0), stop=(c == KC - 1)),
                         reads=[(base, "w3", slot), ("XB", n)], writes=[pbk])
                P.op("act", lambda e: e.activation(out=sa[:], in_=pa[:], func=AF.Silu), reads=[pak], writes=[sak])
                if gt is None:
                    P.op("dve", lambda e: e.tensor_tensor(out=hb[:, j, :], in0=pb[:], in1=sa[:], op=ALU.mult),
                         reads=[pbk, sak], writes=[hk])
                else:
                    ht, htk = HT[si % 2], (base, "ht", si % 2)
                    P.op("pool", lambda e: e.tensor_tensor(out=ht[:], in0=sa[:], in1=gt[:, sl], op=ALU.mult),
                         reads=[sak, gk], writes=[htk])
                    P.op("dve", lambda e: e.tensor_tensor(out=hb[:, j, :], in0=pb[:], in1=ht[:], op=ALU.mult),
                         reads=[pbk, htk], writes=[hk])
                st["si"] += 1

        def phase2(s):
            k, n = steps[s]
            slot = k % NB
            sl = slice(n * 512, (n + 1) * 512)
            hb = H[s % 2]
            hk = (base, "h", s % 2)
            for m in range(KC):
                gi = st["gi"]
                py, pyk = self.PS[4 + gi % 4], self.psk[4 + gi % 4]
                st["gi"] += 1
                for j in range(G):
                    P.op("pe", lambda e: e.matmul(py[:], W2[slot][:, j, m * 128:(m + 1) * 128], hb[:, j, :], start=(j == 0), stop=(j == G - 1)),
                         reads=[(base, "w2", slot), hk], writes=[pyk])
                P.op("dve", lambda e: e.tensor_tensor(out=self.XT[:, m, sl], in0=py[:], in1=self.XT[:, m, sl], op=ALU.add),
                     reads=[pyk, ("XT", n)], writes=[("XT", n)])

        if len(groups) > 1:
            issue_load(1)
        phase1(0)
        for s in range(len(steps)):
            if s + 1 < len(steps):
                phase1(s + 1)
            phase2(s)
            kk_, nn_ = steps[s]
            if nn_ == NCH - 1 and kk_ + 2 < len(groups):
                issue_load(kk_ + 2)

    def router(self):
        P = self.P
        WR = P.sb("wr", [128, KC, 8])
        LG = P.sb("r_lg", [128, NT, 8])
        M8 = P.sb("r_m8", [128, NT, 8])
        EX = P.sb("r_ex", [128, NT, 8])
        SELM = P.sb("r_sel", [128, NT, 8])
        DEN = P.sb("r_den", [128, NT])
        GT = P.sb("r_gt", [8, S])
        SEL8 = P.sb("r_sel8", [8, 8, 128])
        P.dma("sp", WR[:], self.din["moe_router"].rearrange("(c p) e -> p c e", p=128), writes=["wr"], key="wr")
        P.dma("sp", SEL8[:], self.dc["sel8"], writes=["sel8"], key="wr")
        ps, pk = self.PS[0], self.psk[0]
        for t in range(NT):
            for c in range(KC):
                P.op("pe", lambda e: e.matmul(ps[:, t * 8:(t + 1) * 8], self.XT[:, c, t * 128:(t + 1) * 128], WR[:, c, :], start=(c == 0), stop=(c == KC - 1)),
                     reads=[("XT", t // 4), "wr"], writes=[pk])
        P.op("dve", lambda e: e.tensor_copy(out=LG[:], in_=ps[:, 0:NT * 8].rearrange("p (t e) -> p t e", e=8)), reads=[pk], writes=["r_lg"])
        for t in range(NT):
            P.op("dve", lambda e: e.max(out=M8[:, t, :], in_=LG[:, t, :]), reads=["r_lg"], writes=["r_m8"])
        m1 = M8[:, :, 0:1].broadcast_to([128, NT, 8])
        m2 = M8[:, :, 1:2].broadcast_to([128, NT, 8])
        P.op("dve", lambda e: e.tensor_tensor(out=EX[:], in0=LG[:], in1=m1, op=ALU.subtract), reads=["r_lg", "r_m8"], writes=["r_ex"])
        P.op("act", lambda e: e.activation(out=EX[:], in_=EX[:], func=AF.Exp), reads=["r_ex"], writes=["r_ex"])
        P.op("dve", lambda e: e.tensor_tensor(out=SELM[:], in0=LG[:], in1=m2, op=ALU.is_ge), reads=["r_lg", "r_m8"], writes=["r_sel"])
        P.op("dve", lambda e: e.tensor_tensor(out=EX[:], in0=EX[:], in1=SELM[:], op=ALU.mult), reads=["r_ex", "r_sel"], writes=["r_ex"])
        P.op("dve", lambda e: e.tensor_reduce(out=DEN[:], in_=EX[:], axis=AX.X, op=ALU.add), reads=["r_ex"], writes=["r_den"])
        P.op("dve", lambda e: e.reciprocal(out=DEN[:], in_=DEN[:]), reads=["r_den"], writes=["r_den"])
        P.op("dve", lambda e: e.tensor_tensor(out=EX[:], in0=EX[:], in1=DEN[:].unsqueeze(2).broadcast_to([128, NT, 8]), op=ALU.mult),
             reads=["r_ex", "r_den"], writes=["r_ex"])
        for q in range(4):
            pt, ptk = self.PS[1 + q % 2], self.psk[1 + q % 2]
            for tt in range(4):
                t = q * 4 + tt
                P.op("pe", lambda e: e.transpose(pt[0:8, tt * 128:(tt + 1) * 128], EX[:, t, :], self.ident[:]), reads=["r_ex", "ident"], writes=[ptk])
            P.op("act", lambda e: e.activation(out=GT[:, q * 512:(q + 1) * 512], in_=pt[0:8, :], func=AF.Copy), reads=[ptk], writes=["r_gt"])
        self.GT = GT
        self.SEL8 = SEL8
        self.GB = [P.sb(f"gb{i}", [128, S], BF16) for i in range(2)]
        if "g" in self.dbg:
            P.dma("sp", self.dbg["g"], EX[:], reads=["r_ex"], writes=["dbg"], key="dbg")

    def gate_bcast(self, eidx):
        P = self.P
        gbt = self.GB[eidx % 2]
        gk = ("gb", eidx % 2)
        for n in range(NCH):
            ps, pk = self.PS[6 + n % 2], self.psk[6 + n % 2]
            P.op("pe", lambda e: e.matmul(ps[:], self.SEL8[:, eidx, :], self.GT[:, n * 512:(n + 1) * 512], start=True, stop=True),
                 reads=["sel8", "r_gt"], writes=[pk])
            P.op("act", lambda e: e.activation(out=gbt[:, n * 512:(n + 1) * 512], in_=ps[:], func=AF.Copy), reads=[pk], writes=[gk])
        return gbt, gk


def build(stages=("load", "dn", "ffn0", "moba", "moe", "store"), dbg=()):
    k = K(stages, dbg)
    P = k.P
    din = k.din
    if "load" in stages:
        P.push()
        k.load_x()
        P.pop()
    if "dn" in stages:
        from_dn(k)
    if "ffn0" in stages:
        P.push()
        k.scale_xt()
        k.ffn([(din["ffn_w1"], din["ffn_w3"], din["ffn_w2"], None)])
        P.pop()
        P.push()
        k.layer_norm(1, k.ln_tmp())
        P.pop()
    if "dbg_x1" in k.dbg:
        k.dbg_store_xt("x1")
    if "moba" in stages:
        from_moba(k)
    if "moe" in stages:
        P.push()
        k.router()
        k.scale_xt()
        k.ffn([(din["moe_w1"][e], din["moe_w3"][e], din["moe_w2"][e], e) for e in range(NE)], gb=k.gate_bcast)
        P.pop()
        P.push()
        k.layer_norm(3, k.ln_tmp())
        P.pop()
    if "store" in stages:
        P.push()
        k.store_x()
        P.pop()
    P.close()
    return k.nc


def _nps(k):
    i = getattr(k, "psi", 0)
    k.psi = (i + 1) % 8
    return k.PS[i], k.psk[i]


DN_STOP = None


def from_dn(k):
    P = k.P
    din = k.din
    dc = k.dc
    TB = 4
    W = TB * 128
    QSCALE = 128.0 ** -0.5
    P.push()

    def cload(name, src, shape, dt=F32, q="sp"):
        t = P.sb(name, shape, dt)
        P.dma(q, t[:], src, writes=[name], key="dnc")
        return t

    TRI_LE = cload("tri_le", dc["tri_le"], [128, 128])
    TRI_GT = cload("tri_gt", dc["tri_gt"], [128, 128])
    NMBD = cload("nmask_bd", dc["nmask_bd"], [128, 128])
    MOFF = cload("mask_off", dc["mask_off"], [128, 128])
    ident, ones = k.ident, k.ones

    def b3(T):
        return T[:].unsqueeze(1).broadcast_to([128, TB, 128])

    def v3(ap):
        return ap.rearrange("p (t n) -> p t n", t=TB)

    cwraw = P.sb("cwraw", [96, 128])
    P.dma("sp", cwraw[:], din["a_conv_w"].rearrange("j (c p) -> (j c) p", p=128), writes=["cwraw"], key="dnc")
    CW = P.sb("CW", [128, 96])
    ps, pk = _nps(k)
    P.op("pe", lambda e: e.transpose(ps[:, 0:96], cwraw[:], ident[0:96, 0:96]), reads=["cwraw", "ident"], writes=[pk])
    P.op("dve", lambda e: e.tensor_copy(out=CW[:], in_=ps[:, 0:96]), reads=[pk], writes=["CW"])
    NW = P.sb("NW", [128, 1])
    P.dma("sp", NW[:], din["a_norm_w"].rearrange("o p -> p o"), writes=["NW"], key="dnc")

    WAB = P.sb("WAB", [128, KC, 16])
    P.dma("sp", WAB[:], din["a_w_in"][:, 4096:4112].rearrange("(c p) f -> p c f", p=128), writes=["WAB"], key="dnc")
    RW = P.sb("RW", [1, 16])
    P.dma("sp", RW[0:1, 0:8], din["a_log_decay"], writes=["RW"], key="dnc")
    P.dma("sp", RW[0:1, 8:16], din["a_dt_bias"], writes=["RW"], key="dnc")
    AB = P.sb("AB", [128, NT, 16])
    BC = P.sb("BC", [128, 16])
    ps, pk = _nps(k)
    for t in range(NT):
        for c in range(KC):
            P.op("pe", lambda e: e.matmul(ps[:, t * 16:(t + 1) * 16], k.XT[:, c, t * 128:(t + 1) * 128], WAB[:, c, :], start=(c == 0), stop=(c == KC - 1)),
                 reads=[("XT", t // 4), "WAB"], writes=[pk])
    P.op("dve", lambda e: e.tensor_copy(out=AB[:], in_=ps[:, 0:NT * 16].rearrange("p (t f) -> p t f", f=16)), reads=[pk], writes=["AB"])
    ps, pk = _nps(k)
    P.op("pe", lambda e: e.matmul(ps[:, 0:16], ones[0:1, :], RW[0:1, :], start=True, stop=True), reads=["ones", "RW"], writes=[pk])
    P.op("dve", lambda e: e.tensor_copy(out=BC[:], in_=ps[:, 0:16]), reads=[pk], writes=["BC"])

    def tm(name):
        return P.sb(name, [128, NT, 8])
    G, BETA, EKD, BEG, EGL, TM1, TM2, GCS = (tm(n) for n in ("G", "BETA", "EKD", "BEG", "EGL", "TM1", "TM2", "GCS"))
    EA = P.sb("EA", [128, 8])
    dtb = BC[:, 8:16].unsqueeze(1).broadcast_to([128, NT, 8])
    P.op("dve", lambda e: e.tensor_tensor(out=TM1[:], in0=AB[:, :, 0:8], in1=dtb, op=ALU.add), reads=["AB", "BC"], writes=["TM1"])
    P.op("act", lambda e: e.activation(out=TM1[:], in_=TM1[:], func=AF.Exp), reads=["TM1"], writes=["TM1"])
    P.op("dve", lambda e: e.tensor_scalar(out=TM1[:], in0=TM1[:], scalar1=1.0, scalar2=None, op0=ALU.add), reads=["TM1"], writes=["TM1"])
    P.op("act", lambda e: e.activation(out=TM1[:], in_=TM1[:], func=AF.Ln), reads=["TM1"], writes=["TM1"])
    P.op("act", lambda e: e.activation(out=EA[:], in_=BC[:, 0:8], func=AF.Exp), reads=["BC"], writes=["EA"])
    P.op("dve", lambda e: e.scalar_tensor_tensor(out=G[:], in0=TM1[:], scalar=-1.0, in1=EA[:].unsqueeze(1).broadcast_to([128, NT, 8]),
                                                 op0=ALU.mult, op1=ALU.mult), reads=["TM1", "EA"], writes=["G"])
    P.op("act", lambda e: e.activation(out=TM2[:], in_=AB[:, :, 8:16], func=AF.Exp, scale=-1.0), reads=["AB"], writes=["TM2"])
    P.op("dve", lambda e: e.tensor_scalar(out=TM2[:], in0=TM2[:], scalar1=1.0, scalar2=None, op0=ALU.add), reads=["TM2"], writes=["TM2"])
    P.op("dve", lambda e: e.reciprocal(out=BETA[:], in_=TM2[:]), reads=["TM2"], writes=["BETA"])
    Gf = G[:].rearrange("p t h -> p (t h)")
    psc, pkc = _nps(k)
    psl, pkl = _nps(k)
    P.op("pe", lambda e: e.matmul(psc[:, 0:128], TRI_LE[:], Gf, start=True, stop=True), reads=["tri_le", "G"], writes=[pkc])
    P.op("pe", lambda e: e.matmul(psl[:, 0:128], ones[:], Gf, start=True, stop=True), reads=["ones", "G"], writes=[pkl])
    f3 = lambda ap: ap.rearrange("p (t h) -> p t h", h=8)
    P.op("act", lambda e: e.activation(out=GCS[:], in_=f3(psc[:, 0:128]), func=AF.Copy), reads=[pkc], writes=["GCS"])
    P.op("act", lambda e: e.activation(out=EGL[:], in_=f3(psl[:, 0:128]), func=AF.Exp), reads=[pkl], writes=["EGL"])
    P.op("dve", lambda e: e.tensor_tensor(out=TM2[:], in0=f3(psl[:, 0:128]), in1=GCS[:], op=ALU.subtract), reads=[pkl, "GCS", "BETA", "EGL"], writes=["TM2"])
    P.op("act", lambda e: e.activation(out=EKD[:], in_=TM2[:], func=AF.Exp), reads=["TM2"], writes=["EKD"])
    P.op("act", lambda e: e.activation(out=TM1[:], in_=GCS[:], func=AF.Exp), reads=["GCS", "G"], writes=["TM1"])
    P.op("dve", lambda e: e.tensor_tensor(out=BEG[:], in0=TM1[:], in1=BETA[:], op=ALU.mult), reads=["TM1", "BETA"], writes=["BEG"])

    k.scale_xt()
    if DN_STOP == "tm":
        P.pop()
        return

    W4 = P.sb("W4", [128, 4, KC, 128], BF16)
    WO = P.sb("dnWO", [128, D], BF16)
    OT = P.sb("OT", [128, S])
    DG = P.sb("DG", [128, 3, 4, 128], BF16)
    QF = P.sb("QF", [128, S])
    KF = P.sb("KF", [128, S])
    VF = P.sb("VF", [128, S])
    SG = P.sb("SG", [128, S], BF16)
    OBP = P.sb("dnOB", [128, S + 3], BF16)
    OB = OBP[:, 3:S + 3]
    PREb = OBP
    RS1 = P.sb("RS1", [128, 512])
    RS2 = P.sb("RS2", [128, 512])
    Sst = P.sb("Sst", [128, 128])
    Sb = P.sb("Sb", [128, 128], BF16)
    TRIG = P.sb("TRIG", [128, TB, 128])
    ED = P.sb("ED", [128, TB, 128])
    EDT = P.sb("EDT", [128, TB, 128])
    EGB = P.sb("EGB", [128, TB, 128])
    LF = P.sb("LF", [128, TB, 128])
    Nn = [P.sb(f"Nn{i}", [128, TB, 128]) for i in range(2)]
    Mm = [P.sb(f"Mm{i}", [128, TB, 128]) for i in range(2)]
    LOFF = P.sb("LOFF", [128, TB, 128])
    LOFFT = P.sb("LOFFT", [128, TB, 128])
    Tt = P.sb("Tt", [128, TB, 128])
    R = P.sb("Rr", [128, TB, 256])
    SOL = P.sb("SOL", [128, TB, 256])
    KDEC = P.sb("KDEC", [128, TB, 128], BF16)
    WT = P.sb("WT", [128, TB, 128], BF16)
    INTRAT = P.sb("INTRAT", [128, TB, 128], BF16)
    QDT = P.sb("QDT", [128, TB, 128], BF16)
    VNEW = [P.sb(f"VNEW{i}", [128, 128], BF16) for i in range(2)]
    P.op("dve", lambda e: e.memset(OBP[:, 0:3], 0.0), writes=[("OB", -1)])

    def ck(nm, n):
        return (nm, n)
    QK4 = lambda nm: [ck(nm, n) for n in range(NCH)]

    def load_w4(hh):
        for f in range(4):
            c0 = f * D + hh * 128
            P.dma("pool", W4[:, f, :, :], din["a_w_in"][:, c0:c0 + 128].rearrange("(c p) f -> p c f", p=128), writes=["W4"], key="W4")

    load_w4(0)
    for h in range(8):
        P.dma("pool", WO[:], din["a_w_out"][h * 128:(h + 1) * 128, :], writes=["dnWO"], key="dnWO")
        for f in range(3):
            for j in range(4):
                ci = f * 8 + h
                P.op("dve", lambda e: e.tensor_scalar(out=DG[:, f, j, :], in0=ident[:], scalar1=CW[:, j * 24 + ci:j * 24 + ci + 1], scalar2=None, op0=ALU.mult),
                     reads=["ident", "CW"], writes=["DG"])
        for f in range(3):
            dst, dk = ((QF, "QF"), (KF, "KF"), (VF, "VF"))[f]
            for n in range(NCH):
                sl = slice(n * 512, (n + 1) * 512)
                ps, pk = _nps(k)
                for c in range(KC):
                    P.op("pe", lambda e: e.matmul(ps[:], W4[:, f, c, :], k.XB[:, c, sl], start=(c == 0), stop=(c == KC - 1)),
                         reads=["W4", ("XB", n)], writes=[pk])
                P.op("act", lambda e: e.activation(out=PREb[:, 3 + n * 512:3 + (n + 1) * 512], in_=ps[:], func=AF.Copy), reads=[pk], writes=[ck("OB", n)])
            for n in range(NCH):
                sl = slice(n * 512, (n + 1) * 512)
                ps, pk = _nps(k)
                for j in range(4):
                    P.op("pe", lambda e: e.matmul(ps[:], DG[:, f, j, :], PREb[:, n * 512 + j:n * 512 + j + 512], start=(j == 0), stop=(j == 3)),
                         reads=["DG", ck("OB", n), ck("OB", n - 1)], writes=[pk])
                P.op("act", lambda e: e.activation(out=dst[:, sl], in_=ps[:], func=AF.Silu), reads=[pk], writes=[ck(dk, n)])
            if f < 2:
                for n in range(NCH):
                    sl = slice(n * 512, (n + 1) * 512)
                    P.op("act", lambda e: e.activation(out=VF[:, sl], in_=dst[:, sl], func=AF.Square), reads=[ck(dk, n)], writes=[ck("VF", n)])
                    ps, pk = _nps(k)
                    P.op("pe", lambda e: e.matmul(ps[:], ones[:], VF[:, sl], start=True, stop=True), reads=["ones", ck("VF", n)], writes=[pk])
                    qs = 128.0 if f == 0 else 1.0
                    P.op("dve", lambda e: e.tensor_scalar(out=RS1[:], in0=ps[:], scalar1=qs, scalar2=DN_EPS * qs, op0=ALU.mult, op1=ALU.add), reads=[pk], writes=["RS1"])
                    P.op("act", lambda e: e.activation(out=RS2[:], in_=RS1[:], func=AF.Sqrt), reads=["RS1"], writes=["RS2"])
                    P.op("dve", lambda e: e.reciprocal(out=RS1[:], in_=RS2[:]), reads=["RS2"], writes=["RS1"])
                    P.op("pool", lambda e: e.tensor_tensor(out=dst[:, sl], in0=dst[:, sl], in1=RS1[:], op=ALU.mult), reads=[ck(dk, n), "RS1"], writes=[ck(dk, n)])
        for n in range(NCH):
            sl = slice(n * 512, (n + 1) * 512)
            ps, pk = _nps(k)
            for c in range(KC):
                P.op("pe", lambda e: e.matmul(ps[:], W4[:, 3, c, :], k.XB[:, c, sl], start=(c == 0), stop=(c == KC - 1)),
                     reads=["W4", ("XB", n)], writes=[pk])
            P.op("act", lambda e: e.activation(out=SG[:, sl], in_=ps[:], func=AF.Silu), reads=[pk], writes=[ck("SG", n)])
        P.op("dve", lambda e: e.memset(Sst[:], 0.0), writes=["Sst"])
        P.op("dve", lambda e: e.memset(Sb[:], 0.0), writes=["Sb"])
        if h + 1 < 8:
            load_w4(h + 1)
        if DN_STOP == "A":
            P.pop()
            return

        for b in range(NT // TB):
            t0 = b * TB

            def bc(T):
                return T[:, t0:t0 + TB, h:h + 1].broadcast_to([128, TB, 128])

            def tl(i):
                return slice((t0 + i) * 128, (t0 + i + 1) * 128)

            def cs(i):
                return slice(i * 128, (i + 1) * 128)
            psk, pkk = _nps(k)
            psv, pkv = _nps(k)
            for i in range(TB):
                P.op("pe", lambda e: e.transpose(psk[:, cs(i)], KF[:, tl(i)], ident[:]), reads=[ck("KF", (t0 + i) // 4), "ident"], writes=[pkk])
                P.op("pe", lambda e: e.transpose(psv[:, cs(i)], VF[:, tl(i)], ident[:]), reads=[ck("VF", (t0 + i) // 4), "ident"], writes=[pkv])
            P.op("dve", lambda e: e.tensor_tensor(out=KDEC[:], in0=v3(psk[:, 0:W]), in1=bc(EKD), op=ALU.mult), reads=[pkk, "EKD"], writes=["KDEC"])
            P.op("dve", lambda e: e.tensor_tensor(out=R[:, :, 128:256], in0=v3(psk[:, 0:W]), in1=bc(BEG), op=ALU.mult), reads=[pkk, "BEG"], writes=["Rr"])
            P.op("dve", lambda e: e.tensor_tensor(out=R[:, :, 0:128], in0=v3(psv[:, 0:W]), in1=bc(BETA), op=ALU.mult), reads=[pkv, "BETA"], writes=["Rr"])
            P.op("pool", lambda e: e.tensor_tensor(out=TRIG[:], in0=b3(TRI_LE), in1=bc(G), op=ALU.mult), reads=["tri_le", "G"], writes=["TRIG"])
            psd, pkd = _nps(k)
            psdt, pkdt = _nps(k)
            psg, pkg = _nps(k)
            for i in range(TB):
                P.op("pe", lambda e: e.matmul(psd[:, cs(i)], TRIG[:, i, :], TRI_GT[:], start=True, stop=True), reads=["TRIG", "tri_gt"], writes=[pkd])
            TRIGf = TRIG[:].rearrange("p t n -> p (t n)")
            P.op("pe", lambda e: e.matmul(psdt[:, 0:W], TRI_GT[:], TRIGf, start=True, stop=True), reads=["TRIG", "tri_gt"], writes=[pkdt])
            P.op("pe", lambda e: e.matmul(psg[:, 0:W], ones[:], TRIGf, start=True, stop=True), reads=["TRIG", "ones"], writes=[pkg])
            P.op("act", lambda e: e.activation(out=ED[:], in_=v3(psd[:, 0:W]), func=AF.Exp), reads=[pkd], writes=["ED"])
            P.op("act", lambda e: e.activation(out=EDT[:], in_=v3(psdt[:, 0:W]), func=AF.Exp), reads=[pkdt], writes=["EDT"])
            P.op("act", lambda e: e.activation(out=EGB[:], in_=v3(psg[:, 0:W]), func=AF.Exp), reads=[pkg], writes=["EGB"])
            pskk, pkkk = _nps(k)
            psqk, pkqk = _nps(k)
            for i in range(TB):
                P.op("pe", lambda e: e.matmul(pskk[:, cs(i)], KF[:, tl(i)], KF[:, tl(i)], start=True, stop=True), reads=[ck("KF", (t0 + i) // 4)], writes=[pkkk])
                P.op("pe", lambda e: e.matmul(psqk[:, cs(i)], KF[:, tl(i)], QF[:, tl(i)], start=True, stop=True), reads=[ck("KF", (t0 + i) // 4), ck("QF", (t0 + i) // 4)], writes=[pkqk])
            P.op("pool", lambda e: e.tensor_tensor(out=ED[:], in0=ED[:], in1=bc(BETA), op=ALU.mult), reads=["ED", "BETA"], writes=["ED"])
            P.op("dve", lambda e: e.tensor_tensor(out=LF[:], in0=v3(pskk[:, 0:W]), in1=ED[:], op=ALU.mult), reads=[pkkk, "ED"], writes=["LF"])
            P.op("pool", lambda e: e.tensor_tensor(out=Nn[0][:], in0=LF[:], in1=b3(NMBD), op=ALU.mult), reads=["LF", "nmask_bd"], writes=["Nn0"])
            P.op("pool", lambda e: e.tensor_tensor(out=LOFF[:], in0=LF[:], in1=b3(MOFF), op=ALU.mult), reads=["LF", "mask_off"], writes=["LOFF"])
            P.op("pool", lambda e: e.tensor_tensor(out=EDT[:], in0=EDT[:], in1=b3(TRI_LE), op=ALU.mult), reads=["EDT", "tri_le"], writes=["EDT"])
            P.op("dve", lambda e: e.tensor_tensor(out=INTRAT[:], in0=v3(psqk[:, 0:W]), in1=EDT[:], op=ALU.mult), reads=[pkqk, "EDT"], writes=["INTRAT"])
            P.op("pool", lambda e: e.tensor_tensor(out=QDT[:], in0=v3(QF[:, t0 * 128:(t0 + TB) * 128]), in1=EGB[:], op=ALU.mult), reads=[ck("QF", t0 // 4), "EGB"], writes=["QDT"])
            if DN_STOP == "B1":
                P.pop()
                return
            psa, pka = _nps(k)
            psb, pkb = _nps(k)
            for i in range(TB):
                P.op("pe", lambda e: e.transpose(psa[:, cs(i)], Nn[0][:, i, :], ident[:]), reads=["Nn0", "ident"], writes=[pka])
                P.op("pe", lambda e: e.transpose(psb[:, cs(i)], LOFF[:, i, :], ident[:]), reads=["LOFF", "ident"], writes=[pkb])
            P.op("act", lambda e: e.activation(out=Mm[0][:], in_=v3(psa[:, 0:W]), func=AF.Copy), reads=[pka], writes=["Mm0"])
            P.op("act", lambda e: e.activation(out=LOFFT[:], in_=v3(psb[:, 0:W]), func=AF.Copy), reads=[pkb], writes=["LOFFT"])
            P.op("dve", lambda e: e.tensor_tensor(out=Tt[:], in0=Mm[0][:], in1=b3(ident), op=ALU.add), reads=["Mm0", "ident"], writes=["Tt"])
            if DN_STOP == "T":
                P.pop()
                return
            for kk in range(1, 6):
                a, bb = (kk - 1) % 2, kk % 2
                if DN_STOP == f"kk{kk}":
                    P.pop()
                    return
                psn, pkn = _nps(k)
                for i in range(TB):
                    P.op("pe", lambda e: e.matmul(psn[:, cs(i)], Mm[a][:, i, :], Nn[a][:, i, :], start=True, stop=True), reads=[f"Mm{a}", f"Nn{a}"], writes=[pkn])
                if kk < 5:
                    psm, pkm = _nps(k)
                    for i in range(TB):
                        P.op("pe", lambda e: e.matmul(psm[:, cs(i)], Nn[a][:, i, :], Mm[a][:, i, :], start=True, stop=True), reads=[f"Mm{a}", f"Nn{a}"], writes=[pkm])
                P.op("act", lambda e: e.activation(out=Nn[bb][:], in_=v3(psn[:, 0:W]), func=AF.Copy), reads=[pkn], writes=[f"Nn{bb}"])
                if kk < 5:
                    P.op("dve", lambda e: e.tensor_copy(out=Mm[bb][:], in_=v3(psm[:, 0:W])), reads=[pkm], writes=[f"Mm{bb}"])
                psc, pkc = _nps(k)
                for i in range(TB):
                    P.op("pe", lambda e: e.matmul(psc[:, cs(i)], Nn[bb][:, i, :], Tt[:, i, :], start=True, stop=True), reads=[f"Nn{bb}", "Tt"], writes=[pkc])
                P.op("dve", lambda e: e.tensor_tensor(out=Tt[:], in0=v3(psc[:, 0:W]), in1=Tt[:], op=ALU.add), reads=[pkc, "Tt"], writes=["Tt"])
            if DN_STOP == "B2":
                P.pop()
                return
            v256 = lambda ap: ap.rearrange("p (t n) -> p t n", t=2)
            for hp in range(TB // 2):
                pss, pks = _nps(k)
                for i2 in range(2):
                    i = 2 * hp + i2
                    P.op("pe", lambda e: e.matmul(pss[:, i2 * 256:(i2 + 1) * 256], Tt[:, i, :], R[:, i, :], start=True, stop=True), reads=["Tt", "Rr"], writes=[pks])
                P.op("act", lambda e: e.activation(out=SOL[:, 2 * hp:2 * hp + 2, :], in_=v256(pss[:]), func=AF.Copy), reads=[pks], writes=["SOL"])
            for hp in range(TB // 2):
                psz, pkz = _nps(k)
                for i2 in range(2):
                    i = 2 * hp + i2
                    P.op("pe", lambda e: e.matmul(psz[:, i2 * 256:(i2 + 1) * 256], LOFFT[:, i, :], SOL[:, i, :], start=True, stop=True), reads=["LOFFT", "SOL"], writes=[pkz])
                P.op("act", lambda e: e.activation(out=R[:, 2 * hp:2 * hp + 2, :], in_=v256(psz[:]), func=AF.Copy), reads=[pkz], writes=["Rr"])
            for hp in range(TB // 2):
                psc2, pkc2 = _nps(k)
                for i2 in range(2):
                    i = 2 * hp + i2
                    P.op("pe", lambda e: e.matmul(psc2[:, i2 * 256:(i2 + 1) * 256], Tt[:, i, :], R[:, i, :], start=True, stop=True), reads=["Tt", "Rr"], writes=[pkc2])
                P.op("dve", lambda e: e.scalar_tensor_tensor(out=SOL[:, 2 * hp:2 * hp + 2, :], in0=v256(psc2[:]), scalar=-1.0, in1=SOL[:, 2 * hp:2 * hp + 2, :], op0=ALU.mult, op1=ALU.add),
                     reads=[pkc2, "SOL"], writes=["SOL"])
            psw, pkw = _nps(k)
            for i in range(TB):
                P.op("pe", lambda e: e.transpose(psw[:, cs(i)], SOL[:, i, 128:256], ident[:]), reads=["SOL", "ident"], writes=[pkw])
            P.op("act", lambda e: e.activation(out=WT[:], in_=v3(psw[:, 0:W]), func=AF.Copy), reads=[pkw], writes=["WT"])
            if DN_STOP == "B3":
                P.pop()
                return
            for i in range(TB):
                t = t0 + i
                vn = VNEW[t % 2]
                vk = f"VNEW{t % 2}"
                ps1, pk1 = _nps(k)
                P.op("pe", lambda e: e.matmul(ps1[:, 0:128], WT[:, i, :], Sb[:], start=True, stop=True), reads=["WT", "Sb"], writes=[pk1])
                P.op("dve", lambda e: e.scalar_tensor_tensor(out=vn[:], in0=ps1[:, 0:128], scalar=-1.0, in1=SOL[:, i, 0:128], op0=ALU.mult, op1=ALU.add),
                     reads=[pk1, "SOL"], writes=[vk])
                ps2, pk2 = _nps(k)
                P.op("pe", lambda e: e.matmul(ps2[:, 0:128], Sb[:], QDT[:, i, :], start=True, stop=False), reads=["Sb", "QDT"], writes=[pk2])
                P.op("pe", lambda e: e.matmul(ps2[:, 0:128], vn[:], INTRAT[:, i, :], start=False, stop=True), reads=[vk, "INTRAT"], writes=[pk2])
                P.op("act", lambda e: e.activation(out=OT[:, t * 128:(t + 1) * 128], in_=ps2[:, 0:128], func=AF.Copy), reads=[pk2], writes=[ck("OT", t // 4)])
                ps3, pk3 = _nps(k)
                P.op("pe", lambda e: e.matmul(ps3[:, 0:128], KDEC[:, i, :], vn[:], start=True, stop=True), reads=["KDEC", vk], writes=[pk3])
                P.op("dve", lambda e: e.scalar_tensor_tensor(out=Sst[:], in0=Sst[:], scalar=EGL[:, t, h:h + 1], in1=ps3[:, 0:128], op0=ALU.mult, op1=ALU.add),
                     reads=[pk3, "Sst", "EGL"], writes=["Sst"])
                P.op("act", lambda e: e.activation(out=Sb[:], in_=Sst[:], func=AF.Copy), reads=["Sst"], writes=["Sb"])
        if DN_STOP == "B4":
            P.pop()
            return
        if DN_STOP == "dump":
            for nm, T, kk_ in (("QF", QF, "QF"), ("KF", KF, "KF"), ("VF", VF, "VF")):
                P.dma("sp", k.dbg[nm], T[:], reads=QK4(kk_), writes=["dbg"], key="dbg")
            P.dma("sp", k.dbg["OT"], OT[:], reads=QK4("OT"), writes=["dbg"], key="dbg")
            for nm, T in (("G", G), ("BETA", BETA), ("EKD", EKD), ("BEG", BEG), ("EGL", EGL)):
                P.dma("sp", k.dbg[nm], T[:], reads=[nm], writes=["dbg"], key="dbg")
            P.pop()
            return
        for n in range(NCH):
            sl = slice(n * 512, (n + 1) * 512)
            P.op("act", lambda e: e.activation(out=VF[:, sl], in_=OT[:, sl], func=AF.Square), reads=[ck("OT", n)], writes=[ck("VF", n)])
            ps, pk = _nps(k)
            P.op("pe", lambda e: e.matmul(ps[:], ones[:], VF[:, sl], start=True, stop=True), reads=["ones", ck("VF", n)], writes=[pk])
            P.op("dve", lambda e: e.tensor_scalar(out=RS1[:], in0=ps[:], scalar1=1.0 / 128.0, scalar2=DN_EPS, op0=ALU.mult, op1=ALU.add), reads=[pk], writes=["RS1"])
            P.op("act", lambda e: e.activation(out=RS2[:], in_=RS1[:], func=AF.Sqrt), reads=["RS1"], writes=["RS2"])
            P.op("dve", lambda e: e.reciprocal(out=RS1[:], in_=RS2[:]), reads=["RS2"], writes=["RS1"])
            P.op("pool", lambda e: e.tensor_tensor(out=VF[:, sl], in0=OT[:, sl], in1=RS1[:], op=ALU.mult), reads=[ck("OT", n), "RS1", ck("VF", n)], writes=[ck("VF", n)])
            P.op("dve", lambda e: e.scalar_tensor_tensor(out=OB[:, sl], in0=VF[:, sl], scalar=NW[:, 0:1], in1=SG[:, sl], op0=ALU.mult, op1=ALU.mult),
                 reads=[ck("VF", n), "NW", ck("SG", n)], writes=[ck("OB", n)])
        for n in range(NCH):
            sl = slice(n * 512, (n + 1) * 512)
            for m in range(KC):
                ps, pk = _nps(k)
                P.op("pe", lambda e: e.matmul(ps[:], WO[:, m * 128:(m + 1) * 128], OB[:, sl], start=True, stop=True), reads=["dnWO", ck("OB", n)], writes=[pk])
                P.op("dve", lambda e: e.tensor_tensor(out=k.XT[:, m, sl], in0=ps[:], in1=k.XT[:, m, sl], op=ALU.add), reads=[pk, ("XT", n)], writes=[("XT", n)])
    P.pop()
    P.push()
    k.layer_norm(0, k.ln_tmp())
    P.pop()


def from_moba(k):
    P = k.P
    din = k.din
    dc = k.dc
    ident, ones, onesb = k.ident, k.ones, k.onesb
    SCALE = 128.0 ** -0.5
    BIG = 30000.0
    P.push()

    def cload(name, src, shape, dt=F32, q="sp"):
        t = P.sb(name, shape, dt)
        P.dma(q, t[:], src, writes=[name], key="mbc")
        return t
    PERM = cload("perm", dc["perm"], [128, 128])
    ROPEC = cload("ropec", dc["ropec"], [128, S])
    ROPES = cload("ropes", dc["ropes"], [128, S])
    PASTB = cload("pastb", dc["pastb"], [128, NT, 8])
    PAST01 = cload("past01", dc["past01"], [128, NT, 8])
    OWN01 = cload("own01", dc["own01"], [128, NT, 8])
    SEL8b = cload("sel8b", dc["sel8"], [8, 8, 128], BF16, q="pool")
    CAUS = cload("caus4", dc["caus4"], [128, 4, 512], BF16, q="pool")

    k.scale_xt()

    Wq = P.sb("mWq", [128, KC, 128], BF16)
    Wk = P.sb("mWk", [128, KC, 128], BF16)
    Wv = P.sb("mWv", [128, KC, 128], BF16)
    WO = P.sb("mWO", [128, D], BF16)
    QF = P.sb("mQF", [128, S])
    KF = P.sb("mKF", [128, S])
    QB = P.sb("mQB", [128, S], BF16)
    KB = P.sb("mKB", [128, S], BF16)
    VTM = P.sb("mVTM", [128, NT, 128], BF16)
    OB = P.sb("mOB", [128, S], BF16)
    RAW = [P.sb(f"mRAW{i}", [128, 512]) for i in range(2)]
    T1 = P.sb("mT1", [128, 512])
    T2 = P.sb("mT2", [128, 512])
    PT = [P.sb(f"mPT{i}", [128, 512], BF16) for i in range(2)]
    RL = P.sb("mRL", [128, 512])
    KM = P.sb("mKM", [128, 8])
    GATE = P.sb("mGATE", [128, NT, 8])
    M8 = P.sb("mM8", [128, NT, 8])
    SEL = P.sb("mSEL", [128, NT, 8])
    BIAST = P.sb("mBIAST", [8, S], BF16)
    ri = 0
    pi = 0
    def load_w(hh):
        P.dma("pool", Wq[:], din["b_w_q"][:, hh * 128:(hh + 1) * 128].rearrange("(c p) f -> p c f", p=128), writes=["mWq"], key="mWq")
        P.dma("pool", Wk[:], din["b_w_kv"][:, hh * 128:(hh + 1) * 128].rearrange("(c p) f -> p c f", p=128), writes=["mWk"], key="mWk")
        P.dma("pool", Wv[:], din["b_w_kv"][:, D + hh * 128:D + (hh + 1) * 128].rearrange("(c p) f -> p c f", p=128), writes=["mWv"], key="mWv")

    load_w(0)
    for h in range(8):
        P.dma("pool", WO[:], din["b_w_o"][h * 128:(h + 1) * 128, :], writes=["mWO"], key="mWO")
        jobs = [(Wt, wk_, Ff, fk, Bb, bk, n) for (Wt, wk_, Ff, fk, Bb, bk) in ((Wq, "mWq", QF, "mQF", QB, "mQB"), (Wk, "mWk", KF, "mKF", KB, "mKB"))
                for n in range(NCH)]

        def r1(job):
            Wt, wk_, Ff, fk, Bb, bk, n = job
            nonlocal ri
            sl = slice(n * 512, (n + 1) * 512)
            ps, pk = _nps(k)
            for c in range(KC):
                P.op("pe", lambda e: e.matmul(ps[:], Wt[:, c, :], k.XB[:, c, sl], start=(c == 0), stop=(c == KC - 1)), reads=[wk_, ("XB", n)], writes=[pk])
            raw, rk = RAW[ri % 2], f"mRAW{ri % 2}"
            ri += 1
            P.op("act", lambda e: e.activation(out=raw[:], in_=ps[:], func=AF.Copy), reads=[pk], writes=[rk])
            return raw, rk

        def r2(job, raw, rk):
            Wt, wk_, Ff, fk, Bb, bk, n = job
            sl = slice(n * 512, (n + 1) * 512)
            ps2, pk2 = _nps(k)
            P.op("pe", lambda e: e.matmul(ps2[:], PERM[:], raw[:], start=True, stop=True), reads=["perm", rk], writes=[pk2])
            P.op("dve", lambda e: e.tensor_tensor(out=T1[:], in0=ps2[:], in1=ROPES[:, sl], op=ALU.mult), reads=[pk2, "ropes"], writes=["mT1"])
            P.op("pool", lambda e: e.tensor_tensor(out=T2[:], in0=raw[:], in1=ROPEC[:, sl], op=ALU.mult), reads=[rk, "ropec"], writes=["mT2"])
            P.op("pool", lambda e: e.tensor_tensor(out=Ff[:, sl], in0=T1[:], in1=T2[:], op=ALU.add), reads=["mT1", "mT2"], writes=[fk])
            P.op("act", lambda e: e.activation(out=Bb[:, sl], in_=Ff[:, sl], func=AF.Copy), reads=[fk], writes=[bk])

        cur = r1(jobs[0])
        for ji in range(len(jobs)):
            nxt = r1(jobs[ji + 1]) if ji + 1 < len(jobs) else None
            r2(jobs[ji], *cur)
            cur = nxt
        P.op("dve", lambda e: e.tensor_reduce(out=KM[:], in_=KF[:].rearrange("p (b n) -> p b n", n=256), axis=AX.X, op=ALU.add), reads=["mKF"], writes=["mKM"])
        P.op("dve", lambda e: e.tensor_scalar(out=KM[:], in0=KM[:], scalar1=1.0 / 256.0, scalar2=None, op0=ALU.mult), reads=["mKM"], writes=["mKM"])
        for g in range(4):
            ps, pk = _nps(k)
            for i in range(4):
                t = 4 * g + i
                for c in range(KC):
                    P.op("pe", lambda e: e.matmul(ps[:, i * 128:(i + 1) * 128], k.XB[:, c, t * 128:(t + 1) * 128], Wv[:, c, :], start=(c == 0), stop=(c == KC - 1)),
                         reads=["mWv", ("XB", g)], writes=[pk])
            P.op("act", lambda e: e.activation(out=VTM[:, 4 * g:4 * g + 4, :], in_=ps[:].rearrange("p (t n) -> p t n", t=4), func=AF.Copy), reads=[pk], writes=["mVTM"])
        if h + 1 < 8:
            load_w(h + 1)
        ps, pk = _nps(k)
        for t in range(NT):
            P.op("pe", lambda e: e.matmul(ps[:, t * 8:(t + 1) * 8], QF[:, t * 128:(t + 1) * 128], KM[:], start=True, stop=True), reads=["mQF", "mKM"], writes=[pk])
        P.op("dve", lambda e: e.tensor_tensor(out=GATE[:], in0=ps[:, 0:NT * 8].rearrange("p (t n) -> p t n", n=8), in1=PASTB[:], op=ALU.add), reads=[pk, "pastb"], writes=["mGATE"])
        for t in range(NT):
            P.op("dve", lambda e: e.max(out=M8[:, t, :], in_=GATE[:, t, :]), reads=["mGATE"], writes=["mM8"])
        P.op("dve", lambda e: e.tensor_tensor(out=SEL[:], in0=GATE[:], in1=M8[:, :, 2:3].broadcast_to([128, NT, 8]), op=ALU.is_ge), reads=["mGATE", "mM8"], writes=["mSEL"])
        P.op("dve", lambda e: e.tensor_tensor(out=SEL[:], in0=SEL[:], in1=PAST01[:], op=ALU.mult), reads=["mSEL", "past01"], writes=["mSEL"])
        P.op("dve", lambda e: e.tensor_tensor(out=SEL[:], in0=SEL[:], in1=OWN01[:], op=ALU.add), reads=["mSEL", "own01"], writes=["mSEL"])
        P.op("dve", lambda e: e.tensor_scalar(out=SEL[:], in0=SEL[:], scalar1=-1.0, scalar2=BIG, op0=ALU.add, op1=ALU.mult), reads=["mSEL"], writes=["mSEL"])
        for g in range(4):
            ps, pk = _nps(k)
            for i in range(4):
                t = 4 * g + i
                P.op("pe", lambda e: e.transpose(ps[0:8, i * 128:(i + 1) * 128], SEL[:, t, :], ident[:]), reads=["mSEL", "ident"], writes=[pk])
            P.op("act", lambda e: e.activation(out=BIAST[:, g * 512:(g + 1) * 512], in_=ps[0:8, :], func=AF.Copy), reads=[pk], writes=["mBIAST"])
        for qc in range(NCH):
            sl = slice(qc * 512, (qc + 1) * 512)
            pso, pko = _nps(k)
            psl, pkl = _nps(k)
            njt = 4 * qc + 4

            def s_stage(jt):
                while k.psi in (int(pko[2:]), int(pkl[2:])):
                    k.psi = (k.psi + 1) % 8
                pss, pks = _nps(k)
                P.op("pe", lambda e: e.matmul(pss[:], KB[:, jt * 128:(jt + 1) * 128], QB[:, sl], start=True, stop=False), reads=["mKB", "mQB"], writes=[pks])
                P.op("pe", lambda e: e.matmul(pss[:], SEL8b[:, jt // 2, :], BIAST[:, sl], start=False, stop=True), reads=["sel8b", "mBIAST"], writes=[pks])
                return pss, pks

            cur = s_stage(0)
            for jt in range(njt):
                nxt = s_stage(jt + 1) if jt + 1 < njt else None
                pss, pks = cur
                pt, ptk = PT[pi % 2], f"mPT{pi % 2}"
                pi += 1
                P.op("act", lambda e: e.activation(out=pt[:], in_=pss[:], func=AF.Exp, scale=SCALE), reads=[pks], writes=[ptk])
                if jt >= 4 * qc:
                    P.op("pool", lambda e: e.tensor_tensor(out=pt[:], in0=pt[:], in1=CAUS[:, jt - 4 * qc, :], op=ALU.mult), reads=[ptk, "caus4"], writes=[ptk])
                P.op("pe", lambda e: e.matmul(pso[:], VTM[:, jt, :], pt[:], start=(jt == 0), stop=(jt == njt - 1)), reads=["mVTM", ptk], writes=[pko])
                P.op("pe", lambda e: e.matmul(psl[:], onesb[:], pt[:], start=(jt == 0), stop=(jt == njt - 1)), reads=["onesb", ptk], writes=[pkl])
                cur = nxt
            P.op("dve", lambda e: e.reciprocal(out=RL[:], in_=psl[:]), reads=[pkl], writes=["mRL"])
            P.op("dve", lambda e: e.tensor_tensor(out=OB[:, sl], in0=pso[:], in1=RL[:], op=ALU.mult), reads=[pko, "mRL"], writes=["mOB"])
        for m in range(KC):
            for n in range(NCH):
                sl = slice(n * 512, (n + 1) * 512)
                ps, pk = _nps(k)
                P.op("pe", lambda e: e.matmul(ps[:], WO[:, m * 128:(m + 1) * 128], OB[:, sl], start=True, stop=True), reads=["mWO", "mOB"], writes=[pk])
                P.op("dve", lambda e: e.tensor_tensor(out=k.XT[:, m, sl], in0=ps[:], in1=k.XT[:, m, sl], op=ALU.add), reads=[pk, ("XT", n)], writes=[("XT", n)])
    P.pop()
    P.push()
    k.layer_norm(2, k.ln_tmp())
    P.pop()


def make_in_maps(inputs, n_cores=8):
    c = _consts()
    shared = {}
    for name in IN_SHAPES:
        if name == "x":
            continue
        a = np.ascontiguousarray(np.asarray(inputs[name], dtype=np.float32))
        shared[name] = a.reshape(IN_SHAPES[name])
    for name, v in c.items():
        shared["c_" + name] = np.ascontiguousarray(v)
    x = np.asarray(inputs["x"], dtype=np.float32)
    maps = []
    for b in range(n_cores):
        m = dict(shared)
        m["x"] = np.ascontiguousarray(x[b])
        maps.append(m)
    return maps


def kernel(**inputs):
    nc = build()
    maps = make_in_maps(inputs)
    res = run_bass_kernel_spmd(nc, maps, core_ids=list(range(8)))
    return np.stack([np.asarray(r["out"], dtype=np.float32) for r in res.results], axis=0)
```

```python
from contextlib import ExitStack
import numpy as np
import concourse.bass as bass
import concourse.mybir as mybir
from concourse.bass_utils import run_bass_kernel_spmd

F32 = mybir.dt.float32
BF16 = mybir.dt.bfloat16
ALU = mybir.AluOpType
AF = mybir.ActivationFunctionType
AX = mybir.AxisListType

S = 2048
D = 1024
NT = 16
NCH = 4
KC = 8
FH = 3584
NF = 28
NE = 8
ALPHA = 4.0 ** 0.25
LN_EPS = 1e-5
DN_EPS = 1e-6
SEM_ROLL = 20000


class Prog:
    ENG = ("pe", "act", "dve", "pool", "sp")

    def __init__(self, nc):
        self.nc = nc
        self.es = ExitStack()
        self.scopes = [self.es]
        self.e = {"pe": nc.tensor, "act": nc.scalar, "dve": nc.vector, "pool": nc.gpsimd, "sp": nc.sync}
        self.sem = {}
        self.cnt = {}
        self.nsem = 0
        for k in self.ENG:
            self._newsem(k)
        self.waited = {k: {} for k in self.ENG}
        self.last_w = {}
        self.readers = {}
        self.dsem = {}
        self.group_reads = []
        self.n_inst = 0

    def _newsem(self, k):
        name = f"s_{k}_{self.nsem}"
        s = self.es.enter_context(self.nc.semaphore(name))
        self.nsem += 1
        self.sem[k] = (name, s)
        self.cnt[k] = 0

    def sb(self, name, shape, dt=F32):
        return self.scopes[-1].enter_context(self.nc.sbuf_tensor(name, list(shape), dt))

    def push(self):
        self.scopes.append(ExitStack())

    def pop(self):
        self.barrier()
        self.scopes.pop().close()

    def ps(self, name, shape, dt=F32):
        return self.es.enter_context(self.nc.psum_tensor(name, list(shape), dt))

    def _deps(self, engine, reads, writes):
        need = {}

        def add(rec):
            name, s, val, e = rec
            if e == engine and engine == "pe":
                return
            if name not in need or need[name][1] < val:
                need[name] = (s, val)
        for r in list(reads) + self.group_reads:
            if r in self.last_w:
                add(self.last_w[r])
        for w in writes:
            if w in self.last_w:
                add(self.last_w[w])
            for rec in self.readers.get(w, ()):
                add(rec)
        for name, (s, val) in need.items():
            if self.waited[engine].get(name, 0) >= val:
                continue
            self.e[engine].wait_ge(s, val)
            self.waited[engine][name] = val

    def _record(self, rec, reads, writes):
        for r in reads:
            self.readers.setdefault(r, []).append(rec)
        for w in writes:
            self.last_w[w] = rec
            self.readers[w] = []

    def op(self, engine, fn, reads=(), writes=()):
        self._deps(engine, reads, writes)
        inst = fn(self.e[engine])
        name, s = self.sem[engine]
        self.cnt[engine] += 1
        val = self.cnt[engine]
        inst.then_inc(s, 1)
        self.n_inst += 1
        self._record((name, s, val, engine), reads, writes)
        if val >= SEM_ROLL:
            self._newsem(engine)
        return inst

    GROUPS = ("c0", "dnc", "mbc", "wr")

    def dma(self, queue, out, in_, reads=(), writes=(), key=None, **kw):
        if key in self.GROUPS:
            gk = ("grp", key)
            writes = list(writes) + [gk]
            if gk not in self.group_reads:
                self.group_reads.append(gk)
        self._deps(queue, reads, writes)
        if key not in self.dsem:
            name = f"d_{self.nsem}"
            s = self.es.enter_context(self.nc.semaphore(name))
            self.nsem += 1
            self.dsem[key] = [name, s, 0]
        ent = self.dsem[key]
        inst = self.e[queue].dma_start(out=out, in_=in_, **kw)
        ent[2] += 16
        inst.then_inc(ent[1], 16)
        self.n_inst += 1
        self._record((ent[0], ent[1], ent[2], "dma"), reads, writes)
        return inst

    def barrier(self):
        recs = []
        for k in self.ENG:
            name, s = self.sem[k]
            if self.cnt[k] > 0:
                recs.append((name, s, self.cnt[k], k))
        for key, ent in self.dsem.items():
            if ent[2] > 0:
                recs.append((ent[0], ent[1], ent[2], "dma"))
        for k in self.ENG:
            for name, s, val, src in recs:
                if self.waited[k].get(name, 0) >= val:
                    continue
                if src == k and k == "pe":
                    continue
                self.e[k].wait_ge(s, val)
                self.waited[k][name] = val

    def close(self):
        self.barrier()
        self.es.close()


def _consts():
    c = {}
    c["ident"] = np.eye(128, dtype=np.float32)
    c["ones"] = np.ones((128, 128), dtype=np.float32)
    half = 64
    inv_freq = (10000.0 ** (-(np.arange(half, dtype=np.float32) / np.float32(half)))).astype(np.float32)
    ang = (np.arange(S, dtype=np.float32)[:, None] * inv_freq[None, :]).astype(np.float32)
    cos = np.cos(ang.astype(np.float64)).astype(np.float32).T
    sin = np.sin(ang.astype(np.float64)).astype(np.float32).T
    c["ropec"] = np.concatenate([cos, cos], axis=0)
    c["ropes"] = np.concatenate([-sin, sin], axis=0)
    sel = np.zeros((8, 8, 128), dtype=np.float32)
    for e in range(8):
        sel[e, e, :] = 1.0
    c["sel8"] = sel.transpose(1, 0, 2).copy()
    r = np.arange(128)[:, None]
    q = np.arange(128)[None, :]
    c["tri_le"] = (r <= q).astype(np.float32)
    c["tri_gt"] = (r > q).astype(np.float32)
    c["nmask_bd"] = -((r > q) & ((r // 64) == (q // 64))).astype(np.float32)
    c["mask_off"] = ((r >= 64) & (q < 64)).astype(np.float32)
    c["perm"] = (r == ((q + 64) % 128)).astype(np.float32)
    pb = np.zeros((128, NT, 8), dtype=np.float32)
    p01 = np.zeros((128, NT, 8), dtype=np.float32)
    own = np.zeros((128, NT, 8), dtype=np.float32)
    for t in range(NT):
        qb = t // 2
        own[:, t, qb] = 1.0
        for n in range(8):
            if n < qb:
                p01[:, t, n] = 1.0
            else:
                pb[:, t, n] = -1.0e30
    c["pastb"] = pb
    c["past01"] = p01
    c["own01"] = own
    caus = np.ones((128, 4, 512), dtype=np.float32)
    j = np.arange(128)[:, None]
    ii = np.arange(512)[None, :]
    for pos in range(4):
        same = (pos // 2) == (ii // 256)
        caus[:, pos, :] = np.where(same & ((128 * pos + j) > ii), 0.0, 1.0)
    c["caus4"] = caus
    return c


CONST_SHAPES = {"ident": [128, 128], "ones": [128, 128], "ropec": [128, S], "ropes": [128, S],
                "sel8": [8, 8, 128], "tri_le": [128, 128], "tri_gt": [128, 128], "nmask_bd": [128, 128],
                "mask_off": [128, 128], "perm": [128, 128], "pastb": [128, NT, 8], "past01": [128, NT, 8],
                "own01": [128, NT, 8], "caus4": [128, 4, 512]}

IN_SHAPES = {
    "x": [S, D], "a_w_in": [D, 4112], "a_conv_w": [4, 3072], "a_log_decay": [1, 8], "a_dt_bias": [1, 8],
    "a_norm_w": [1, 128], "a_w_out": [D, D], "b_w_kv": [D, 2 * D], "b_w_q": [D, D], "b_w_o": [D, D],
    "ffn_w1": [D, FH], "ffn_w3": [D, FH], "ffn_w2": [FH, D], "moe_router": [D, 8],
    "moe_w1": [8, D, FH], "moe_w3": [8, D, FH], "moe_w2": [8, FH, D], "ln_g": [4, D], "ln_b": [4, D],
}


class K:
    def __init__(self, stages, dbg=()):
        self.stages = stages
        nc = bass.Bass("TRN2", target_bir_lowering=False)
        self.nc = nc
        self.P = Prog(nc)
        P = self.P
        self.din = {k: nc.dram_tensor(k, v, F32, kind="ExternalInput").ap() for k, v in IN_SHAPES.items()}
        self.dc = {k: nc.dram_tensor("c_" + k, v, F32, kind="ExternalInput").ap() for k, v in CONST_SHAPES.items()}
        self.out = nc.dram_tensor("out", [S, D], F32, kind="ExternalOutput").ap()
        self.dbg = {}
        for name, shape in dbg:
            self.dbg[name] = nc.dram_tensor("dbg_" + name, shape, F32, kind="ExternalOutput").ap()
        self.uid = 0
        self.XT = P.sb("XT", [128, KC, S])
        self.XB = P.sb("XB", [128, KC, S], BF16)
        self.ident = P.sb("ident", [128, 128])
        self.identb = P.sb("identb", [128, 128], BF16)
        self.ones = P.sb("ones", [128, 128])
        self.onesb = P.sb("onesb", [128, 128], BF16)
        self.lng = P.sb("lng", [128, 4, KC])
        self.lnb = P.sb("lnb", [128, 4, KC])
        self.PS = [P.ps(f"PS{i}", [128, 512]) for i in range(8)]
        self.psk = [f"PS{i}" for i in range(8)]

        P.dma("sp", self.ident[:], self.dc["ident"], writes=["ident"], key="c0")
        P.dma("sp", self.ones[:], self.dc["ones"], writes=["ones"], key="c0")
        lnraw = P.sb("lnraw", [64, 128])
        P.dma("sp", lnraw[0:32, :], self.din["ln_g"].rearrange("l (c p) -> (l c) p", p=128), writes=["lnraw"], key="c0")
        P.dma("sp", lnraw[32:64, :], self.din["ln_b"].rearrange("l (c p) -> (l c) p", p=128), writes=["lnraw"], key="c0")
        P.op("pe", lambda e: e.transpose(self.PS[0][:, 0:64], lnraw[:], self.ident[0:64, 0:64]), reads=["lnraw", "ident"], writes=["PS0"])
        P.op("dve", lambda e: e.tensor_copy(out=self.lng[:], in_=self.PS[0][:, 0:32].rearrange("p (l c) -> p l c", l=4)), reads=["PS0"], writes=["lng"])
        P.op("dve", lambda e: e.tensor_copy(out=self.lnb[:], in_=self.PS[0][:, 32:64].rearrange("p (l c) -> p l c", l=4)), reads=["PS0"], writes=["lnb"])
        P.op("dve", lambda e: e.tensor_copy(out=self.identb[:], in_=self.ident[:]), reads=["ident"], writes=["identb"])
        P.op("dve", lambda e: e.tensor_copy(out=self.onesb[:], in_=self.ones[:]), reads=["ones"], writes=["onesb"])
        self.lneps = P.sb("lneps", [128, 1])
        P.op("dve", lambda e: e.memset(self.lneps[:], LN_EPS), writes=["lneps"])

    def u(self, s):
        self.uid += 1
        return f"{s}{self.uid}"

    def load_x(self):
        P = self.P
        xin = [P.sb(f"xin{i}", [128, D]) for i in range(2)]
        for t in range(NT):
            b = xin[t % 2]
            bk = f"xin{t % 2}"
            P.dma("sp", b[:], self.din["x"][t * 128:(t + 1) * 128, :], writes=[bk], key=bk)
            for hlf in range(2):
                ps = self.PS[2 * (t % 2) + hlf]
                pk = self.psk[2 * (t % 2) + hlf]
                for c4 in range(4):
                    c = hlf * 4 + c4
                    P.op("pe", lambda e: e.transpose(ps[:, c4 * 128:(c4 + 1) * 128], b[:, c * 128:(c + 1) * 128], self.ident[:]),
                         reads=[bk, "ident"], writes=[pk])
                src = ps[:].rearrange("p (c n) -> p c n", c=4)
                P.op("act", lambda e: e.activation(out=self.XT[:, hlf * 4:(hlf + 1) * 4, t * 128:(t + 1) * 128], in_=src, func=AF.Copy),
                     reads=[pk], writes=[("XT", t // 4)])
                P.op("dve", lambda e: e.tensor_copy(out=self.XB[:, hlf * 4:(hlf + 1) * 4, t * 128:(t + 1) * 128],
                                                    in_=self.XT[:, hlf * 4:(hlf + 1) * 4, t * 128:(t + 1) * 128]),
                     reads=[("XT", t // 4)], writes=[("XB", t // 4)])

    def store_x(self):
        P = self.P
        xo = [P.sb(f"xo{i}", [128, D]) for i in range(2)]
        for t in range(NT):
            b = xo[t % 2]
            bk = f"xo{t % 2}"
            for hlf in range(2):
                ps = self.PS[2 * (t % 2) + hlf]
                pk = self.psk[2 * (t % 2) + hlf]
                for c4 in range(4):
                    c = hlf * 4 + c4
                    P.op("pe", lambda e: e.transpose(ps[:, c4 * 128:(c4 + 1) * 128], self.XT[:, c, t * 128:(t + 1) * 128], self.ident[:]),
                         reads=[("XT", t // 4), "ident"], writes=[pk])
                if hlf == 0:
                    P.op("act", lambda e: e.activation(out=b[:, 0:512], in_=ps[:], func=AF.Copy), reads=[pk], writes=[bk])
                else:
                    P.op("dve", lambda e: e.tensor_copy(out=b[:, 512:1024], in_=ps[:]), reads=[pk], writes=[bk])
            P.dma("sp", self.out[t * 128:(t + 1) * 128, :], b[:], reads=[bk], writes=["out"], key=bk)

    def dbg_store_xt(self, name):
        P = self.P
        for c in range(KC):
            P.dma("sp", self.dbg[name][c], self.XT[:, c, :], reads=[("XT", n) for n in range(NCH)], writes=["dbg"], key="dbg")

    def layer_norm(self, idx, tmp):
        P = self.P
        SQ, R1, R2, MEAN, RSTD = tmp["sq"], tmp["r1"], tmp["r2"], tmp["mean"], tmp["rstd"]
        for n in range(NCH):
            sl = slice(n * 512, (n + 1) * 512)
            xk = ("XT", n)
            Yv = self.XT[:, :, sl]
            P.op("act", lambda e: e.activation(out=SQ[:], in_=Yv, func=AF.Square), reads=[xk], writes=["ln_sq"])
            P.op("dve", lambda e: e.tensor_reduce(out=R1[:], in_=Yv.rearrange("p c n -> p n c"), axis=AX.X, op=ALU.add),
                 reads=[xk], writes=["ln_r1"])
            P.op("dve", lambda e: e.tensor_reduce(out=R2[:], in_=SQ[:].rearrange("p c n -> p n c"), axis=AX.X, op=ALU.add),
                 reads=["ln_sq"], writes=["ln_r2"])
            ps1, pk1 = self.PS[6], self.psk[6]
            ps2, pk2 = self.PS[7], self.psk[7]
            P.op("pe", lambda e: e.matmul(ps1[:], self.ones[:], R1[:], start=True, stop=True), reads=["ones", "ln_r1"], writes=[pk1])
            P.op("pe", lambda e: e.matmul(ps2[:], self.ones[:], R2[:], start=True, stop=True), reads=["ones", "ln_r2"], writes=[pk2])
            P.op("act", lambda e: e.activation(out=MEAN[:], in_=ps1[:], func=AF.Copy, scale=1.0 / D), reads=[pk1], writes=["ln_mean"])
            P.op("dve", lambda e: e.tensor_tensor(out=R1[:], in0=MEAN[:], in1=MEAN[:], op=ALU.mult), reads=["ln_mean"], writes=["ln_r1"])
            P.op("dve", lambda e: e.scalar_tensor_tensor(out=R2[:], in0=ps2[:], scalar=1.0 / D, in1=R1[:], op0=ALU.mult, op1=ALU.subtract),
                 reads=[pk2, "ln_r1"], writes=["ln_r2"])
            P.op("act", lambda e: e.activation(out=R1[:], in_=R2[:], func=AF.Ln, bias=self.lneps[:, 0:1]), reads=["ln_r2", "lneps"], writes=["ln_r1"])
            P.op("act", lambda e: e.activation(out=RSTD[:], in_=R1[:], func=AF.Exp, scale=-0.5), reads=["ln_r1"], writes=["ln_rstd"])
            mb = MEAN[:].unsqueeze(1).broadcast_to([128, KC, 512])
            rb = RSTD[:].unsqueeze(1).broadcast_to([128, KC, 512])
            P.op("dve", lambda e: e.tensor_tensor(out=SQ[:], in0=Yv, in1=mb, op=ALU.subtract), reads=[xk, "ln_mean"], writes=["ln_sq"])
            P.op("pool", lambda e: e.tensor_tensor(out=SQ[:], in0=SQ[:], in1=rb, op=ALU.mult), reads=["ln_sq", "ln_rstd"], writes=["ln_sq"])
            for c in range(KC):
                P.op("act", lambda e: e.activation(out=self.XT[:, c, sl], in_=SQ[:, c, :], func=AF.Identity,
                                                   scale=self.lng[:, idx, c:c + 1], bias=self.lnb[:, idx, c:c + 1]),
                     reads=["ln_sq", "lng", "lnb"], writes=[xk])
                P.op("dve", lambda e: e.tensor_scalar(out=self.XB[:, c, sl], in0=SQ[:, c, :], scalar1=self.lng[:, idx, c:c + 1],
                                                      scalar2=self.lnb[:, idx, c:c + 1], op0=ALU.mult, op1=ALU.add),
                     reads=["ln_sq", "lng", "lnb"], writes=[("XB", n)])

    def ln_tmp(self):
        P = self.P
        return {"sq": P.sb(self.u("ln_sq"), [128, KC, 512]), "r1": P.sb(self.u("ln_r1"), [128, 512]),
                "r2": P.sb(self.u("ln_r2"), [128, 512]), "mean": P.sb(self.u("ln_mean"), [128, 512]),
                "rstd": P.sb(self.u("ln_rstd"), [128, 512])}

    def scale_xt(self):
        P = self.P
        for n in range(NCH):
            sl = slice(n * 512, (n + 1) * 512)
            eng = "act" if n % 2 == 0 else "dve"
            if eng == "act":
                P.op("act", lambda e: e.activation(out=self.XT[:, :, sl], in_=self.XT[:, :, sl], func=AF.Copy, scale=ALPHA),
                     reads=[("XT", n)], writes=[("XT", n)])
            else:
                P.op("dve", lambda e: e.tensor_scalar(out=self.XT[:, :, sl], in0=self.XT[:, :, sl], scalar1=ALPHA, scalar2=None, op0=ALU.mult),
                     reads=[("XT", n)], writes=[("XT", n)])

    def ffn(self, experts, gb=None):
        P = self.P
        G = 4
        NB = 2
        W1 = [P.sb(self.u("w1g"), [128, KC, G * 128], BF16) for _ in range(NB)]
        W3 = [P.sb(self.u("w3g"), [128, KC, G * 128], BF16) for _ in range(NB)]
        W2 = [P.sb(self.u("w2g"), [128, G, D], BF16) for _ in range(NB)]
        H = [P.sb(self.u("hg"), [128, G, 512], BF16) for _ in range(2)]
        SA = [P.sb(self.u("sa"), [128, 512], BF16) for _ in range(2)]
        HT = [P.sb(self.u("ht"), [128, 512], BF16) for _ in range(2)]
        base = self.u("ffn")
        gi = 0
        si = 0
        hi = 0
        groups = [(ei, ex, fg) for ei, ex in enumerate(experts) for fg in range(NF // G)]

        def issue_load(k):
            ei, ex, fg = groups[k]
            w1, w3, w2, _ = ex
            slot = k % NB
            f0 = fg * G * 128
            P.dma("pool", W1[slot][:], w1[:, f0:f0 + G * 128].rearrange("(c p) f -> p c f", p=128),
                  writes=[(base, "w1", slot)], key=(base, "w1", slot))
            P.dma("pool", W3[slot][:], w3[:, f0:f0 + G * 128].rearrange("(c p) f -> p c f", p=128),
                  writes=[(base, "w3", slot)], key=(base, "w3", slot))
            P.dma("pool", W2[slot][:], w2[f0:f0 + G * 128, :].rearrange("(j p) m -> p j m", p=128),
                  writes=[(base, "w2", slot)], key=(base, "w2", slot))

        issue_load(0)
        st = {"gt": None, "gk": None, "si": 0, "gi": 0}
        steps = [(k, n) for k in range(len(groups)) for n in range(NCH)]

        def phase1(s):
            k, n = steps[s]
            ei, ex, fg = groups[k]
            slot = k % NB
            if n == 0:
                if ex[3] is None:
                    st["gt"] = None
                elif fg == 0:
                    st["gt"], st["gk"] = gb(ex[3])
            gt, gk = st["gt"], st["gk"]
            sl = slice(n * 512, (n + 1) * 512)
            hb = H[s % 2]
            hk = (base, "h", s % 2)
            for j in range(G):
                si = st["si"]
                pa, pak = self.PS[2 * (si % 2)], self.psk[2 * (si % 2)]
                pb, pbk = self.PS[2 * (si % 2) + 1], self.psk[2 * (si % 2) + 1]
                sa, sak = SA[si % 2], (base, "sa", si % 2)
                for c in range(KC):
                    P.op("pe", lambda e: e.matmul(pa[:], W1[slot][:, c, j * 128:(j + 1) * 128], self.XB[:, c, sl], start=(c == 0), stop=(c == KC - 1)),
                         reads=[(base, "w1", slot), ("XB", n)], writes=[pak])
                for c in range(KC):
                    P.op("pe", lambda e: e.matmul(pb[:], W3[slot][:, c, j * 128:(j + 1) * 128], self.XB[:, c, sl], start=(c == 0), stop=(c == KC - 1)),
                         reads=[(base, "w3", slot), ("XB", n)], writes=[pbk])
                P.op("act", lambda e: e.activation(out=sa[:], in_=pa[:], func=AF.Silu), reads=[pak], writes=[sak])
                if gt is None:
                    P.op("dve", lambda e: e.tensor_tensor(out=hb[:, j, :], in0=pb[:], in1=sa[:], op=ALU.mult),
                         reads=[pbk, sak], writes=[hk])
                else:
                    ht, htk = HT[si % 2], (base, "ht", si % 2)
                    P.op("pool", lambda e: e.tensor_tensor(out=ht[:], in0=sa[:], in1=gt[:, sl], op=ALU.mult),
                         reads=[sak, gk], writes=[htk])
                    P.op("dve", lambda e: e.tensor_tensor(out=hb[:, j, :], in0=pb[:], in1=ht[:], op=ALU.mult),
                         reads=[pbk, htk], writes=[hk])
                st["si"] += 1

        def phase2(s):
            k, n = steps[s]
            slot = k % NB
            sl = slice(n * 512, (n + 1) * 512)
            hb = H[s % 2]
            hk = (base, "h", s % 2)
            for m in range(KC):
                gi = st["gi"]
                py, pyk = self.PS[4 + gi % 4], self.psk[4 + gi % 4]
                st["gi"] += 1
                for j in range(G):
                    P.op("pe", lambda e: e.matmul(py[:], W2[slot][:, j, m * 128:(m + 1) * 128], hb[:, j, :], start=(j == 0), stop=(j == G - 1)),
                         reads=[(base, "w2", slot), hk], writes=[pyk])
                P.op("dve", lambda e: e.tensor_tensor(out=self.XT[:, m, sl], in0=py[:], in1=self.XT[:, m, sl], op=ALU.add),
                     reads=[pyk, ("XT", n)], writes=[("XT", n)])

        if len(groups) > 1:
            issue_load(1)
        phase1(0)
        for s in range(len(steps)):
            if s + 1 < len(steps):
                phase1(s + 1)
            phase2(s)
            kk_, nn_ = steps[s]
            if nn_ == NCH - 1 and kk_ + 2 < len(groups):
                issue_load(kk_ + 2)

    def router(self):
        P = self.P
        WR = P.sb("wr", [128, KC, 8])
        LG = P.sb("r_lg", [128, NT, 8])
        M8 = P.sb("r_m8", [128, NT, 8])
        EX = P.sb("r_ex", [128, NT, 8])
        SELM = P.sb("r_sel", [128, NT, 8])
        DEN = P.sb("r_den", [128, NT])
        GT = P.sb("r_gt", [8, S])
        SEL8 = P.sb("r_sel8", [8, 8, 128])
        P.dma("sp", WR[:], self.din["moe_router"].rearrange("(c p) e -> p c e", p=128), writes=["wr"], key="wr")
        P.dma("sp", SEL8[:], self.dc["sel8"], writes=["sel8"], key="wr")
        ps, pk = self.PS[0], self.psk[0]
        for t in range(NT):
            for c in range(KC):
                P.op("pe", lambda e: e.matmul(ps[:, t * 8:(t + 1) * 8], self.XT[:, c, t * 128:(t + 1) * 128], WR[:, c, :], start=(c == 0), stop=(c == KC - 1)),
                     reads=[("XT", t // 4), "wr"], writes=[pk])
        P.op("dve", lambda e: e.tensor_copy(out=LG[:], in_=ps[:, 0:NT * 8].rearrange("p (t e) -> p t e", e=8)), reads=[pk], writes=["r_lg"])
        for t in range(NT):
            P.op("dve", lambda e: e.max(out=M8[:, t, :], in_=LG[:, t, :]), reads=["r_lg"], writes=["r_m8"])
        m1 = M8[:, :, 0:1].broadcast_to([128, NT, 8])
        m2 = M8[:, :, 1:2].broadcast_to([128, NT, 8])
        P.op("dve", lambda e: e.tensor_tensor(out=EX[:], in0=LG[:], in1=m1, op=ALU.subtract), reads=["r_lg", "r_m8"], writes=["r_ex"])
        P.op("act", lambda e: e.activation(out=EX[:], in_=EX[:], func=AF.Exp), reads=["r_ex"], writes=["r_ex"])
        P.op("dve", lambda e: e.tensor_tensor(out=SELM[:], in0=LG[:], in1=m2, op=ALU.is_ge), reads=["r_lg", "r_m8"], writes=["r_sel"])
        P.op("dve", lambda e: e.tensor_tensor(out=EX[:], in0=EX[:], in1=SELM[:], op=ALU.mult), reads=["r_ex", "r_sel"], writes=["r_ex"])
        P.op("dve", lambda e: e.tensor_reduce(out=DEN[:], in_=EX[:], axis=AX.X, op=ALU.add), reads=["r_ex"], writes=["r_den"])
        P.op("dve", lambda e: e.reciprocal(out=DEN[:], in_=DEN[:]), reads=["r_den"], writes=["r_den"])
        P.op("dve", lambda e: e.tensor_tensor(out=EX[:], in0=EX[:], in1=DEN[:].unsqueeze(2).broadcast_to([128, NT, 8]), op=ALU.mult),
             reads=["r_ex", "r_den"], writes=["r_ex"])
        for q in range(4):
            pt, ptk = self.PS[1 + q % 2], self.psk[1 + q % 2]
            for tt in range(4):
                t = q * 4 + tt
                P.op("pe", lambda e: e.transpose(pt[0:8, tt * 128:(tt + 1) * 128], EX[:, t, :], self.ident[:]), reads=["r_ex", "ident"], writes=[ptk])
            P.op("act", lambda e: e.activation(out=GT[:, q * 512:(q + 1) * 512], in_=pt[0:8, :], func=AF.Copy), reads=[ptk], writes=["r_gt"])
        self.GT = GT
        self.SEL8 = SEL8
        self.GB = [P.sb(f"gb{i}", [128, S], BF16) for i in range(2)]
        if "g" in self.dbg:
            P.dma("sp", self.dbg["g"], EX[:], reads=["r_ex"], writes=["dbg"], key="dbg")

    def gate_bcast(self, eidx):
        P = self.P
        gbt = self.GB[eidx % 2]
        gk = ("gb", eidx % 2)
        for n in range(NCH):
            ps, pk = self.PS[6 + n % 2], self.psk[6 + n % 2]
            P.op("pe", lambda e: e.matmul(ps[:], self.SEL8[:, eidx, :], self.GT[:, n * 512:(n + 1) * 512], start=True, stop=True),
                 reads=["sel8", "r_gt"], writes=[pk])
            P.op("act", lambda e: e.activation(out=gbt[:, n * 512:(n + 1) * 512], in_=ps[:], func=AF.Copy), reads=[pk], writes=[gk])
        return gbt, gk


def build(stages=("load", "dn", "ffn0", "moba", "moe", "store"), dbg=()):
    k = K(stages, dbg)
    P = k.P
    din = k.din
    if "load" in stages:
        P.push()
        k.load_x()
        P.pop()
    if "dn" in stages:
        from_dn(k)
    if "ffn0" in stages:
        P.push()
        k.scale_xt()
        k.ffn([(din["ffn_w1"], din["ffn_w3"], din["ffn_w2"], None)])
        P.pop()
        P.push()
        k.layer_norm(1, k.ln_tmp())
        P.pop()
    if "dbg_x1" in k.dbg:
        k.dbg_store_xt("x1")
    if "moba" in stages:
        from_moba(k)
    if "moe" in stages:
        P.push()
        k.router()
        k.scale_xt()
        k.ffn([(din["moe_w1"][e], din["moe_w3"][e], din["moe_w2"][e], e) for e in range(NE)], gb=k.gate_bcast)
        P.pop()
        P.push()
        k.layer_norm(3, k.ln_tmp())
        P.pop()
    if "store" in stages:
        P.push()
        k.store_x()
        P.pop()
    P.close()
    return k.nc


import threading


def _weave(P, fa, fb, ra, rb):
    cond = threading.Condition()
    st = {"turn": 0, "cnt": 0, "done": [False, False], "err": None}
    quota = (ra, rb)
    tls = threading.local()
    orig_op = P.op

    def op(engine, fn, reads=(), writes=()):
        tid = tls.tid
        with cond:
            if st["cnt"] >= quota[tid] and not st["done"][1 - tid]:
                st["cnt"] = 0
                st["turn"] = 1 - tid
                cond.notify_all()
            while st["turn"] != tid:
                cond.wait()
            st["cnt"] += 1
        return orig_op(engine, fn, reads, writes)

    def runner(tid, f):
        tls.tid = tid
        try:
            with cond:
                while st["turn"] != tid:
                    cond.wait()
            f()
        except BaseException as ex:
            st["err"] = ex
        finally:
            with cond:
                st["done"][tid] = True
                st["turn"] = 1 - tid
                st["cnt"] = 0
                cond.notify_all()

    P.op = op
    try:
        ths = [threading.Thread(target=runner, args=(i, f)) for i, f in enumerate((fa, fb))]
        for t in ths:
            t.start()
        for t in ths:
            t.join()
    finally:
        del P.op
    if st["err"] is not None:
        raise st["err"]


def _nps(k):
    i = getattr(k, "psi", 0)
    k.psi = (i + 1) % 8
    return k.PS[i], k.psk[i]


DN_STOP = None


def from_dn(k):
    P = k.P
    din = k.din
    dc = k.dc
    TB = 4
    W = TB * 128
    QSCALE = 128.0 ** -0.5
    P.push()

    def cload(name, src, shape, dt=F32, q="sp"):
        t = P.sb(name, shape, dt)
        P.dma(q, t[:], src, writes=[name], key="dnc")
        return t

    TRI_LE = cload("tri_le", dc["tri_le"], [128, 128])
    TRI_GT = cload("tri_gt", dc["tri_gt"], [128, 128])
    NMBD = cload("nmask_bd", dc["nmask_bd"], [128, 128])
    MOFF = cload("mask_off", dc["mask_off"], [128, 128])
    ident, ones = k.ident, k.ones

    EPSB = P.sb("EPSB", [128, 2])
    P.op("dve", lambda e: e.memset(EPSB[:, 0:1], DN_EPS * 128.0), writes=["EPSB"])
    P.op("dve", lambda e: e.memset(EPSB[:, 1:2], DN_EPS), writes=["EPSB"])
    ONES128 = P.sb("ones128", [128, 128])
    ONESINV = P.sb("onesinv", [128, 128])
    P.op("dve", lambda e: e.tensor_scalar(out=ONES128[:], in0=ones[:], scalar1=128.0, scalar2=None, op0=ALU.mult), reads=["ones"], writes=["ones128"])
    P.op("dve", lambda e: e.tensor_scalar(out=ONESINV[:], in0=ones[:], scalar1=1.0 / 128.0, scalar2=None, op0=ALU.mult), reads=["ones"], writes=["onesinv"])

    def b3(T):
        return T[:].unsqueeze(1).broadcast_to([128, TB, 128])

    def v3(ap):
        return ap.rearrange("p (t n) -> p t n", t=TB)

    cwraw = P.sb("cwraw", [96, 128])
    P.dma("sp", cwraw[:], din["a_conv_w"].rearrange("j (c p) -> (j c) p", p=128), writes=["cwraw"], key="dnc")
    CW = P.sb("CW", [128, 96])
    ps, pk = _nps(k)
    P.op("pe", lambda e: e.transpose(ps[:, 0:96], cwraw[:], ident[0:96, 0:96]), reads=["cwraw", "ident"], writes=[pk])
    P.op("dve", lambda e: e.tensor_copy(out=CW[:], in_=ps[:, 0:96]), reads=[pk], writes=["CW"])
    NW = P.sb("NW", [128, 1])
    P.dma("sp", NW[:], din["a_norm_w"].rearrange("o p -> p o"), writes=["NW"], key="dnc")

    WAB = P.sb("WAB", [128, KC, 16])
    P.dma("sp", WAB[:], din["a_w_in"][:, 4096:4112].rearrange("(c p) f -> p c f", p=128), writes=["WAB"], key="dnc")
    RW = P.sb("RW", [1, 16])
    P.dma("sp", RW[0:1, 0:8], din["a_log_decay"], writes=["RW"], key="dnc")
    P.dma("sp", RW[0:1, 8:16], din["a_dt_bias"], writes=["RW"], key="dnc")
    AB = P.sb("AB", [128, NT, 16])
    BC = P.sb("BC", [128, 16])
    ps, pk = _nps(k)
    for t in range(NT):
        for c in range(KC):
            P.op("pe", lambda e: e.matmul(ps[:, t * 16:(t + 1) * 16], k.XT[:, c, t * 128:(t + 1) * 128], WAB[:, c, :], start=(c == 0), stop=(c == KC - 1)),
                 reads=[("XT", t // 4), "WAB"], writes=[pk])
    P.op("dve", lambda e: e.tensor_copy(out=AB[:], in_=ps[:, 0:NT * 16].rearrange("p (t f) -> p t f", f=16)), reads=[pk], writes=["AB"])
    ps, pk = _nps(k)
    P.op("pe", lambda e: e.matmul(ps[:, 0:16], ones[0:1, :], RW[0:1, :], start=True, stop=True), reads=["ones", "RW"], writes=[pk])
    P.op("dve", lambda e: e.tensor_copy(out=BC[:], in_=ps[:, 0:16]), reads=[pk], writes=["BC"])

    def tm(name):
        return P.sb(name, [128, NT, 8])
    G, BETA, EKD, BEG, EGL, TM1, TM2, GCS = (tm(n) for n in ("G", "BETA", "EKD", "BEG", "EGL", "TM1", "TM2", "GCS"))
    EA = P.sb("EA", [128, 8])
    dtb = BC[:, 8:16].unsqueeze(1).broadcast_to([128, NT, 8])
    P.op("dve", lambda e: e.tensor_tensor(out=TM1[:], in0=AB[:, :, 0:8], in1=dtb, op=ALU.add), reads=["AB", "BC"], writes=["TM1"])
    P.op("act", lambda e: e.activation(out=TM1[:], in_=TM1[:], func=AF.Exp), reads=["TM1"], writes=["TM1"])
    P.op("dve", lambda e: e.tensor_scalar(out=TM1[:], in0=TM1[:], scalar1=1.0, scalar2=None, op0=ALU.add), reads=["TM1"], writes=["TM1"])
    P.op("act", lambda e: e.activation(out=TM1[:], in_=TM1[:], func=AF.Ln), reads=["TM1"], writes=["TM1"])
    P.op("act", lambda e: e.activation(out=EA[:], in_=BC[:, 0:8], func=AF.Exp), reads=["BC"], writes=["EA"])
    P.op("dve", lambda e: e.scalar_tensor_tensor(out=G[:], in0=TM1[:], scalar=-1.0, in1=EA[:].unsqueeze(1).broadcast_to([128, NT, 8]),
                                                 op0=ALU.mult, op1=ALU.mult), reads=["TM1", "EA"], writes=["G"])
    P.op("act", lambda e: e.activation(out=TM2[:], in_=AB[:, :, 8:16], func=AF.Exp, scale=-1.0), reads=["AB"], writes=["TM2"])
    P.op("dve", lambda e: e.tensor_scalar(out=TM2[:], in0=TM2[:], scalar1=1.0, scalar2=None, op0=ALU.add), reads=["TM2"], writes=["TM2"])
    P.op("dve", lambda e: e.reciprocal(out=BETA[:], in_=TM2[:]), reads=["TM2"], writes=["BETA"])
    Gf = G[:].rearrange("p t h -> p (t h)")
    psc, pkc = _nps(k)
    psl, pkl = _nps(k)
    P.op("pe", lambda e: e.matmul(psc[:, 0:128], TRI_LE[:], Gf, start=True, stop=True), reads=["tri_le", "G"], writes=[pkc])
    P.op("pe", lambda e: e.matmul(psl[:, 0:128], ones[:], Gf, start=True, stop=True), reads=["ones", "G"], writes=[pkl])
    f3 = lambda ap: ap.rearrange("p (t h) -> p t h", h=8)
    P.op("act", lambda e: e.activation(out=GCS[:], in_=f3(psc[:, 0:128]), func=AF.Copy), reads=[pkc], writes=["GCS"])
    P.op("act", lambda e: e.activation(out=EGL[:], in_=f3(psl[:, 0:128]), func=AF.Exp), reads=[pkl], writes=["EGL"])
    P.op("dve", lambda e: e.tensor_tensor(out=TM2[:], in0=f3(psl[:, 0:128]), in1=GCS[:], op=ALU.subtract), reads=[pkl, "GCS", "BETA", "EGL"], writes=["TM2"])
    P.op("act", lambda e: e.activation(out=EKD[:], in_=TM2[:], func=AF.Exp), reads=["TM2"], writes=["EKD"])
    P.op("act", lambda e: e.activation(out=TM1[:], in_=GCS[:], func=AF.Exp), reads=["GCS", "G"], writes=["TM1"])
    P.op("dve", lambda e: e.tensor_tensor(out=BEG[:], in0=TM1[:], in1=BETA[:], op=ALU.mult), reads=["TM1", "BETA"], writes=["BEG"])

    k.scale_xt()
    if DN_STOP == "tm":
        P.pop()
        return

    W4 = P.sb("W4", [128, 4, KC, 128], BF16)
    WO = P.sb("dnWO", [128, D], BF16)
    OT = P.sb("OT", [128, S])
    DG = P.sb("DG", [128, 3, 4, 128], BF16)
    QF = P.sb("QF", [128, S])
    KF = P.sb("KF", [128, S])
    VF = P.sb("VF", [128, S])
    SG = P.sb("SG", [128, S], BF16)
    OBP = P.sb("dnOB", [128, S + 3], BF16)
    OB = OBP[:, 3:S + 3]
    PREb = OBP
    RS1 = P.sb("RS1", [128, 512])
    RS2 = P.sb("RS2", [128, 512])
    Sst = P.sb("Sst", [128, 128])
    Sb = P.sb("Sb", [128, 128], BF16)
    TRIG = P.sb("TRIG", [128, TB, 128])
    ED = P.sb("ED", [128, TB, 128])
    EDT = P.sb("EDT", [128, TB, 128])
    EGB = P.sb("EGB", [128, TB, 128])
    LF = P.sb("LF", [128, TB, 128])
    Nf = P.sb("Nf", [128, TB, 128])
    Nn = [P.sb(f"Nn{i}", [128, TB, 128], BF16) for i in range(2)]
    Mm = [P.sb(f"Mm{i}", [128, TB, 128], BF16) for i in range(2)]
    LOFF = P.sb("LOFF", [128, TB, 128])
    LOFFT = P.sb("LOFFT", [128, TB, 128], BF16)
    Tt = P.sb("Tt", [128, TB, 128], BF16)
    R = P.sb("Rr", [128, TB, 256], BF16)
    SOL2 = [P.sb(f"SOL{i}", [128, TB, 256]) for i in range(2)]
    SOLb = P.sb("SOLb", [128, TB, 256], BF16)
    identb = k.identb
    KDEC2 = [P.sb(f"KDEC{i}", [128, TB, 128], BF16) for i in range(2)]
    WT2 = [P.sb(f"WT{i}", [128, TB, 128], BF16) for i in range(2)]
    INTRAT2 = [P.sb(f"INTRAT{i}", [128, TB, 128], BF16) for i in range(2)]
    QDT2 = [P.sb(f"QDT{i}", [128, TB, 128], BF16) for i in range(2)]
    VNEW = [P.sb(f"VNEW{i}", [128, 128], BF16) for i in range(2)]
    P.op("dve", lambda e: e.memset(OBP[:, 0:3], 0.0), writes=[("OB", -1)])

    def ck(nm, n):
        return (nm, n)
    QK4 = lambda nm: [ck(nm, n) for n in range(NCH)]

    def load_w4(hh):
        for f in range(4):
            c0 = f * D + hh * 128
            P.dma("pool", W4[:, f, :, :], din["a_w_in"][:, c0:c0 + 128].rearrange("(c p) f -> p c f", p=128), writes=["W4"], key="W4")

    load_w4(0)
    for h in range(8):
        P.dma("pool", WO[:], din["a_w_out"][h * 128:(h + 1) * 128, :], writes=["dnWO"], key="dnWO")
        for f in range(3):
            for j in range(4):
                ci = f * 8 + h
                P.op("dve", lambda e: e.tensor_scalar(out=DG[:, f, j, :], in0=ident[:], scalar1=CW[:, j * 24 + ci:j * 24 + ci + 1], scalar2=None, op0=ALU.mult),
                     reads=["ident", "CW"], writes=["DG"])
        for f in range(3):
            dst, dk = ((QF, "QF"), (KF, "KF"), (VF, "VF"))[f]
            for n in range(NCH):
                sl = slice(n * 512, (n + 1) * 512)
                ps, pk = _nps(k)
                for c in range(KC):
                    P.op("pe", lambda e: e.matmul(ps[:], W4[:, f, c, :], k.XB[:, c, sl], start=(c == 0), stop=(c == KC - 1)),
                         reads=["W4", ("XB", n)], writes=[pk])
                P.op("act", lambda e: e.activation(out=PREb[:, 3 + n * 512:3 + (n + 1) * 512], in_=ps[:], func=AF.Copy), reads=[pk], writes=[ck("OB", n)])
            for n in range(NCH):
                sl = slice(n * 512, (n + 1) * 512)
                ps, pk = _nps(k)
                for j in range(4):
                    P.op("pe", lambda e: e.matmul(ps[:], DG[:, f, j, :], PREb[:, n * 512 + j:n * 512 + j + 512], start=(j == 0), stop=(j == 3)),
                         reads=["DG", ck("OB", n), ck("OB", n - 1)], writes=[pk])
                P.op("act", lambda e: e.activation(out=dst[:, sl], in_=ps[:], func=AF.Silu), reads=[pk], writes=[ck(dk, n)])
            if f < 2:
                for n in range(NCH):
                    sl = slice(n * 512, (n + 1) * 512)
                    P.op("act", lambda e: e.activation(out=VF[:, sl], in_=dst[:, sl], func=AF.Square), reads=[ck(dk, n)], writes=[ck("VF", n)])
                    ps, pk = _nps(k)
                    om = ONES128 if f == 0 else ones
                    eb = EPSB[:, 0:1] if f == 0 else EPSB[:, 1:2]
                    P.op("pe", lambda e: e.matmul(ps[:], om[:], VF[:, sl], start=True, stop=True), reads=["ones", "ones128", ck("VF", n)], writes=[pk])
                    rs, rk_ = (RS1, "RS1") if n % 2 == 0 else (RS2, "RS2")
                    P.op("act", lambda e: e.activation(out=rs[:], in_=ps[:], func=AF.Ln, bias=eb), reads=[pk, "EPSB"], writes=[rk_])
                    P.op("act", lambda e: e.activation(out=rs[:], in_=rs[:], func=AF.Exp, scale=-0.5), reads=[rk_], writes=[rk_])
                    P.op("pool", lambda e: e.tensor_tensor(out=dst[:, sl], in0=dst[:, sl], in1=rs[:], op=ALU.mult), reads=[ck(dk, n), rk_], writes=[ck(dk, n)])
        for n in range(NCH):
            sl = slice(n * 512, (n + 1) * 512)
            ps, pk = _nps(k)
            for c in range(KC):
                P.op("pe", lambda e: e.matmul(ps[:], W4[:, 3, c, :], k.XB[:, c, sl], start=(c == 0), stop=(c == KC - 1)),
                     reads=["W4", ("XB", n)], writes=[pk])
            P.op("act", lambda e: e.activation(out=SG[:, sl], in_=ps[:], func=AF.Silu), reads=[pk], writes=[ck("SG", n)])
        P.op("dve", lambda e: e.memset(Sst[:], 0.0), writes=["Sst"])
        P.op("dve", lambda e: e.memset(Sb[:], 0.0), writes=["Sb"])
        if h + 1 < 8:
            load_w4(h + 1)
        if DN_STOP == "A":
            P.pop()
            return

        def pre(b):
            bi = b % 2
            KDEC, WT, INTRAT, QDT, SOL = KDEC2[bi], WT2[bi], INTRAT2[bi], QDT2[bi], SOL2[bi]
            t0 = b * TB

            def bc(T):
                return T[:, t0:t0 + TB, h:h + 1].broadcast_to([128, TB, 128])

            def tl(i):
                return slice((t0 + i) * 128, (t0 + i + 1) * 128)

            def cs(i):
                return slice(i * 128, (i + 1) * 128)
            psk, pkk = _nps(k)
            psv, pkv = _nps(k)
            for i in range(TB):
                P.op("pe", lambda e: e.transpose(psk[:, cs(i)], KF[:, tl(i)], ident[:]), reads=[ck("KF", (t0 + i) // 4), "ident"], writes=[pkk])
                P.op("pe", lambda e: e.transpose(psv[:, cs(i)], VF[:, tl(i)], ident[:]), reads=[ck("VF", (t0 + i) // 4), "ident"], writes=[pkv])
            P.op("dve", lambda e: e.tensor_tensor(out=KDEC[:], in0=v3(psk[:, 0:W]), in1=bc(EKD), op=ALU.mult), reads=[pkk, "EKD"], writes=[("KDEC", bi)])
            P.op("dve", lambda e: e.tensor_tensor(out=R[:, :, 128:256], in0=v3(psk[:, 0:W]), in1=bc(BEG), op=ALU.mult), reads=[pkk, "BEG"], writes=["Rr"])
            P.op("dve", lambda e: e.tensor_tensor(out=R[:, :, 0:128], in0=v3(psv[:, 0:W]), in1=bc(BETA), op=ALU.mult), reads=[pkv, "BETA"], writes=["Rr"])
            P.op("pool", lambda e: e.tensor_tensor(out=TRIG[:], in0=b3(TRI_LE), in1=bc(G), op=ALU.mult), reads=["tri_le", "G"], writes=["TRIG"])
            psd, pkd = _nps(k)
            psdt, pkdt = _nps(k)
            psg, pkg = _nps(k)
            for i in range(TB):
                P.op("pe", lambda e: e.matmul(psd[:, cs(i)], TRIG[:, i, :], TRI_GT[:], start=True, stop=True), reads=["TRIG", "tri_gt"], writes=[pkd])
            TRIGf = TRIG[:].rearrange("p t n -> p (t n)")
            P.op("pe", lambda e: e.matmul(psdt[:, 0:W], TRI_GT[:], TRIGf, start=True, stop=True), reads=["TRIG", "tri_gt"], writes=[pkdt])
            P.op("pe", lambda e: e.matmul(psg[:, 0:W], ones[:], TRIGf, start=True, stop=True), reads=["TRIG", "ones"], writes=[pkg])
            P.op("act", lambda e: e.activation(out=ED[:], in_=v3(psd[:, 0:W]), func=AF.Exp), reads=[pkd], writes=["ED"])
            P.op("act", lambda e: e.activation(out=EDT[:], in_=v3(psdt[:, 0:W]), func=AF.Exp), reads=[pkdt], writes=["EDT"])
            P.op("act", lambda e: e.activation(out=EGB[:], in_=v3(psg[:, 0:W]), func=AF.Exp), reads=[pkg], writes=["EGB"])
            pskk, pkkk = _nps(k)
            psqk, pkqk = _nps(k)
            for i in range(TB):
                P.op("pe", lambda e: e.matmul(pskk[:, cs(i)], KF[:, tl(i)], KF[:, tl(i)], start=True, stop=True), reads=[ck("KF", (t0 + i) // 4)], writes=[pkkk])
                P.op("pe", lambda e: e.matmul(psqk[:, cs(i)], KF[:, tl(i)], QF[:, tl(i)], start=True, stop=True), reads=[ck("KF", (t0 + i) // 4), ck("QF", (t0 + i) // 4)], writes=[pkqk])
            P.op("pool", lambda e: e.tensor_tensor(out=ED[:], in0=ED[:], in1=bc(BETA), op=ALU.mult), reads=["ED", "BETA"], writes=["ED"])
            P.op("dve", lambda e: e.tensor_tensor(out=LF[:], in0=v3(pskk[:, 0:W]), in1=ED[:], op=ALU.mult), reads=[pkkk, "ED"], writes=["LF"])
            P.op("pool", lambda e: e.tensor_tensor(out=Nf[:], in0=LF[:], in1=b3(NMBD), op=ALU.mult), reads=["LF", "nmask_bd"], writes=["Nf"])
            P.op("pool", lambda e: e.tensor_tensor(out=Nn[0][:], in0=LF[:], in1=b3(NMBD), op=ALU.mult), reads=["LF", "nmask_bd"], writes=["Nn0"])
            P.op("pool", lambda e: e.tensor_tensor(out=LOFF[:], in0=LF[:], in1=b3(MOFF), op=ALU.mult), reads=["LF", "mask_off"], writes=["LOFF"])
            P.op("pool", lambda e: e.tensor_tensor(out=EDT[:], in0=EDT[:], in1=b3(TRI_LE), op=ALU.mult), reads=["EDT", "tri_le"], writes=["EDT"])
            P.op("dve", lambda e: e.tensor_tensor(out=INTRAT[:], in0=v3(psqk[:, 0:W]), in1=EDT[:], op=ALU.mult), reads=[pkqk, "EDT"], writes=[("INTRAT", bi)])
            P.op("pool", lambda e: e.tensor_tensor(out=QDT[:], in0=v3(QF[:, t0 * 128:(t0 + TB) * 128]), in1=EGB[:], op=ALU.mult), reads=[ck("QF", t0 // 4), "EGB"], writes=[("QDT", bi)])
            psa, pka = _nps(k)
            psb, pkb = _nps(k)
            for i in range(TB):
                P.op("pe", lambda e: e.transpose(psa[:, cs(i)], Nf[:, i, :], ident[:]), reads=["Nf", "ident"], writes=[pka])
                P.op("pe", lambda e: e.transpose(psb[:, cs(i)], LOFF[:, i, :], ident[:]), reads=["LOFF", "ident"], writes=[pkb])
            P.op("act", lambda e: e.activation(out=Mm[0][:], in_=v3(psa[:, 0:W]), func=AF.Copy), reads=[pka], writes=["Mm0"])
            P.op("act", lambda e: e.activation(out=LOFFT[:], in_=v3(psb[:, 0:W]), func=AF.Copy), reads=[pkb], writes=["LOFFT"])
            P.op("dve", lambda e: e.tensor_tensor(out=Tt[:], in0=Mm[0][:], in1=b3(identb), op=ALU.add), reads=["Mm0", "identb"], writes=["Tt"])
            for kk in range(1, 6):
                a, bb = (kk - 1) % 2, kk % 2
                psn, pkn = _nps(k)
                for i in range(TB):
                    P.op("pe", lambda e: e.matmul(psn[:, cs(i)], Mm[a][:, i, :], Nn[a][:, i, :], start=True, stop=True), reads=[f"Mm{a}", f"Nn{a}"], writes=[pkn])
                if kk < 5:
                    psm, pkm = _nps(k)
                    for i in range(TB):
                        P.op("pe", lambda e: e.matmul(psm[:, cs(i)], Nn[a][:, i, :], Mm[a][:, i, :], start=True, stop=True), reads=[f"Mm{a}", f"Nn{a}"], writes=[pkm])
                P.op("act", lambda e: e.activation(out=Nn[bb][:], in_=v3(psn[:, 0:W]), func=AF.Copy), reads=[pkn], writes=[f"Nn{bb}"])
                if kk < 5:
                    P.op("dve", lambda e: e.tensor_copy(out=Mm[bb][:], in_=v3(psm[:, 0:W])), reads=[pkm], writes=[f"Mm{bb}"])
                psc, pkc = _nps(k)
                for i in range(TB):
                    P.op("pe", lambda e: e.matmul(psc[:, cs(i)], Nn[bb][:, i, :], Tt[:, i, :], start=True, stop=True), reads=[f"Nn{bb}", "Tt"], writes=[pkc])
                P.op("dve", lambda e: e.tensor_tensor(out=Tt[:], in0=v3(psc[:, 0:W]), in1=Tt[:], op=ALU.add), reads=[pkc, "Tt"], writes=["Tt"])
            v256 = lambda ap: ap.rearrange("p (t n) -> p t n", t=2)
            for hp in range(TB // 2):
                pss, pks = _nps(k)
                for i2 in range(2):
                    i = 2 * hp + i2
                    P.op("pe", lambda e: e.matmul(pss[:, i2 * 256:(i2 + 1) * 256], Tt[:, i, :], R[:, i, :], start=True, stop=True), reads=["Tt", "Rr"], writes=[pks])
                P.op("act", lambda e: e.activation(out=SOLb[:, 2 * hp:2 * hp + 2, :], in_=v256(pss[:]), func=AF.Copy), reads=[pks], writes=["SOLb"])
                P.op("act", lambda e: e.activation(out=SOL[:, 2 * hp:2 * hp + 2, :], in_=v256(pss[:]), func=AF.Copy), reads=[pks], writes=[("SOL", bi)])
            for hp in range(TB // 2):
                psz, pkz = _nps(k)
                for i2 in range(2):
                    i = 2 * hp + i2
                    P.op("pe", lambda e: e.matmul(psz[:, i2 * 256:(i2 + 1) * 256], LOFFT[:, i, :], SOLb[:, i, :], start=True, stop=True), reads=["LOFFT", "SOLb"], writes=[pkz])
                P.op("act", lambda e: e.activation(out=R[:, 2 * hp:2 * hp + 2, :], in_=v256(psz[:]), func=AF.Copy), reads=[pkz], writes=["Rr"])
            for hp in range(TB // 2):
                psc2, pkc2 = _nps(k)
                for i2 in range(2):
                    i = 2 * hp + i2
                    P.op("pe", lambda e: e.matmul(psc2[:, i2 * 256:(i2 + 1) * 256], Tt[:, i, :], R[:, i, :], start=True, stop=True), reads=["Tt", "Rr"], writes=[pkc2])
                P.op("dve", lambda e: e.scalar_tensor_tensor(out=SOL[:, 2 * hp:2 * hp + 2, :], in0=v256(psc2[:]), scalar=-1.0, in1=SOL[:, 2 * hp:2 * hp + 2, :], op0=ALU.mult, op1=ALU.add),
                     reads=[pkc2, ("SOL", bi)], writes=[("SOL", bi)])
            psw, pkw = _nps(k)
            for i in range(TB):
                P.op("pe", lambda e: e.transpose(psw[:, cs(i)], SOL[:, i, 128:256], ident[:]), reads=[("SOL", bi), "ident"], writes=[pkw])
            P.op("act", lambda e: e.activation(out=WT[:], in_=v3(psw[:, 0:W]), func=AF.Copy), reads=[pkw], writes=[("WT", bi)])

        def scan(b):
            bi = b % 2
            KDEC, WT, INTRAT, QDT, SOL = KDEC2[bi], WT2[bi], INTRAT2[bi], QDT2[bi], SOL2[bi]
            t0 = b * TB

            def tl(i):
                return slice((t0 + i) * 128, (t0 + i + 1) * 128)

            def cs(i):
                return slice(i * 128, (i + 1) * 128)
            for i in range(TB):
                t = t0 + i
                vn = VNEW[t % 2]
                vk = f"VNEW{t % 2}"
                ps1, pk1 = _nps(k)
                P.op("pe", lambda e: e.matmul(ps1[:, 0:128], WT[:, i, :], Sb[:], start=True, stop=True), reads=[("WT", bi), "Sb"], writes=[pk1])
                P.op("dve", lambda e: e.scalar_tensor_tensor(out=vn[:], in0=ps1[:, 0:128], scalar=-1.0, in1=SOL[:, i, 0:128], op0=ALU.mult, op1=ALU.add),
                     reads=[pk1, ("SOL", bi)], writes=[vk])
                ps2, pk2 = _nps(k)
                P.op("pe", lambda e: e.matmul(ps2[:, 0:128], Sb[:], QDT[:, i, :], start=True, stop=False), reads=["Sb", ("QDT", bi)], writes=[pk2])
                P.op("pe", lambda e: e.matmul(ps2[:, 0:128], vn[:], INTRAT[:, i, :], start=False, stop=True), reads=[vk, ("INTRAT", bi)], writes=[pk2])
                P.op("act", lambda e: e.activation(out=OT[:, t * 128:(t + 1) * 128], in_=ps2[:, 0:128], func=AF.Copy), reads=[pk2], writes=[ck("OT", t // 4)])
                ps3, pk3 = _nps(k)
                P.op("pe", lambda e: e.matmul(ps3[:, 0:128], KDEC[:, i, :], vn[:], start=True, stop=True), reads=[("KDEC", bi), vk], writes=[pk3])
                P.op("dve", lambda e: e.scalar_tensor_tensor(out=Sst[:], in0=Sst[:], scalar=EGL[:, t, h:h + 1], in1=ps3[:, 0:128], op0=ALU.mult, op1=ALU.add),
                     reads=[pk3, "Sst", "EGL"], writes=["Sst"])
                P.op("act", lambda e: e.activation(out=Sb[:], in_=Sst[:], func=AF.Copy), reads=["Sst"], writes=["Sb"])

        NBT = NT // TB
        pre(0)
        for b in range(NBT):
            if b + 1 < NBT:
                _weave(P, (lambda: pre(b + 1)), (lambda: scan(b)), 4, 1)
            else:
                scan(b)
        if DN_STOP == "B4":
            P.pop()
            return
        if DN_STOP == "dump":
            for nm, T, kk_ in (("QF", QF, "QF"), ("KF", KF, "KF"), ("VF", VF, "VF")):
                P.dma("sp", k.dbg[nm], T[:], reads=QK4(kk_), writes=["dbg"], key="dbg")
            P.dma("sp", k.dbg["OT"], OT[:], reads=QK4("OT"), writes=["dbg"], key="dbg")
            for nm, T in (("G", G), ("BETA", BETA), ("EKD", EKD), ("BEG", BEG), ("EGL", EGL)):
                P.dma("sp", k.dbg[nm], T[:], reads=[nm], writes=["dbg"], key="dbg")
            P.pop()
            return
        for n in range(NCH):
            sl = slice(n * 512, (n + 1) * 512)
            P.op("act", lambda e: e.activation(out=VF[:, sl], in_=OT[:, sl], func=AF.Square), reads=[ck("OT", n)], writes=[ck("VF", n)])
            ps, pk = _nps(k)
            P.op("pe", lambda e: e.matmul(ps[:], ONESINV[:], VF[:, sl], start=True, stop=True), reads=["onesinv", ck("VF", n)], writes=[pk])
            rs, rk_ = (RS1, "RS1") if n % 2 == 0 else (RS2, "RS2")
            P.op("act", lambda e: e.activation(out=rs[:], in_=ps[:], func=AF.Ln, bias=EPSB[:, 1:2]), reads=[pk, "EPSB"], writes=[rk_])
            P.op("act", lambda e: e.activation(out=rs[:], in_=rs[:], func=AF.Exp, scale=-0.5), reads=[rk_], writes=[rk_])
            P.op("pool", lambda e: e.tensor_tensor(out=VF[:, sl], in0=OT[:, sl], in1=rs[:], op=ALU.mult), reads=[ck("OT", n), rk_, ck("VF", n)], writes=[ck("VF", n)])
            P.op("dve", lambda e: e.scalar_tensor_tensor(out=OB[:, sl], in0=VF[:, sl], scalar=NW[:, 0:1], in1=SG[:, sl], op0=ALU.mult, op1=ALU.mult),
                 reads=[ck("VF", n), "NW", ck("SG", n)], writes=[ck("OB", n)])
        for n in range(NCH):
            sl = slice(n * 512, (n + 1) * 512)
            for m in range(KC):
                ps, pk = _nps(k)
                P.op("pe", lambda e: e.matmul(ps[:], WO[:, m * 128:(m + 1) * 128], OB[:, sl], start=True, stop=True), reads=["dnWO", ck("OB", n)], writes=[pk])
                P.op("dve", lambda e: e.tensor_tensor(out=k.XT[:, m, sl], in0=ps[:], in1=k.XT[:, m, sl], op=ALU.add), reads=[pk, ("XT", n)], writes=[("XT", n)])
    P.pop()
    P.push()
    k.layer_norm(0, k.ln_tmp())
    P.pop()


def from_moba(k):
    P = k.P
    din = k.din
    dc = k.dc
    ident, ones, onesb = k.ident, k.ones, k.onesb
    SCALE = 128.0 ** -0.5
    BIG = 30000.0
    P.push()

    def cload(name, src, shape, dt=F32, q="sp"):
        t = P.sb(name, shape, dt)
        P.dma(q, t[:], src, writes=[name], key="mbc")
        return t
    PERM = cload("perm", dc["perm"], [128, 128])
    ROPEC = cload("ropec", dc["ropec"], [128, S])
    ROPES = cload("ropes", dc["ropes"], [128, S])
    PASTB = cload("pastb", dc["pastb"], [128, NT, 8])
    PAST01 = cload("past01", dc["past01"], [128, NT, 8])
    OWN01 = cload("own01", dc["own01"], [128, NT, 8])
    SEL8b = cload("sel8b", dc["sel8"], [8, 8, 128], BF16, q="pool")
    CAUS = cload("caus4", dc["caus4"], [128, 4, 512], BF16, q="pool")

    k.scale_xt()

    Wq = P.sb("mWq", [128, KC, 128], BF16)
    Wk = P.sb("mWk", [128, KC, 128], BF16)
    Wv = P.sb("mWv", [128, KC, 128], BF16)
    WO = P.sb("mWO", [128, D], BF16)
    QF = P.sb("mQF", [128, S])
    KF = P.sb("mKF", [128, S])
    QB = P.sb("mQB", [128, S], BF16)
    KB = P.sb("mKB", [128, S], BF16)
    VTM = P.sb("mVTM", [128, NT, 128], BF16)
    OB = P.sb("mOB", [128, S], BF16)
    RAW = [P.sb(f"mRAW{i}", [128, 512]) for i in range(2)]
    T1 = P.sb("mT1", [128, 512])
    T2 = P.sb("mT2", [128, 512])
    PT = [P.sb(f"mPT{i}", [128, 512], BF16) for i in range(2)]
    RL = P.sb("mRL", [128, 512])
    KM = P.sb("mKM", [128, 8])
    GATE = P.sb("mGATE", [128, NT, 8])
    M8 = P.sb("mM8", [128, NT, 8])
    SEL = P.sb("mSEL", [128, NT, 8])
    BIAST = P.sb("mBIAST", [8, S], BF16)
    ri = 0
    pi = 0
    def load_w(hh):
        P.dma("pool", Wq[:], din["b_w_q"][:, hh * 128:(hh + 1) * 128].rearrange("(c p) f -> p c f", p=128), writes=["mWq"], key="mWq")
        P.dma("pool", Wk[:], din["b_w_kv"][:, hh * 128:(hh + 1) * 128].rearrange("(c p) f -> p c f", p=128), writes=["mWk"], key="mWk")
        P.dma("pool", Wv[:], din["b_w_kv"][:, D + hh * 128:D + (hh + 1) * 128].rearrange("(c p) f -> p c f", p=128), writes=["mWv"], key="mWv")

    load_w(0)
    for h in range(8):
        P.dma("pool", WO[:], din["b_w_o"][h * 128:(h + 1) * 128, :], writes=["mWO"], key="mWO")
        jobs = [(Wt, wk_, Ff, fk, Bb, bk, n) for (Wt, wk_, Ff, fk, Bb, bk) in ((Wq, "mWq", QF, "mQF", QB, "mQB"), (Wk, "mWk", KF, "mKF", KB, "mKB"))
                for n in range(NCH)]

        def r1(job):
            Wt, wk_, Ff, fk, Bb, bk, n = job
            nonlocal ri
            sl = slice(n * 512, (n + 1) * 512)
            ps, pk = _nps(k)
            for c in range(KC):
                P.op("pe", lambda e: e.matmul(ps[:], Wt[:, c, :], k.XB[:, c, sl], start=(c == 0), stop=(c == KC - 1)), reads=[wk_, ("XB", n)], writes=[pk])
            raw, rk = RAW[ri % 2], f"mRAW{ri % 2}"
            ri += 1
            P.op("act", lambda e: e.activation(out=raw[:], in_=ps[:], func=AF.Copy), reads=[pk], writes=[rk])
            return raw, rk

        def r2(job, raw, rk):
            Wt, wk_, Ff, fk, Bb, bk, n = job
            sl = slice(n * 512, (n + 1) * 512)
            ps2, pk2 = _nps(k)
            P.op("pe", lambda e: e.matmul(ps2[:], PERM[:], raw[:], start=True, stop=True), reads=["perm", rk], writes=[pk2])
            P.op("dve", lambda e: e.tensor_tensor(out=T1[:], in0=ps2[:], in1=ROPES[:, sl], op=ALU.mult), reads=[pk2, "ropes"], writes=["mT1"])
            P.op("pool", lambda e: e.tensor_tensor(out=T2[:], in0=raw[:], in1=ROPEC[:, sl], op=ALU.mult), reads=[rk, "ropec"], writes=["mT2"])
            P.op("pool", lambda e: e.tensor_tensor(out=Ff[:, sl], in0=T1[:], in1=T2[:], op=ALU.add), reads=["mT1", "mT2"], writes=[fk])
            P.op("act", lambda e: e.activation(out=Bb[:, sl], in_=Ff[:, sl], func=AF.Copy), reads=[fk], writes=[bk])

        cur = r1(jobs[0])
        for ji in range(len(jobs)):
            nxt = r1(jobs[ji + 1]) if ji + 1 < len(jobs) else None
            r2(jobs[ji], *cur)
            cur = nxt
        P.op("dve", lambda e: e.tensor_reduce(out=KM[:], in_=KF[:].rearrange("p (b n) -> p b n", n=256), axis=AX.X, op=ALU.add), reads=["mKF"], writes=["mKM"])
        P.op("dve", lambda e: e.tensor_scalar(out=KM[:], in0=KM[:], scalar1=1.0 / 256.0, scalar2=None, op0=ALU.mult), reads=["mKM"], writes=["mKM"])
        for g in range(4):
            ps, pk = _nps(k)
            for i in range(4):
                t = 4 * g + i
                for c in range(KC):
                    P.op("pe", lambda e: e.matmul(ps[:, i * 128:(i + 1) * 128], k.XB[:, c, t * 128:(t + 1) * 128], Wv[:, c, :], start=(c == 0), stop=(c == KC - 1)),
                         reads=["mWv", ("XB", g)], writes=[pk])
            P.op("act", lambda e: e.activation(out=VTM[:, 4 * g:4 * g + 4, :], in_=ps[:].rearrange("p (t n) -> p t n", t=4), func=AF.Copy), reads=[pk], writes=["mVTM"])
        if h + 1 < 8:
            load_w(h + 1)
        ps, pk = _nps(k)
        for t in range(NT):
            P.op("pe", lambda e: e.matmul(ps[:, t * 8:(t + 1) * 8], QF[:, t * 128:(t + 1) * 128], KM[:], start=True, stop=True), reads=["mQF", "mKM"], writes=[pk])
        P.op("dve", lambda e: e.tensor_tensor(out=GATE[:], in0=ps[:, 0:NT * 8].rearrange("p (t n) -> p t n", n=8), in1=PASTB[:], op=ALU.add), reads=[pk, "pastb"], writes=["mGATE"])
        for t in range(NT):
            P.op("dve", lambda e: e.max(out=M8[:, t, :], in_=GATE[:, t, :]), reads=["mGATE"], writes=["mM8"])
        P.op("dve", lambda e: e.tensor_tensor(out=SEL[:], in0=GATE[:], in1=M8[:, :, 2:3].broadcast_to([128, NT, 8]), op=ALU.is_ge), reads=["mGATE", "mM8"], writes=["mSEL"])
        P.op("dve", lambda e: e.tensor_tensor(out=SEL[:], in0=SEL[:], in1=PAST01[:], op=ALU.mult), reads=["mSEL", "past01"], writes=["mSEL"])
        P.op("dve", lambda e: e.tensor_tensor(out=SEL[:], in0=SEL[:], in1=OWN01[:], op=ALU.add), reads=["mSEL", "own01"], writes=["mSEL"])
        P.op("dve", lambda e: e.tensor_scalar(out=SEL[:], in0=SEL[:], scalar1=-1.0, scalar2=BIG, op0=ALU.add, op1=ALU.mult), reads=["mSEL"], writes=["mSEL"])
        for g in range(4):
            ps, pk = _nps(k)
            for i in range(4):
                t = 4 * g + i
                P.op("pe", lambda e: e.transpose(ps[0:8, i * 128:(i + 1) * 128], SEL[:, t, :], ident[:]), reads=["mSEL", "ident"], writes=[pk])
            P.op("act", lambda e: e.activation(out=BIAST[:, g * 512:(g + 1) * 512], in_=ps[0:8, :], func=AF.Copy), reads=[pk], writes=["mBIAST"])
        for qc in range(NCH):
            sl = slice(qc * 512, (qc + 1) * 512)
            pso, pko = _nps(k)
            psl, pkl = _nps(k)
            njt = 4 * qc + 4

            def s_stage(jt):
                while k.psi in (int(pko[2:]), int(pkl[2:])):
                    k.psi = (k.psi + 1) % 8
                pss, pks = _nps(k)
                P.op("pe", lambda e: e.matmul(pss[:], KB[:, jt * 128:(jt + 1) * 128], QB[:, sl], start=True, stop=False), reads=["mKB", "mQB"], writes=[pks])
                P.op("pe", lambda e: e.matmul(pss[:], SEL8b[:, jt // 2, :], BIAST[:, sl], start=False, stop=True), reads=["sel8b", "mBIAST"], writes=[pks])
                return pss, pks

            cur = s_stage(0)
            for jt in range(njt):
                nxt = s_stage(jt + 1) if jt + 1 < njt else None
                pss, pks = cur
                pt, ptk = PT[pi % 2], f"mPT{pi % 2}"
                pi += 1
                P.op("act", lambda e: e.activation(out=pt[:], in_=pss[:], func=AF.Exp, scale=SCALE), reads=[pks], writes=[ptk])
                if jt >= 4 * qc:
                    P.op("pool", lambda e: e.tensor_tensor(out=pt[:], in0=pt[:], in1=CAUS[:, jt - 4 * qc, :], op=ALU.mult), reads=[ptk, "caus4"], writes=[ptk])
                P.op("pe", lambda e: e.matmul(pso[:], VTM[:, jt, :], pt[:], start=(jt == 0), stop=(jt == njt - 1)), reads=["mVTM", ptk], writes=[pko])
                P.op("pe", lambda e: e.matmul(psl[:], onesb[:], pt[:], start=(jt == 0), stop=(jt == njt - 1)), reads=["onesb", ptk], writes=[pkl])
                cur = nxt
            P.op("act", lambda e: e.activation(out=RL[:], in_=psl[:], func=AF.Ln), reads=[pkl], writes=["mRL"])
            P.op("act", lambda e: e.activation(out=RL[:], in_=RL[:], func=AF.Exp, scale=-1.0), reads=["mRL"], writes=["mRL"])
            P.op("dve", lambda e: e.tensor_tensor(out=OB[:, sl], in0=pso[:], in1=RL[:], op=ALU.mult), reads=[pko, "mRL"], writes=["mOB"])
        for m in range(KC):
            for n in range(NCH):
                sl = slice(n * 512, (n + 1) * 512)
                ps, pk = _nps(k)
                P.op("pe", lambda e: e.matmul(ps[:], WO[:, m * 128:(m + 1) * 128], OB[:, sl], start=True, stop=True), reads=["mWO", "mOB"], writes=[pk])
                P.op("dve", lambda e: e.tensor_tensor(out=k.XT[:, m, sl], in0=ps[:], in1=k.XT[:, m, sl], op=ALU.add), reads=[pk, ("XT", n)], writes=[("XT", n)])
    P.pop()
    P.push()
    k.layer_norm(2, k.ln_tmp())
    P.pop()


def make_in_maps(inputs, n_cores=8):
    c = _consts()
    shared = {}
    for name in IN_SHAPES:
        if name == "x":
            continue
        a = np.ascontiguousarray(np.asarray(inputs[name], dtype=np.float32))
        shared[name] = a.reshape(IN_SHAPES[name])
    for name, v in c.items():
        shared["c_" + name] = np.ascontiguousarray(v)
    x = np.asarray(inputs["x"], dtype=np.float32)
    maps = []
    for b in range(n_cores):
        m = dict(shared)
        m["x"] = np.ascontiguousarray(x[b])
        maps.append(m)
    return maps


def kernel(**inputs):
    nc = build()
    maps = make_in_maps(inputs)
    res = run_bass_kernel_spmd(nc, maps, core_ids=list(range(8)))
    return np.stack([np.asarray(r["out"], dtype=np.float32) for r in res.results], axis=0)
```
